# Optimizing a Trainium2 kernel written in Bass

```python
import math
import jax, jax.numpy as jnp
from jax import lax
import numpy as np

D_MODEL = 1024
BATCH = 4
SEQ = 8192
DEPTH = 1

CHUNK = 64
Q_BLOCK = 128
EPS = 1e-6
ROPE_THETA = 10000.0

DA_HEADS = 4
DA_HEAD_DIM = 64
DA_V_DIM = 2 * DA_HEAD_DIM
DA_QK_WIDTH = DA_HEADS * 2 * DA_HEAD_DIM
DA_WIDTH = DA_HEADS * DA_V_DIM

HG_HEADS = 4
HG_KEY_DIM = 128
HG_V_DIM = 128
HG_WIDTH = HG_HEADS * HG_KEY_DIM

N_BRANCHES = 2
IN_SPLITS = [DA_QK_WIDTH, DA_QK_WIDTH, DA_WIDTH,
             HG_WIDTH, HG_WIDTH, HG_HEADS * HG_V_DIM, HG_HEADS * HG_V_DIM,
             D_MODEL, D_MODEL]
IN_COLS = sum(IN_SPLITS)

N_EXPERTS = 32
TOP_K = 4
D_EXPERT = D_MODEL
SWIGLU_LIMIT = 7.0
SWIGLU_ALPHA = 1.702
MOE_BLOCK = 256

kernel_name = "hybrid_diffattn_hgrn2_moe_block"


def rms_norm(x, w):
    xf = x.astype(jnp.float32)
    y = xf * lax.rsqrt(jnp.mean(xf * xf, axis=-1, keepdims=True) + EPS)
    return (y * w.astype(jnp.float32)).astype(x.dtype)


def rope_tables(seq, dtype):
    pos = jnp.arange(seq, dtype=jnp.float32)
    inv = ROPE_THETA ** (-jnp.arange(0, DA_HEAD_DIM, 2, dtype=jnp.float32) / DA_HEAD_DIM)
    ang = pos[:, None] * inv[None, :]
    cos = jnp.cos(ang)[:, None, None, :].astype(dtype)
    sin = jnp.sin(ang)[:, None, None, :].astype(dtype)
    return cos, sin


def apply_rope(x, cos, sin):
    x1, x2 = jnp.split(x, 2, axis=-1)
    return jnp.concatenate([x1 * cos - x2 * sin, x2 * cos + x1 * sin], axis=-1)


def diff_attention(q, k, v, lam, lambda_init, subln_w, cos, sin):
    B, S = q.shape[0], q.shape[1]
    q = apply_rope(q, cos, sin).transpose(0, 2, 3, 1, 4)
    k = apply_rope(k, cos, sin).transpose(0, 2, 3, 1, 4)
    v = v.transpose(0, 2, 1, 3)
    nb = S // Q_BLOCK
    q_blocks = q.reshape(B, DA_HEADS, 2, nb, Q_BLOCK, DA_HEAD_DIM).transpose(3, 0, 1, 2, 4, 5)
    key_chunk = jnp.arange(S) // CHUNK
    scale = DA_HEAD_DIM ** -0.5
    neg = jnp.finfo(jnp.float32).min

    def one_block(args):
        qb, bi = args
        s = jnp.einsum('bhmqd,bhmkd->bhmqk', qb, k).astype(jnp.float32) * scale
        q_chunk = (bi * Q_BLOCK + jnp.arange(Q_BLOCK)) // CHUNK
        mask = key_chunk[None, :] <= q_chunk[:, None]
        p = jax.nn.softmax(jnp.where(mask, s, neg), axis=-1)
        a = p[:, :, 0] - lam * p[:, :, 1]
        return jnp.einsum('bhqk,bhkv->bhqv', a.astype(v.dtype), v)

    o = lax.map(one_block, (q_blocks, jnp.arange(nb)))
    o = o.transpose(1, 0, 3, 2, 4).reshape(B, S, DA_HEADS, DA_V_DIM)
    o = rms_norm(o, subln_w) * (1.0 - lambda_init)
    return o.reshape(B, S, DA_WIDTH)


def hgrn2(q_raw, f_raw, i_in, g_raw, lb, norm_w):
    B, S = q_raw.shape[0], q_raw.shape[1]
    n = S // CHUNK
    lb = lb.reshape(HG_HEADS, HG_KEY_DIM)
    q = jax.nn.silu(q_raw.astype(jnp.float32))
    f = lb + (1.0 - lb) * jax.nn.sigmoid(f_raw.astype(jnp.float32))
    k = 1.0 - f
    logf = jnp.log(f)
    v = i_in.astype(jnp.float32)

    def to_chunks(t):
        return t.reshape(B, n, CHUNK, HG_HEADS, t.shape[-1]).transpose(0, 3, 1, 2, 4)

    q, k, v, logf = to_chunks(q), to_chunks(k), to_chunks(v), to_chunks(logf)
    G = jnp.cumsum(logf, axis=3)
    G_last = G[:, :, :, -1:, :]
    q_t = q * jnp.exp(G)
    k_t = k * jnp.exp(-G)
    k_dec = k * jnp.exp(G_last - G)
    causal = jnp.tril(jnp.ones((CHUNK, CHUNK), dtype=bool))
    A = jnp.where(causal, jnp.einsum('bhncd,bhnsd->bhncs', q_t, k_t), 0.0)
    o_intra = jnp.einsum('bhncs,bhnsv->bhncv', A, v)
    dS = jnp.einsum('bhncd,bhncv->bhndv', k_dec, v)
    decay = jnp.exp(G_last[:, :, :, 0, :])

    def step(state, inp):
        dec, ds = inp
        return dec[..., None] * state + ds, state

    init = jnp.zeros((B, HG_HEADS, HG_KEY_DIM, HG_V_DIM), jnp.float32)
    _, S_prev = lax.scan(step, init, (decay.transpose(2, 0, 1, 3), dS.transpose(2, 0, 1, 3, 4)))
    o_inter = jnp.einsum('bhncd,nbhdv->bhncv', q_t, S_prev)
    o = (o_intra + o_inter).transpose(0, 2, 3, 1, 4).reshape(B, S, HG_HEADS, HG_V_DIM)
    o = rms_norm(o, norm_w) * jax.nn.silu(g_raw.astype(jnp.float32))
    return o.reshape(B, S, HG_HEADS * HG_V_DIM).astype(q_raw.dtype)


def hybrid_mixer(h, w_in, lam, lambda_init, da_subln, lb, hg_norm,
                 w_branch_a, w_branch_b, w_out, cos, sin):
    B, S, _ = h.shape
    proj = h @ w_in
    da_q, da_k, da_v, hg_q, hg_f, hg_i, hg_g, gate_a, gate_b = jnp.split(
        proj, np.cumsum(IN_SPLITS)[:-1].tolist(), axis=-1)
    ya = diff_attention(da_q.reshape(B, S, DA_HEADS, 2, DA_HEAD_DIM),
                        da_k.reshape(B, S, DA_HEADS, 2, DA_HEAD_DIM),
                        da_v.reshape(B, S, DA_HEADS, DA_V_DIM),
                        lam, lambda_init, da_subln, cos, sin)
    hs = lambda t: t.reshape(B, S, HG_HEADS, -1)
    yb = hgrn2(hs(hg_q), hs(hg_f), hs(hg_i), hs(hg_g), lb, hg_norm)
    merged = jax.nn.sigmoid(gate_a) * (ya @ w_branch_a) + jax.nn.sigmoid(gate_b) * (yb @ w_branch_b)
    return merged @ w_out


def moe(h, router_w, router_b, w_gate_up, b_gate_up, w_down, b_down):
    B, S, D = h.shape
    T = B * S
    xf = h.reshape(T, D)
    logits = (xf @ router_w + router_b).astype(jnp.float32)
    top_v, top_i = lax.top_k(logits, TOP_K)
    gates = jax.nn.softmax(top_v, axis=-1)
    n_assign = T * TOP_K
    flat_e = top_i.reshape(-1)
    flat_tok = jnp.arange(n_assign) // TOP_K
    flat_w = gates.reshape(-1)
    order = jnp.argsort(flat_e)
    sorted_e = flat_e[order]
    counts = jnp.zeros((N_EXPERTS,), jnp.int32).at[flat_e].add(1)
    starts = jnp.cumsum(counts) - counts
    padded = ((counts + MOE_BLOCK - 1) // MOE_BLOCK) * MOE_BLOCK
    pad_ends = jnp.cumsum(padded)
    pad_starts = pad_ends - padded
    dest = pad_starts[sorted_e] + (jnp.arange(n_assign) - starts[sorted_e])
    P = ((n_assign + MOE_BLOCK - 1) // MOE_BLOCK) * MOE_BLOCK + N_EXPERTS * MOE_BLOCK
    row_tok = jnp.zeros((P,), jnp.int32).at[dest].set(flat_tok[order])
    row_w = jnp.zeros((P,), jnp.float32).at[dest].set(flat_w[order])
    n_blocks = P // MOE_BLOCK
    block_e = jnp.clip(jnp.searchsorted(pad_ends, jnp.arange(n_blocks) * MOE_BLOCK, side='right'),
                       0, N_EXPERTS - 1)

    def expert_block(args):
        toks, e = args
        xb = xf[toks]
        gu = xb @ w_gate_up[e] + b_gate_up[e]
        g, u = jnp.split(gu, 2, axis=-1)
        g = jnp.minimum(g, SWIGLU_LIMIT)
        u = jnp.clip(u, -SWIGLU_LIMIT, SWIGLU_LIMIT)
        glu = g * jax.nn.sigmoid(g * SWIGLU_ALPHA)
        return ((u + 1.0) * glu) @ w_down[e] + b_down[e]

    Y = lax.map(expert_block, (row_tok.reshape(n_blocks, MOE_BLOCK), block_e)).reshape(P, D)
    out = jnp.zeros((T, D), Y.dtype).at[row_tok].add(Y * row_w[:, None].astype(Y.dtype))
    return out.reshape(B, S, D)


def setup_inputs(seed: int = 0) -> dict:
    key = jax.random.key(seed)
    ks = jax.random.split(key, 32)
    nrm = lambda k, shape, s: jax.random.normal(k, shape, jnp.float32) * s
    gain = lambda k, shape: 1.0 + 0.05 * jax.random.normal(k, shape, jnp.float32)
    L, D, E = DEPTH, D_MODEL, N_EXPERTS
    return {
        "x": nrm(ks[0], (BATCH, SEQ, D), 1.0),
        "c": nrm(ks[1], (BATCH, D), 1.0),
        "w_mod": nrm(ks[2], (L, D, 6 * D), 0.5 * D ** -0.5),
        "b_mod": nrm(ks[3], (L, 6 * D), 0.02),
        "norm_pre_mix": gain(ks[4], (L, D)),
        "norm_post_mix": gain(ks[5], (L, D)),
        "w_in": nrm(ks[6], (L, D, IN_COLS), D ** -0.5),
        "da_lambda_q1": nrm(ks[7], (L, DA_HEAD_DIM), 0.1),
        "da_lambda_k1": nrm(ks[8], (L, DA_HEAD_DIM), 0.1),
        "da_lambda_q2": nrm(ks[9], (L, DA_HEAD_DIM), 0.1),
        "da_lambda_k2": nrm(ks[10], (L, DA_HEAD_DIM), 0.1),
        "da_subln": gain(ks[11], (L, DA_V_DIM)),
        "hg_lb_logits": nrm(ks[12], (L + 1, HG_WIDTH), 0.1),
        "hg_norm": gain(ks[13], (L, HG_V_DIM)),
        "w_branch_a": nrm(ks[14], (L, DA_WIDTH, D), DA_WIDTH ** -0.5),
        "w_branch_b": nrm(ks[15], (L, HG_HEADS * HG_V_DIM, D), (HG_HEADS * HG_V_DIM) ** -0.5),
        "w_out": nrm(ks[16], (L, D, D), D ** -0.5),
        "norm_pre_ffn": gain(ks[17], (L, D)),
        "norm_post_ffn": gain(ks[18], (L, D)),
        "router_w": nrm(ks[19], (L, D, E), D ** -0.5),
        "router_b": nrm(ks[20], (L, E), 0.01),
        "w_gate_up": nrm(ks[21], (L, E, D, 2 * D_EXPERT), D ** -0.5),
        "b_gate_up": nrm(ks[22], (L, E, 2 * D_EXPERT), 0.02),
        "w_down": nrm(ks[23], (L, E, D_EXPERT, D), D_EXPERT ** -0.5),
        "b_down": nrm(ks[24], (L, E, D), 0.02),
    }


def reference(x, c, w_mod, b_mod, norm_pre_mix, norm_post_mix, w_in,
              da_lambda_q1, da_lambda_k1, da_lambda_q2, da_lambda_k2, da_subln,
              hg_lb_logits, hg_norm, w_branch_a, w_branch_b, w_out,
              norm_pre_ffn, norm_post_ffn, router_w, router_b,
              w_gate_up, b_gate_up, w_down, b_down):
    S = x.shape[1]
    cos, sin = rope_tables(S, x.dtype)
    lb_all = jnp.cumsum(jax.nn.softmax(hg_lb_logits.astype(jnp.float32), axis=0), axis=0)
    c_act = jax.nn.silu(c)
    for l in range(DEPTH):
        mod = (c_act @ w_mod[l] + b_mod[l])[:, None, :]
        sh1, sc1, g1, sh2, sc2, g2 = jnp.split(mod, 6, axis=-1)
        lambda_init = 0.8 - 0.6 * math.exp(-0.3 * l)
        lam = (jnp.exp(jnp.sum(da_lambda_q1[l] * da_lambda_k1[l]).astype(jnp.float32))
               - jnp.exp(jnp.sum(da_lambda_q2[l] * da_lambda_k2[l]).astype(jnp.float32))
               + lambda_init)
        h = rms_norm(x, norm_pre_mix[l]) * (1.0 + sc1) + sh1
        y = hybrid_mixer(h, w_in[l], lam, lambda_init, da_subln[l], lb_all[l], hg_norm[l],
                         w_branch_a[l], w_branch_b[l], w_out[l], cos, sin)
        x = x + g1 * rms_norm(y, norm_post_mix[l])
        h = rms_norm(x, norm_pre_ffn[l]) * (1.0 + sc2) + sh2
        y = moe(h, router_w[l], router_b[l], w_gate_up[l], b_gate_up[l], w_down[l], b_down[l])
        x = x + g2 * rms_norm(y, norm_post_ffn[l])
    return x
```

```python
import numpy as np
from contextlib import ExitStack
import concourse.bass as bass
import concourse.mybir as mybir
from concourse.bass_utils import run_bass_kernel_spmd

F32 = mybir.dt.float32
BF16 = mybir.dt.bfloat16
U32 = mybir.dt.uint32
I32 = mybir.dt.int32
ALU = mybir.AluOpType
AF = mybir.ActivationFunctionType

D = 1024
SEQ = 8192
NB = 4
TOWN = 4096
NT = 64
NE = 32
BLK = 512
NBLK = 63
NSLOT = NBLK * BLK
EPS = 1e-6
IN_COLS = 5632


class Buf:
    __slots__ = ("writers", "readers")

    def __init__(self):
        self.writers = {}
        self.readers = {}


class T:
    def __init__(self, t):
        self.t = t
        self.b = Buf()

    def __getitem__(self, k):
        return self.t[k]


class Prog:
    def __init__(self, nc, es, n_dma_sems=32):
        self.nc = nc
        self.eng = {"pe": nc.tensor, "act": nc.scalar, "dve": nc.vector, "pool": nc.gpsimd, "sp": nc.sync}
        self.sem = {}
        self.cnt = {}
        for k in self.eng:
            self.sem[k] = es.enter_context(nc.semaphore("sem_" + k))
            self.cnt[k] = 0
        self.rings = {}
        for rn, n in (("main", n_dma_sems), ("pre", 8), ("sw", 24)):
            self.rings[rn] = {"sem": [es.enter_context(nc.semaphore("dsem_%s%d" % (rn, i))) for i in range(n)], "cnt": [0] * n, "next": 0}
        self.seen = {k: {} for k in self.eng}
        self.nwaits = 0
        self.nops = 0

    def _wait(self, eng, tok):
        sem, val = tok
        key = id(sem)
        if self.seen[eng].get(key, 0) >= val:
            return
        self.eng[eng].wait_ge(sem, val)
        self.seen[eng][key] = val
        self.nwaits += 1

    def op(self, eng, fn, reads=(), writes=(), dma=False, ring="main"):
        pe_sem = self.sem["pe"]
        for t in reads:
            for tok in t.b.writers.values():
                if eng == "pe" and tok[0] is pe_sem:
                    continue
                self._wait(eng, tok)
        for t in writes:
            for tok in t.b.writers.values():
                if eng == "pe" and tok[0] is pe_sem:
                    continue
                self._wait(eng, tok)
            for tok in t.b.readers.values():
                if eng == "pe" and tok[0] is pe_sem:
                    continue
                self._wait(eng, tok)
        if dma:
            if eng == "pool" and ring == "main":
                ring = "sw"
            rg = self.rings[ring]
            i = rg["next"]
            rg["next"] = (i + 1) % len(rg["sem"])
            sem = rg["sem"][i]
            if rg["cnt"][i] > 0:
                self._wait(eng, (sem, rg["cnt"][i]))
            ins = fn(self.eng[eng])
            rg["cnt"][i] += 16
            ins.then_inc(sem, 16)
            tok = (sem, rg["cnt"][i])
        else:
            ins = fn(self.eng[eng])
            self.cnt[eng] += 1
            ins.then_inc(self.sem[eng], 1)
            tok = (self.sem[eng], self.cnt[eng])
        self.nops += 1
        k = id(tok[0])
        for t in reads:
            t.b.readers[k] = tok
        for t in writes:
            t.b.writers[k] = tok
            t.b.readers = {}
        return tok

    def barrier(self, engines=None):
        engines = engines or list(self.eng)
        for e in engines:
            for o in self.eng:
                if o != e and self.cnt[o] > 0:
                    self._wait(e, (self.sem[o], self.cnt[o]))
            for rg in self.rings.values():
                for i, s in enumerate(rg["sem"]):
                    if rg["cnt"][i] > 0:
                        self._wait(e, (s, rg["cnt"][i]))


def build_nc(upto=99, dbg=False):
    nc = bass.Bass("TRN2", target_bir_lowering=False)

    def din(name, shape, dt=F32):
        return nc.dram_tensor(name, list(shape), dt, kind="ExternalInput").ap()

    skind = "ExternalOutput" if dbg else "Internal"

    def dscr(name, shape, dt):
        return nc.dram_tensor(name, list(shape), dt, kind=skind).ap()

    xall = din("xall", [SEQ, D])
    cosT = din("cosT", [128, SEQ])
    sinT = din("sinT", [128, SEQ])
    flag_d = din("flag", [128, 1])
    c2_d = din("c2", [128, 8])
    w_mod = din("w_mod", [D, 6 * D])
    b_mod = din("b_mod", [1, 6 * D])
    n_pre_mix = din("norm_pre_mix", [1, D])
    n_post_mix = din("norm_post_mix", [1, D])
    w_in = din("w_in", [D, IN_COLS])
    lq1 = din("da_lambda_q1", [1, 64])
    lk1 = din("da_lambda_k1", [1, 64])
    lq2 = din("da_lambda_q2", [1, 64])
    lk2 = din("da_lambda_k2", [1, 64])
    da_subln = din("da_subln", [1, 128])
    subln_col = din("subln_col", [128, 1])
    lbl_d = din("lbl", [128, 2, 4])
    hg_norm = din("hg_norm", [1, 128])
    w_ba = din("w_branch_a", [512, D])
    w_bb = din("w_branch_b", [512, D])
    w_out = din("w_out", [D, D])
    n_pre_ffn = din("norm_pre_ffn", [1, D])
    n_post_ffn = din("norm_post_ffn", [1, D])
    router_w = din("router_w", [D, NE])
    router_b = din("router_b", [1, NE])
    if upto >= 6:
        w_gu = din("w_gate_up", [NE * D, 2 * D])
        bgu_d = din("bgu", [NE * 128, 16])
        w_dn = din("w_down", [NE * D, D])
        b_dn = din("b_down", [NE, D])
    y_out = nc.dram_tensor("y", [TOWN, D], F32, kind="ExternalOutput").ap()

    HT = dscr("HT", [NT, 128, 8, 128], BF16)
    KT = dscr("KT", [4, 128, SEQ], BF16)
    QT = dscr("QT", [4, 128, TOWN], BF16)
    VS = dscr("VS", [4, 128, NT, 130], BF16)
    YAT = dscr("YAT", [4, 128, TOWN], BF16)
    YBT = dscr("YBT", [4, 128, TOWN], BF16)
    X1 = dscr("X1", [TOWN, D], F32)
    H2TM = dscr("H2TM", [TOWN, D], BF16)
    XS = dscr("XS", [NSLOT, D], BF16)
    YS = dscr("YS", [NSLOT, D], F32)
    WGUB = nc.dram_tensor("WGUB", [NE * D, 2 * D], BF16).ap()
    WDNB = nc.dram_tensor("WDNB", [NE * D, D], BF16).ap()
    if dbg:
        GDBG = dscr("GDBG", [128, 32, NE], F32)
        RDBG = dscr("RDBG", [128, 32 * 4 + 32 * 4 + 64], F32)

    w_in_v = w_in.rearrange("(kc p) n -> p kc n", p=128)

    es = ExitStack()
    with es:
        P = Prog(nc, es)

        def sb(stack, name, shape, dt):
            return T(stack.enter_context(nc.sbuf_tensor(name, list(shape), dt)))

        def ps(stack, name, shape, dt):
            return T(stack.enter_context(nc.psum_tensor(name, list(shape), dt)))

        def rstd_from_ss(ss, n, tmp):
            P.op("dve", lambda e: e.tensor_scalar(out=tmp[:, 0:1], in0=ss[:, 0:1], scalar1=1.0 / n, scalar2=EPS,
                                                  op0=ALU.mult, op1=ALU.add), reads=[ss], writes=[tmp])
            P.op("act", lambda e: e.activation(out=tmp[:, 0:1], in_=tmp[:, 0:1], func=AF.Ln), reads=[tmp], writes=[tmp])
            P.op("act", lambda e: e.activation(out=ss[:, 0:1], in_=tmp[:, 0:1], func=AF.Exp, scale=-0.5), reads=[tmp], writes=[ss])

        ident = sb(es, "ident", [128, 128], BF16)
        P.op("pool", lambda e: e.memset(ident[:], 1.0), writes=[ident])
        P.op("pool", lambda e: e.affine_select(out=ident[:], in_=ident[:], pattern=[[-1, 128]], compare_op=ALU.is_equal,
                                               fill=0.0, base=0, channel_multiplier=1), reads=[ident], writes=[ident])
        flag = sb(es, "flag_t", [128, 1], F32)
        P.op("sp", lambda e: e.dma_start(out=flag[:], in_=flag_d[:, :]), writes=[flag], dma=True)
        nlam = sb(es, "nlam", [128, 1], F32)
        subln_b = sb(es, "subln_b", [128, 128], F32)
        hgn_b = sb(es, "hgn_b", [128, 128], F32)
        lb = sb(es, "lb", [128, 4], F32)
        oml = sb(es, "oml", [128, 4], F32)
        Gall = sb(es, "Gall", [128, 32, NE], F32)
        dest4u = sb(es, "dest4u", [128, 32 * 4], U32)
        G4 = sb(es, "G4", [128, 32, 4], F32)
        OFFW = sb(es, "OFFW", [128, NBLK, 8], U32)
        OFFB = sb(es, "OFFB", [128, NBLK], U32)
        OFFD = sb(es, "OFFD", [128, NBLK], U32)
        G2t = sb(es, "G2t", [128, D], F32)
        mes = ExitStack()
        modb = sb(mes, "modb", [128, 6 * D], F32)
        B1 = lambda: modb[:, 0:D]
        A1 = lambda: modb[:, D:2 * D]
        G1 = lambda: modb[:, 2 * D:3 * D]
        B2 = lambda: modb[:, 3 * D:4 * D]
        A2 = lambda: modb[:, 4 * D:5 * D]
        G2 = lambda: modb[:, 5 * D:6 * D]

        with ExitStack() as ph:
            c2 = sb(ph, "c2t", [128, 8], F32)
            cb = sb(ph, "cb", [128, 8, 128], F32)
            ones1 = sb(ph, "ones1", [1, 128], F32)
            wm = [sb(ph, "wm%d" % i, [128, 8, 512], F32) for i in range(2)]
            bm = [sb(ph, "bm%d" % i, [1, 512], F32) for i in range(2)]
            pmod = [ps(ph, "pmod%d" % i, [128, 512], F32) for i in range(2)]
            nb4 = [sb(ph, "nb%d" % i, [128, D], F32) for i in range(4)]
            l4 = sb(ph, "l4", [128, 4, 64], F32)
            lt = sb(ph, "lt", [128, 2, 64], F32)
            ls = sb(ph, "ls", [128, 2], F32)
            lbl = sb(ph, "lblt", [128, 2, 4], F32)
            P.op("sp", lambda e: e.dma_start(out=c2[:], in_=c2_d[:, :]), writes=[c2], dma=True)
            P.op("act", lambda e: e.activation(out=c2[:], in_=c2[:], func=AF.Silu), reads=[c2], writes=[c2])
            P.op("dve", lambda e: e.tensor_copy(out=cb[:], in_=c2[:].unsqueeze(2).to_broadcast([128, 8, 128])), reads=[c2], writes=[cb])
            P.op("pool", lambda e: e.memset(ones1[:], 1.0), writes=[ones1])
            w_mod_v = w_mod.rearrange("(kc p) n -> p kc n", p=128)
            for ci in range(12):
                w_ = wm[ci % 2]
                b_ = bm[ci % 2]
                pm_ = pmod[ci % 2]
                P.op("sp", lambda e: e.dma_start(out=w_[:], in_=w_mod_v[:, :, ci * 512:(ci + 1) * 512]), writes=[w_], dma=True)
                P.op("sp", lambda e: e.dma_start(out=b_[:], in_=b_mod[0:1, ci * 512:(ci + 1) * 512]), writes=[b_], dma=True)
                for kc in range(8):
                    P.op("pe", lambda e: e.matmul(pm_[:], lhsT=cb[:, kc, :], rhs=w_[:, kc, :], start=(kc == 0), stop=False),
                         reads=[cb, w_], writes=[pm_])
                P.op("pe", lambda e: e.matmul(pm_[:], lhsT=ones1[0:1, :], rhs=b_[0:1, :], start=False, stop=True),
                     reads=[ones1, b_], writes=[pm_])
                P.op("dve", lambda e: e.tensor_copy(out=modb[:, ci * 512:(ci + 1) * 512], in_=pm_[:]), reads=[pm_], writes=[modb])
            for i, src in enumerate([n_pre_mix, n_post_mix, n_pre_ffn, n_post_ffn]):
                P.op("sp", lambda e: e.dma_start(out=nb4[i][:], in_=src[0:1, :].partition_broadcast(128)), writes=[nb4[i]], dma=True)
            P.op("dve", lambda e: e.scalar_tensor_tensor(out=A1(), in0=A1(), scalar=1.0, in1=nb4[0][:], op0=ALU.add, op1=ALU.mult),
                 reads=[modb, nb4[0]], writes=[modb])
            P.op("dve", lambda e: e.tensor_tensor(out=G1(), in0=G1(), in1=nb4[1][:], op=ALU.mult), reads=[modb, nb4[1]], writes=[modb])
            P.op("dve", lambda e: e.scalar_tensor_tensor(out=A2(), in0=A2(), scalar=1.0, in1=nb4[2][:], op0=ALU.add, op1=ALU.mult),
                 reads=[modb, nb4[2]], writes=[modb])
            P.op("dve", lambda e: e.tensor_tensor(out=G2(), in0=G2(), in1=nb4[3][:], op=ALU.mult), reads=[modb, nb4[3]], writes=[modb])
            for i, src in enumerate([lq1, lk1, lq2, lk2]):
                P.op("sp", lambda e: e.dma_start(out=l4[:, i, :], in_=src[0:1, :].partition_broadcast(128)), writes=[l4], dma=True)
            P.op("dve", lambda e: e.tensor_tensor(out=lt[:, 0, :], in0=l4[:, 0, :], in1=l4[:, 1, :], op=ALU.mult), reads=[l4], writes=[lt])
            P.op("dve", lambda e: e.tensor_tensor(out=lt[:, 1, :], in0=l4[:, 2, :], in1=l4[:, 3, :], op=ALU.mult), reads=[l4], writes=[lt])
            P.op("dve", lambda e: e.reduce_sum(out=ls[:], in_=lt[:], axis=mybir.AxisListType.X), reads=[lt], writes=[ls])
            P.op("act", lambda e: e.activation(out=ls[:], in_=ls[:], func=AF.Exp), reads=[ls], writes=[ls])
            P.op("dve", lambda e: e.tensor_tensor(out=nlam[:], in0=ls[:, 1:2], in1=ls[:, 0:1], op=ALU.subtract), reads=[ls], writes=[nlam])
            P.op("dve", lambda e: e.tensor_scalar(out=nlam[:], in0=nlam[:], scalar1=-0.2, scalar2=None, op0=ALU.add), reads=[nlam], writes=[nlam])
            P.op("sp", lambda e: e.dma_start(out=subln_b[:], in_=da_subln[0:1, :].partition_broadcast(128)), writes=[subln_b], dma=True)
            P.op("dve", lambda e: e.tensor_scalar(out=subln_b[:], in0=subln_b[:], scalar1=0.8, scalar2=None, op0=ALU.mult), reads=[subln_b], writes=[subln_b])
            P.op("sp", lambda e: e.dma_start(out=hgn_b[:], in_=hg_norm[0:1, :].partition_broadcast(128)), writes=[hgn_b], dma=True)
            P.op("sp", lambda e: e.dma_start(out=lbl[:], in_=lbl_d[:, :, :]), writes=[lbl], dma=True)
            P.op("dve", lambda e: e.tensor_tensor(out=lb[:], in0=lbl[:, 0, :], in1=lbl[:, 1, :], op=ALU.subtract), reads=[lbl], writes=[lb])
            P.op("act", lambda e: e.activation(out=lb[:], in_=lb[:], func=AF.Sigmoid), reads=[lb], writes=[lb])
            P.op("dve", lambda e: e.tensor_scalar(out=oml[:], in0=lb[:], scalar1=-1.0, scalar2=1.0, op0=ALU.mult, op1=ALU.add), reads=[lb], writes=[oml])
            P.barrier()

        if upto >= 1:
            with ExitStack() as ph:
                xt = [sb(ph, "xt%d" % i, [128, D], F32) for i in range(3)]
                junk = sb(ph, "junk", [128, D], F32)
                tmp = sb(ph, "tmp", [128, D], F32)
                hb = [sb(ph, "hb%d" % i, [128, D], BF16) for i in range(2)]
                hTt = [sb(ph, "hTt%d" % i, [128, 8, 128], BF16) for i in range(2)]
                ss = [sb(ph, "ss%d" % i, [128, 1], F32) for i in range(2)]
                st = [sb(ph, "st%d" % i, [128, 1], F32) for i in range(2)]
                pT = [ps(ph, "pT%d" % i, [128, 8, 128], BF16) for i in range(2)]

                def ld(i):
                    P.op("sp", lambda e: e.dma_start(out=xt[i % 3][:], in_=xall[i * 128:(i + 1) * 128, :]), writes=[xt[i % 3]], dma=True)

                ld(0)
                ld(1)

                def stA(i):
                    x_ = xt[i % 3]
                    s_ = ss[i % 2]
                    t_ = st[i % 2]
                    h_ = hb[i % 2]
                    P.op("act", lambda e: e.activation(out=junk[:], in_=x_[:], func=AF.Square, accum_out=s_[:, 0:1]), reads=[x_], writes=[junk, s_])
                    rstd_from_ss(s_, D, t_)
                    P.op("dve", lambda e: e.scalar_tensor_tensor(out=tmp[:], in0=x_[:], scalar=s_[:, 0:1], in1=A1(), op0=ALU.mult, op1=ALU.mult),
                         reads=[x_, s_, modb], writes=[tmp])
                    P.op("dve", lambda e: e.tensor_tensor(out=h_[:], in0=tmp[:], in1=B1(), op=ALU.add), reads=[tmp, modb], writes=[h_])

                stA(0)
                for i in range(NT):
                    if i + 2 < NT:
                        ld(i + 2)
                    if i + 1 < NT:
                        stA(i + 1)
                    h_ = hb[i % 2]
                    p_ = pT[i % 2]
                    o_ = hTt[i % 2]
                    for kc in range(8):
                        P.op("pe", lambda e: e.transpose(out=p_[:, kc, :], in_=h_[:, kc * 128:(kc + 1) * 128], identity=ident[:]),
                             reads=[h_, ident], writes=[p_])
                    P.op("act", lambda e: e.copy(out=o_[:], in_=p_[:]), reads=[p_], writes=[o_])
                    P.op("sp", lambda e: e.dma_start(out=HT[i], in_=o_[:]), reads=[o_], dma=True)
                P.barrier()

        if upto >= 2:
            with ExitStack() as ph:
                wq = sb(ph, "wq", [128, 8, 512], BF16)
                wk = sb(ph, "wk", [128, 8, 512], BF16)
                wv = sb(ph, "wv", [128, 8, 512], BF16)
                wqs = sb(ph, "wqs", [128, 8, 512], BF16)
                wks = sb(ph, "wks", [128, 8, 512], BF16)
                for w_, c0 in ((wq, 0), (wk, 512), (wv, 1024)):
                    for kh in range(2):
                        P.op("pool", lambda e: e.dma_start(out=w_[:, kh * 4:(kh + 1) * 4, :], in_=w_in_v[:, kh * 4:(kh + 1) * 4, c0:c0 + 512]),
                             writes=[w_], dma=True)
                for w_, ws_ in ((wq, wqs), (wk, wks)):
                    src = w_[:].rearrange("p k (g two j) -> p k g two j", two=2, j=32)
                    dst = ws_[:].rearrange("p k (g two j) -> p k g two j", two=2, j=32)
                    for kc in range(8):
                        P.op("dve", lambda e: e.tensor_copy(out=dst[:, kc, :, 0, :], in_=src[:, kc, :, 1, :]), reads=[w_], writes=[ws_])
                        P.op("dve", lambda e: e.tensor_copy(out=dst[:, kc, :, 1, :], in_=src[:, kc, :, 0, :]), reads=[w_], writes=[ws_])
                hTb = [sb(ph, "hTb%d" % i, [128, 4, 8, 128], BF16) for i in range(2)]
                cs = [sb(ph, "cs%d" % i, [128, 512], F32) for i in range(2)]
                sn = [sb(ph, "sn%d" % i, [128, 512], F32) for i in range(2)]
                t1 = [sb(ph, "t1_%d" % i, [128, 512], F32) for i in range(2)]
                t2 = [sb(ph, "t2_%d" % i, [128, 512], F32) for i in range(2)]
                kts = [sb(ph, "kts%d" % i, [128, 4, 512], BF16) for i in range(2)]
                qts = [sb(ph, "qts%d" % i, [128, 4, 512], BF16) for i in range(2)]
                vst = [sb(ph, "vst%d" % i, [128, 4, 4, 130], BF16) for i in range(2)]
                pA = [ps(ph, "pA%d" % i, [128, 512], F32) for i in range(2)]
                pB = [ps(ph, "pB%d" % i, [128, 512], F32) for i in range(2)]
                pV = [ps(ph, "pV%d" % i, [128, 512], F32) for i in range(2)]
                for v_ in vst:
                    P.op("pool", lambda e: e.memset(v_[:], 0.0), writes=[v_])

                def ld2(bi):
                    P.op("sp", lambda e: e.dma_start(out=hTb[bi % 2][:], in_=HT[bi * 4:(bi + 1) * 4].rearrange("t p k j -> p t k j")),
                         writes=[hTb[bi % 2]], dma=True)
                    P.op("sp", lambda e: e.dma_start(out=cs[bi % 2][:], in_=cosT[:, bi * 512:(bi + 1) * 512]), writes=[cs[bi % 2]], dma=True)
                    P.op("sp", lambda e: e.dma_start(out=sn[bi % 2][:], in_=sinT[:, bi * 512:(bi + 1) * 512]), writes=[sn[bi % 2]], dma=True)

                ld2(0)
                cnt = 0
                for bi in range(16):
                    if bi + 1 < 16:
                        ld2(bi + 1)
                    own = bi >= 8
                    hb_ = hTb[bi % 2]
                    cs_ = cs[bi % 2]
                    sn_ = sn[bi % 2]
                    jobs = [(wk, wks, kts[bi % 2])]
                    if own:
                        jobs.append((wq, wqs, qts[bi % 2]))
                    for (w_, ws_, dst_) in jobs:
                        for h in range(4):
                            a_ = pA[cnt % 2]
                            b_ = pB[cnt % 2]
                            u1 = t1[cnt % 2]
                            u2 = t2[cnt % 2]
                            cnt += 1
                            for kc in range(8):
                                P.op("pe", lambda e: e.matmul(a_[:].rearrange("p (t j) -> p t j", j=128), lhsT=w_[:, kc, h * 128:(h + 1) * 128],
                                                              rhs=hb_[:, :, kc, :], start=(kc == 0), stop=(kc == 7)), reads=[w_, hb_], writes=[a_])
                            for kc in range(8):
                                P.op("pe", lambda e: e.matmul(b_[:].rearrange("p (t j) -> p t j", j=128), lhsT=ws_[:, kc, h * 128:(h + 1) * 128],
                                                              rhs=hb_[:, :, kc, :], start=(kc == 0), stop=(kc == 7)), reads=[ws_, hb_], writes=[b_])
                            P.op("dve", lambda e: e.tensor_tensor(out=u1[:], in0=a_[:], in1=cs_[:], op=ALU.mult), reads=[a_, cs_], writes=[u1])
                            P.op("dve", lambda e: e.tensor_tensor(out=u2[:], in0=b_[:], in1=sn_[:], op=ALU.mult), reads=[b_, sn_], writes=[u2])
                            P.op("dve", lambda e: e.tensor_tensor(out=dst_[:, h, :], in0=u1[:], in1=u2[:], op=ALU.add), reads=[u1, u2], writes=[dst_])
                    for h in range(4):
                        P.op("sp", lambda e: e.dma_start(out=KT[h, :, bi * 512:(bi + 1) * 512], in_=kts[bi % 2][:, h, :]), reads=[kts[bi % 2]], dma=True)
                        if own:
                            P.op("sp", lambda e: e.dma_start(out=QT[h, :, (bi - 8) * 512:(bi - 7) * 512], in_=qts[bi % 2][:, h, :]),
                                 reads=[qts[bi % 2]], dma=True)
                    v_ = vst[bi % 2]
                    for sub in range(4):
                        p_ = pV[sub % 2]
                        for kc in range(8):
                            P.op("pe", lambda e: e.matmul(p_[:], lhsT=hb_[:, sub, kc, :], rhs=wv[:, kc, :], start=(kc == 0), stop=(kc == 7)),
                                 reads=[hb_, wv], writes=[p_])
                        pv3 = p_[:].rearrange("p (h v) -> p h v", v=128)
                        if own:
                            P.op("act", lambda e: e.copy(out=v_[:, sub, :, 0:128], in_=pv3), reads=[p_], writes=[v_])
                        else:
                            P.op("dve", lambda e: e.tensor_scalar(out=v_[:, sub, :, 0:128], in0=pv3, scalar1=flag[:, 0:1], scalar2=None, op0=ALU.mult),
                                 reads=[p_, flag], writes=[v_])
                    if own:
                        P.op("pool", lambda e: e.memset(v_[:, :, :, 128:129], 1.0), writes=[v_])
                    else:
                        P.op("dve", lambda e: e.tensor_copy(out=v_[:, :, :, 128:129], in_=flag[:, 0:1].unsqueeze(1).unsqueeze(1).to_broadcast([128, 4, 4, 1])),
                             reads=[flag], writes=[v_])
                    for h in range(4):
                        P.op("sp", lambda e: e.dma_start(out=VS[h, :, bi * 4:(bi + 1) * 4, :], in_=v_[:, :, h, :]), reads=[v_], dma=True)
                P.barrier()

        if upto >= 3:
            with ExitStack() as ph:
                KTh = [sb(ph, "KTh%d" % i, [128, SEQ], BF16) for i in range(2)]
                QTh = [sb(ph, "QTh%d" % i, [128, 2, TOWN], BF16) for i in range(2)]
                for q_ in QTh:
                    P.op("dve", lambda e: e.memset(q_[64:128, 0, :], 0.0), writes=[q_])
                    P.op("dve", lambda e: e.memset(q_[0:64, 1, :], 0.0), writes=[q_])
                Vh = [sb(ph, "Vh%d" % i, [128, NT, 130], BF16) for i in range(2)]
                PT = [sb(ph, "PT%d" % i, [128, 512], BF16) for i in range(6)]
                Sps = [ps(ph, "Sps%d" % i, [128, 512], F32) for i in range(4)]
                OT1 = [ps(ph, "OT_%d" % m, [128, 512], F32) for m in range(2)]
                OT = [OT1, OT1]
                sumP = [ps(ph, "sumP_%d" % m, [128, 512], F32) for m in range(2)]
                pE = OT1[1]
                o0s = sb(ph, "o0s", [128, 512], F32)
                o1s = sb(ph, "o1s", [128, 512], F32)
                racc = [[sb(ph, "racc%d_%d" % (jp, m), [128, 512], F32) for m in range(2)] for jp in range(2)]
                ones32 = sb(ph, "ones32", [128, 128], F32)
                subcol = sb(ph, "subcol", [128, 1], F32)
                rr = [sb(ph, "rr%d" % i, [128, 512], F32) for i in range(2)]
                tq = sb(ph, "tq", [128, 512], F32)
                uq = sb(ph, "uq", [128, 512], F32)
                oq = sb(ph, "oq", [128, 512], F32)
                o2 = sb(ph, "o2", [128, 512], F32)
                rs = sb(ph, "rs", [128, 512], F32)
                yaTs = [sb(ph, "yaTs%d" % i, [128, 512], BF16) for i in range(2)]
                P.op("pool", lambda e: e.memset(ones32[:], 1.0), writes=[ones32])
                onesB = sb(ph, "onesB", [128, 128], BF16)
                onesF = sb(ph, "onesF", [128, 128], BF16)
                P.op("pool", lambda e: e.memset(onesB[:], 1.0), writes=[onesB])
                P.op("dve", lambda e: e.tensor_scalar(out=onesF[:], in0=ones32[:], scalar1=flag[:, 0:1], scalar2=None, op0=ALU.mult),
                     reads=[ones32, flag], writes=[onesF])
                P.op("sp", lambda e: e.dma_start(out=subcol[:], in_=subln_col[:, :]), writes=[subcol], dma=True)
                P.op("dve", lambda e: e.tensor_scalar(out=subcol[:], in0=subcol[:], scalar1=0.8, scalar2=None, op0=ALU.mult), reads=[subcol], writes=[subcol])

                def ld3(h):
                    P.op("sp", lambda e: e.dma_start(out=KTh[h % 2][:], in_=KT[h]), writes=[KTh[h % 2]], dma=True)
                    P.op("sp", lambda e: e.dma_start(out=QTh[h % 2][0:64, 0, :], in_=QT[h, 0:64, :]), writes=[QTh[h % 2]], dma=True)
                    P.op("sp", lambda e: e.dma_start(out=QTh[h % 2][64:128, 1, :], in_=QT[h, 64:128, :]), writes=[QTh[h % 2]], dma=True)
                    P.op("sp", lambda e: e.dma_start(out=Vh[h % 2][:], in_=VS[h]), writes=[Vh[h % 2]], dma=True)

                ld3(0)
                if upto >= 6:
                    stg = [sb(ph, "stg%d" % i, [128, 4, 2 * D], BF16) for i in range(2)]
                    ci = 0
                    WG4 = WGUB.rearrange("(e p k) n -> e p k n", p=128, k=8)
                    WD4 = WDNB.rearrange("(e p k) n -> e p k n", p=128, k=8)
                    for c in range(NE * D // 512):
                        st_ = stg[ci % 2]
                        ci += 1
                        P.op("pool", lambda e: e.dma_start(out=st_[:], in_=w_gu[c * 512:(c + 1) * 512, :].rearrange("(a p) n -> p a n", p=128)),
                             writes=[st_], dma=True, ring="pre")
                        P.op("pool", lambda e: e.dma_start(out=WG4[c // 2, :, (c % 2) * 4:(c % 2) * 4 + 4, :], in_=st_[:]),
                             reads=[st_], dma=True, ring="pre")
                    for c in range(NE):
                        st_ = stg[ci % 2]
                        ci += 1
                        v_ = st_[:].rearrange("p a (b n) -> p (a b) n", b=2)
                        P.op("pool", lambda e: e.dma_start(out=v_, in_=w_dn[c * 1024:(c + 1) * 1024, :].rearrange("(a p) n -> p a n", p=128)),
                             writes=[st_], dma=True, ring="pre")
                        P.op("pool", lambda e: e.dma_start(out=WD4[c, :, :, :], in_=v_), reads=[st_], dma=True, ring="pre")
                gs_ = 0
                for h in range(4):
                    if h + 1 < 4:
                        ld3(h + 1)
                    K_ = KTh[h % 2]
                    Q_ = QTh[h % 2]
                    V_ = Vh[h % 2]
                    steps = []
                    for j in range(8):
                        for m in range(2):
                            kts = list(range(32)) + [32 + t for t in range(4 * j + 4)]
                            for idx, kt in enumerate(kts):
                                steps.append((j, m, idx, kt, idx == len(kts) - 1))

                    def geom(st):
                        j, m, idx, kt, last = st
                        own_t = kt - 32
                        dsub = own_t - 4 * j if own_t >= 4 * j else -1
                        return dsub, max(dsub, 0) * 128

                    def front(si):
                        j, m, idx, kt, last = steps[si]
                        dsub, c0 = geom(steps[si])
                        sp_ = Sps[(gs_ + si) % 4]
                        pt_ = PT[(gs_ + si) % 6]
                        P.op("pe", lambda e: e.matmul(sp_[:, c0:512], lhsT=K_[:, kt * 128:(kt + 1) * 128],
                                                      rhs=Q_[:, m, j * 512 + c0:(j + 1) * 512], start=True, stop=True),
                             reads=[K_, Q_], writes=[sp_])
                        P.op("act", lambda e: e.activation(out=pt_[:, c0:512], in_=sp_[:, c0:512], func=AF.Exp, scale=0.125),
                             reads=[sp_], writes=[pt_])
                        if dsub >= 0:
                            P.op("act", lambda e: e.mul(out=pt_[64:128, c0:c0 + 64], in_=pt_[64:128, c0:c0 + 64], mul=0.0), reads=[pt_], writes=[pt_])

                    def back(si):
                        j, m, idx, kt, last = steps[si]
                        dsub, c0 = geom(steps[si])
                        pt_ = PT[(gs_ + si) % 6]
                        ra = racc[j % 2][m]
                        ot = OT[j % 2][m]
                        sp2 = sumP[m]
                        if idx % 2 == 0:
                            P.op("pe", lambda e: e.matmul(sp2[:, c0:512], lhsT=(onesF if kt < 32 else onesB)[:], rhs=pt_[:, c0:512], start=(idx == 0), stop=False),
                                 reads=[onesF, onesB, pt_], writes=[sp2])
                        elif idx == 1:
                            P.op("dve", lambda e: e.tensor_scalar(out=ra[:], in0=pt_[:], scalar1=flag[:, 0:1], scalar2=None, op0=ALU.mult),
                                 reads=[pt_, flag], writes=[ra])
                        elif kt < 32:
                            P.op("dve", lambda e: e.scalar_tensor_tensor(out=ra[:], in0=pt_[:], scalar=flag[:, 0:1], in1=ra[:], op0=ALU.mult, op1=ALU.add),
                                 reads=[ra, pt_, flag], writes=[ra])
                        else:
                            P.op("dve", lambda e: e.tensor_tensor(out=ra[:, c0:512], in0=ra[:, c0:512], in1=pt_[:, c0:512], op=ALU.add),
                                 reads=[ra, pt_], writes=[ra])
                        P.op("pe", lambda e: e.matmul(ot[:, c0:512], lhsT=V_[:, kt, 0:128], rhs=pt_[:, c0:512], start=(idx == 0), stop=last),
                             reads=[V_, pt_], writes=[ot])
                        if last and m == 1:
                            epi(j)

                    def epi(j):
                        ys_ = yaTs[j % 2]
                        for m in range(2):
                            P.op("pe", lambda e: e.matmul(sumP[m][:], lhsT=ones32[:], rhs=racc[j % 2][m][:], start=False, stop=True),
                                 reads=[ones32, racc[j % 2][m]], writes=[sumP[m]])
                        P.op("act", lambda e: e.copy(out=o0s[:], in_=OT[j % 2][0][:]), reads=[OT[j % 2][0]], writes=[o0s])
                        P.op("dve", lambda e: e.reciprocal(out=rr[0][:], in_=sumP[0][:]), reads=[sumP[0]], writes=[rr[0]])
                        P.op("act", lambda e: e.copy(out=o1s[:], in_=OT[j % 2][1][:]), reads=[OT[j % 2][1]], writes=[o1s])
                        P.op("dve", lambda e: e.reciprocal(out=rr[1][:], in_=sumP[1][:]), reads=[sumP[1]], writes=[rr[1]])
                        P.op("dve", lambda e: e.tensor_tensor(out=tq[:], in0=o0s[:], in1=rr[0][:], op=ALU.mult), reads=[o0s, rr[0]], writes=[tq])
                        P.op("dve", lambda e: e.tensor_scalar(out=rr[1][:], in0=rr[1][:], scalar1=nlam[:, 0:1], scalar2=None, op0=ALU.mult),
                             reads=[rr[1], nlam], writes=[rr[1]])
                        P.op("dve", lambda e: e.tensor_tensor(out=uq[:], in0=o1s[:], in1=rr[1][:], op=ALU.mult), reads=[o1s, rr[1]], writes=[uq])
                        P.op("dve", lambda e: e.tensor_tensor(out=oq[:], in0=tq[:], in1=uq[:], op=ALU.add), reads=[tq, uq], writes=[oq])
                        P.op("dve", lambda e: e.tensor_tensor(out=o2[:], in0=oq[:], in1=oq[:], op=ALU.mult), reads=[oq], writes=[o2])
                        P.op("pe", lambda e: e.matmul(pE[:], lhsT=ones32[:], rhs=o2[:], start=True, stop=True), reads=[ones32, o2], writes=[pE])
                        P.op("act", lambda e: e.activation(out=rs[:], in_=pE[:], func=AF.Ln, bias=EPS, scale=1.0 / 128), reads=[pE], writes=[rs])
                        P.op("act", lambda e: e.activation(out=rs[:], in_=rs[:], func=AF.Exp, scale=-0.5), reads=[rs], writes=[rs])
                        P.op("dve", lambda e: e.scalar_tensor_tensor(out=ys_[:], in0=oq[:], scalar=subcol[:, 0:1], in1=rs[:], op0=ALU.mult, op1=ALU.mult),
                             reads=[oq, subcol, rs], writes=[ys_])
                        P.op("sp", lambda e: e.dma_start(out=YAT[h, :, j * 512:(j + 1) * 512], in_=ys_[:]), reads=[ys_], dma=True)

                    n = len(steps)
                    for si in range(n + 3):
                        if si < n:
                            front(si)
                        if si >= 3:
                            back(si - 3)
                    gs_ += n
                P.barrier()

        if upto >= 4:
            with ExitStack() as ph:
                whq = sb(ph, "whq", [128, 8, 512], BF16)
                whf = sb(ph, "whf", [128, 8, 512], BF16)
                whi = sb(ph, "whi", [128, 8, 512], BF16)
                whg = sb(ph, "whg", [128, 8, 512], BF16)
                for w_, c0 in ((whq, 1536), (whf, 2048), (whi, 2560), (whg, 3072)):
                    for kh in range(2):
                        P.op("pool", lambda e: e.dma_start(out=w_[:, kh * 4:(kh + 1) * 4, :], in_=w_in_v[:, kh * 4:(kh + 1) * 4, c0:c0 + 512]),
                             writes=[w_], dma=True)
                rm = sb(ph, "rm", [128, 512], F32)
                P.op("pool", lambda e: e.memset(rm[:], 1.0), writes=[rm])
                for t in range(4):
                    P.op("pool", lambda e: e.memset(rm[:, t * 128:t * 128 + 1], 0.0), writes=[rm])
                cmask = sb(ph, "cmask", [128, 128], F32)
                P.op("pool", lambda e: e.memset(cmask[:], 1.0), writes=[cmask])
                P.op("pool", lambda e: e.affine_select(out=cmask[:], in_=cmask[:], pattern=[[1, 128]], compare_op=ALU.is_ge,
                                                       fill=0.0, base=0, channel_multiplier=-1), reads=[cmask], writes=[cmask])
                hTb = [sb(ph, "hTb4_%d" % i, [128, 4, 8, 128], BF16) for i in range(2)]
                sg = [sb(ph, "sg%d" % i, [128, 512], F32) for i in range(4)]
                lf = [sb(ph, "lf%d" % i, [128, 512], F32) for i in range(4)]
                kk = [sb(ph, "kk%d" % i, [128, 512], F32) for i in range(4)]
                Gc = [sb(ph, "Gc%d" % i, [128, 512], F32) for i in range(4)]
                eg = [sb(ph, "eg%d" % i, [128, 512], F32) for i in range(4)]
                kt32 = [sb(ph, "kt32_%d" % i, [128, 512], F32) for i in range(4)]
                qs = [sb(ph, "qs%d" % i, [128, 512], F32) for i in range(4)]
                qraw = [sb(ph, "qraw%d" % i, [128, 512], F32) for i in range(4)]
                gex = [sb(ph, "gex%d" % i, [128, 512], F32) for i in range(2)]
                dec_all = sb(ph, "dec_all", [128, 4, NT], F32)
                q_t = sb(ph, "q_t", [128, 4, 512], BF16)
                k_t = sb(ph, "k_t", [128, 4, 512], BF16)
                kdec = sb(ph, "kdec", [128, 4, 512], BF16)
                kdT = sb(ph, "kdT", [128, 4, 4, 128], BF16)
                vtm = sb(ph, "vtm", [128, 4, 512], BF16)
                gsn = sb(ph, "gsn", [128, 4, 512], F32)
                S32 = sb(ph, "S32", [128, 4, 128], F32)
                yb = [sb(ph, "yb%d" % i, [128, 512], BF16) for i in range(2)]
                ybTs = [sb(ph, "ybTs%d" % i, [128, 4, 512], BF16) for i in range(2)]
                pF = [ps(ph, "pF%d" % i, [128, 512], F32) for i in range(2)]
                pAT = [ps(ph, "pAT%d" % i, [128, 4, 128], F32) for i in range(2)]
                pO = [ps(ph, "pO%d" % i, [128, 4, 128], F32) for i in range(2)]
                pS = ps(ph, "pS", [128, 4, 128], F32)
                AT4 = [sb(ph, "AT4_%d" % i, [128, 4, 128], BF16) for i in range(2)]
                Sbf4 = sb(ph, "Sbf4", [128, 4, 128], BF16)
                sq4 = sb(ph, "sq4", [128, 4, 128], F32)
                tm4 = sb(ph, "tm4", [128, 4, 128], F32)
                ss4 = [sb(ph, "ss4_%d" % i, [128, 4], F32) for i in range(2)]
                st4 = [sb(ph, "st4_%d" % i, [128, 4], F32) for i in range(2)]
                P.op("pool", lambda e: e.memset(Sbf4[:], 0.0), writes=[Sbf4])
                P.op("dve", lambda e: e.memset(S32[:], 0.0), writes=[S32])
                pTk16 = ps(ph, "pTk16", [128, 4, 128], BF16)
                if upto >= 5:
                    zt = sb(ph, "zt", [128, 1024], BF16)
                    P.op("pool", lambda e: e.memset(zt[:], 0.0), writes=[zt])
                    for i in range(NSLOT // 128):
                        P.op("pool", lambda e: e.dma_start(out=XS[i * 128:(i + 1) * 128, :], in_=zt[:]), reads=[zt], dma=True, ring="pre")

                def ld4(bi):
                    P.op("sp", lambda e: e.dma_start(out=hTb[bi % 2][:], in_=HT[bi * 4:(bi + 1) * 4].rearrange("t p k j -> p t k j")),
                         writes=[hTb[bi % 2]], dma=True)

                ld4(0)
                cnt = 0
                oc = 0
                for bi in range(16):
                    if bi + 1 < 16:
                        ld4(bi + 1)
                    own = bi >= 8
                    hb_ = hTb[bi % 2]
                    rr3 = lambda ap: ap.rearrange("p (t j) -> p t j", j=128)
                    for h in range(4):
                        p_ = pF[cnt % 2]
                        cnt += 1
                        for kc in range(8):
                            P.op("pe", lambda e: e.matmul(rr3(p_[:]), lhsT=whf[:, kc, h * 128:(h + 1) * 128],
                                                          rhs=hb_[:, :, kc, :], start=(kc == 0), stop=(kc == 7)), reads=[whf, hb_], writes=[p_])
                        P.op("act", lambda e: e.activation(out=sg[h][:], in_=p_[:], func=AF.Exp, scale=-1.0), reads=[p_], writes=[sg[h]])
                    if own:
                        for h in range(4):
                            p_ = pF[cnt % 2]
                            cnt += 1
                            for kc in range(8):
                                P.op("pe", lambda e: e.matmul(rr3(p_[:]), lhsT=whq[:, kc, h * 128:(h + 1) * 128],
                                                              rhs=hb_[:, :, kc, :], start=(kc == 0), stop=(kc == 7)), reads=[whq, hb_], writes=[p_])
                            P.op("act", lambda e: e.copy(out=qraw[h][:], in_=p_[:]), reads=[p_], writes=[qraw[h]])
                            P.op("act", lambda e: e.activation(out=qs[h][:], in_=p_[:], func=AF.Exp, scale=-1.0), reads=[p_], writes=[qs[h]])
                    for h in range(4):
                        P.op("act", lambda e: e.activation(out=sg[h][:], in_=sg[h][:], func=AF.Ln, bias=1.0, scale=1.0), reads=[sg[h]], writes=[sg[h]])
                        P.op("act", lambda e: e.activation(out=sg[h][:], in_=sg[h][:], func=AF.Exp, scale=-1.0), reads=[sg[h]], writes=[sg[h]])
                        P.op("dve", lambda e: e.tensor_scalar(out=sg[h][:], in0=sg[h][:], scalar1=oml[:, h:h + 1], scalar2=lb[:, h:h + 1],
                                                              op0=ALU.mult, op1=ALU.add), reads=[sg[h], oml, lb], writes=[sg[h]])
                    for h in range(4):
                        P.op("act", lambda e: e.activation(out=lf[h][:], in_=sg[h][:], func=AF.Ln), reads=[sg[h]], writes=[lf[h]])
                    for h in range(4):
                        P.op("dve", lambda e: e.tensor_scalar(out=kk[h][:], in0=sg[h][:], scalar1=-1.0, scalar2=1.0, op0=ALU.mult, op1=ALU.add),
                             reads=[sg[h]], writes=[kk[h]])
                        P.op("dve", lambda e: e.tensor_tensor_scan(out=Gc[h][:], data0=rm[:], data1=lf[h][:], initial=0.0, op0=ALU.mult, op1=ALU.add),
                             reads=[rm, lf[h]], writes=[Gc[h]])
                    for h in range(4):
                        P.op("act", lambda e: e.activation(out=eg[h][:], in_=Gc[h][:], func=AF.Exp, scale=-1.0), reads=[Gc[h]], writes=[eg[h]])
                        P.op("act", lambda e: e.activation(out=dec_all[:, h, bi * 4:(bi + 1) * 4], in_=rr3(Gc[h][:])[:, :, 127], func=AF.Exp),
                             reads=[Gc[h]], writes=[dec_all])
                    for h in range(4):
                        P.op("dve", lambda e: e.tensor_tensor(out=kt32[h][:], in0=kk[h][:], in1=eg[h][:], op=ALU.mult), reads=[kk[h], eg[h]], writes=[kt32[h]])
                        P.op("dve", lambda e: e.tensor_tensor(out=rr3(kdec[:, h, :]), in0=rr3(kt32[h][:]),
                                                              in1=dec_all[:, h, bi * 4:(bi + 1) * 4].unsqueeze(2).to_broadcast([128, 4, 128]),
                                                              op=ALU.mult), reads=[kt32[h], dec_all], writes=[kdec])
                    for h in range(4):
                        for sub in range(4):
                            P.op("pe", lambda e: e.transpose(out=pTk16[:, sub, :], in_=kdec[:, h, sub * 128:(sub + 1) * 128], identity=ident[:]),
                                 reads=[kdec, ident], writes=[pTk16])
                        P.op("act", lambda e: e.copy(out=kdT[:, :, h, :], in_=pTk16[:]), reads=[pTk16], writes=[kdT])
                    if own:
                        for h in range(4):
                            P.op("act", lambda e: e.copy(out=k_t[:, h, :], in_=kt32[h][:]), reads=[kt32[h]], writes=[k_t])
                            P.op("act", lambda e: e.activation(out=eg[h][:], in_=Gc[h][:], func=AF.Exp), reads=[Gc[h]], writes=[eg[h]])
                        for h in range(4):
                            P.op("act", lambda e: e.activation(out=qs[h][:], in_=qs[h][:], func=AF.Ln, bias=1.0, scale=1.0), reads=[qs[h]], writes=[qs[h]])
                            P.op("act", lambda e: e.activation(out=qs[h][:], in_=qs[h][:], func=AF.Exp, scale=-1.0), reads=[qs[h]], writes=[qs[h]])
                            P.op("dve", lambda e: e.tensor_tensor(out=qs[h][:], in0=qs[h][:], in1=qraw[h][:], op=ALU.mult), reads=[qs[h], qraw[h]], writes=[qs[h]])
                            P.op("dve", lambda e: e.tensor_tensor(out=q_t[:, h, :], in0=qs[h][:], in1=eg[h][:], op=ALU.mult), reads=[qs[h], eg[h]], writes=[q_t])
                    for sub in range(4):
                        p_ = pF[cnt % 2]
                        cnt += 1
                        for kc in range(8):
                            P.op("pe", lambda e: e.matmul(p_[:], lhsT=hb_[:, sub, kc, :], rhs=whi[:, kc, :], start=(kc == 0), stop=(kc == 7)),
                                 reads=[hb_, whi], writes=[p_])
                        if own:
                            P.op("act", lambda e: e.copy(out=vtm[:, sub, :], in_=p_[:]), reads=[p_], writes=[vtm])
                        else:
                            P.op("dve", lambda e: e.tensor_scalar(out=vtm[:, sub, :], in0=p_[:], scalar1=flag[:, 0:1], scalar2=None, op0=ALU.mult),
                                 reads=[p_, flag], writes=[vtm])
                        if own:
                            p2 = pF[cnt % 2]
                            cnt += 1
                            gx = gex[sub % 2]
                            for kc in range(8):
                                P.op("pe", lambda e: e.matmul(p2[:], lhsT=hb_[:, sub, kc, :], rhs=whg[:, kc, :], start=(kc == 0), stop=(kc == 7)),
                                     reads=[hb_, whg], writes=[p2])
                            P.op("act", lambda e: e.activation(out=gx[:], in_=p2[:], func=AF.Exp, scale=-1.0), reads=[p2], writes=[gx])
                            P.op("act", lambda e: e.copy(out=gsn[:, sub, :], in_=p2[:]), reads=[p2], writes=[gsn])
                            P.op("act", lambda e: e.activation(out=gx[:], in_=gx[:], func=AF.Ln, bias=1.0, scale=1.0), reads=[gx], writes=[gx])
                            P.op("act", lambda e: e.activation(out=gx[:], in_=gx[:], func=AF.Exp, scale=-1.0), reads=[gx], writes=[gx])
                            P.op("dve", lambda e: e.tensor_tensor(out=gsn[:, sub, :], in0=gsn[:, sub, :], in1=gx[:], op=ALU.mult), reads=[gsn, gx], writes=[gsn])
                            P.op("dve", lambda e: e.tensor_tensor(out=gsn[:, sub, :].rearrange("p (h v) -> p h v", v=128),
                                                                  in0=gsn[:, sub, :].rearrange("p (h v) -> p h v", v=128),
                                                                  in1=hgn_b[:].unsqueeze(1).to_broadcast([128, 4, 128]), op=ALU.mult),
                                 reads=[gsn, hgn_b], writes=[gsn])
                    for sub in range(4):
                        tile = bi * 4 + sub
                        yb_ = yb[sub % 2]
                        cols = slice(sub * 128, (sub + 1) * 128)
                        if own:
                            at_p = pAT[oc % 2]
                            at_s = AT4[oc % 2]
                            o_p = pO[oc % 2]
                            s_ = ss4[oc % 2]
                            st_ = st4[oc % 2]
                            oc += 1
                            for h in range(4):
                                P.op("pe", lambda e: e.matmul(at_p[:, h, :], lhsT=k_t[:, h, cols], rhs=q_t[:, h, cols], start=True, stop=True),
                                     reads=[k_t, q_t], writes=[at_p])
                            P.op("dve", lambda e: e.tensor_tensor(out=at_s[:], in0=at_p[:], in1=cmask[:].unsqueeze(1).to_broadcast([128, 4, 128]), op=ALU.mult),
                                 reads=[at_p, cmask], writes=[at_s])
                            for h in range(4):
                                P.op("pe", lambda e: e.matmul(o_p[:, h, :], lhsT=at_s[:, h, :], rhs=vtm[:, sub, h * 128:(h + 1) * 128], start=True, stop=False),
                                     reads=[at_s, vtm], writes=[o_p])
                                P.op("pe", lambda e: e.matmul(o_p[:, h, :], lhsT=q_t[:, h, cols], rhs=Sbf4[:, h, :], start=False, stop=True),
                                     reads=[q_t, Sbf4], writes=[o_p])
                            P.op("act", lambda e: e.activation(out=sq4[:], in_=o_p[:], func=AF.Square), reads=[o_p], writes=[sq4])
                            P.op("dve", lambda e: e.reduce_sum(out=s_[:], in_=sq4[:], axis=mybir.AxisListType.X), reads=[sq4], writes=[s_])
                            P.op("dve", lambda e: e.tensor_scalar(out=st_[:], in0=s_[:], scalar1=1.0 / 128, scalar2=EPS, op0=ALU.mult, op1=ALU.add),
                                 reads=[s_], writes=[st_])
                            P.op("act", lambda e: e.activation(out=st_[:], in_=st_[:], func=AF.Ln), reads=[st_], writes=[st_])
                            P.op("act", lambda e: e.activation(out=s_[:], in_=st_[:], func=AF.Exp, scale=-0.5), reads=[st_], writes=[s_])
                            P.op("dve", lambda e: e.tensor_tensor(out=tm4[:], in0=o_p[:], in1=s_[:].unsqueeze(2).to_broadcast([128, 4, 128]), op=ALU.mult),
                                 reads=[o_p, s_], writes=[tm4])
                            P.op("dve", lambda e: e.tensor_tensor(out=yb_[:], in0=tm4[:].rearrange("p h v -> p (h v)"), in1=gsn[:, sub, :], op=ALU.mult),
                                 reads=[tm4, gsn], writes=[yb_])
                        for h in range(4):
                            P.op("pe", lambda e: e.matmul(pS[:, h, :], lhsT=kdT[:, sub, h, :], rhs=vtm[:, sub, h * 128:(h + 1) * 128], start=True, stop=True),
                                 reads=[kdT, vtm], writes=[pS])
                        P.op("dve", lambda e: e.tensor_tensor(out=S32[:], in0=S32[:], in1=dec_all[:, :, tile:tile + 1].to_broadcast([128, 4, 128]), op=ALU.mult),
                             reads=[S32, dec_all], writes=[S32])
                        P.op("dve", lambda e: e.tensor_tensor(out=S32[:], in0=S32[:], in1=pS[:], op=ALU.add), reads=[S32, pS], writes=[S32])
                        P.op("act", lambda e: e.copy(out=Sbf4[:], in_=S32[:]), reads=[S32], writes=[Sbf4])
                        if own:
                            for h in range(4):
                                P.op("pe", lambda e: e.transpose(out=pTk16[:, h, :], in_=yb_[:, h * 128:(h + 1) * 128], identity=ident[:]),
                                     reads=[yb_, ident], writes=[pTk16])
                            P.op("act", lambda e: e.copy(out=ybTs[bi % 2][:, :, sub * 128:(sub + 1) * 128], in_=pTk16[:]), reads=[pTk16], writes=[ybTs[bi % 2]])
                    if own:
                        for h in range(4):
                            P.op("sp", lambda e: e.dma_start(out=YBT[h, :, (bi - 8) * 512:(bi - 7) * 512], in_=ybTs[bi % 2][:, h, :]),
                                 reads=[ybTs[bi % 2]], dma=True)
                P.barrier()

        if upto >= 5:
            with ExitStack() as ph:
                wga = sb(ph, "wga", [128, 8, D], BF16)
                wgb = sb(ph, "wgb", [128, 8, D], BF16)
                wa = sb(ph, "wa", [128, 4, D], BF16)
                wb = sb(ph, "wb", [128, 4, D], BF16)
                wo = sb(ph, "wo", [128, 8, D], BF16)
                rw = sb(ph, "rw", [128, 8, NE], BF16)
                rbb = sb(ph, "rbb", [128, NE], F32)
                for w_, c0 in ((wga, 3584), (wgb, 4608)):
                    for kh in range(4):
                        P.op("pool", lambda e: e.dma_start(out=w_[:, kh * 2:(kh + 1) * 2, :], in_=w_in_v[:, kh * 2:(kh + 1) * 2, c0:c0 + D]),
                             writes=[w_], dma=True)
                P.op("pool", lambda e: e.dma_start(out=wa[:], in_=w_ba.rearrange("(h p) n -> p h n", p=128)), writes=[wa], dma=True)
                P.op("pool", lambda e: e.dma_start(out=wb[:], in_=w_bb.rearrange("(h p) n -> p h n", p=128)), writes=[wb], dma=True)
                for kh in range(4):
                    P.op("pool", lambda e: e.dma_start(out=wo[:, kh * 2:(kh + 1) * 2, :], in_=w_out.rearrange("(kc p) n -> p kc n", p=128)[:, kh * 2:(kh + 1) * 2, :]),
                         writes=[wo], dma=True)
                P.op("pool", lambda e: e.dma_start(out=rw[:], in_=router_w.rearrange("(kc p) n -> p kc n", p=128)), writes=[rw], dma=True)
                P.op("sp", lambda e: e.dma_start(out=rbb[:], in_=router_b[0:1, :].partition_broadcast(128)), writes=[rbb], dma=True)
                hTb = [sb(ph, "hTb5_%d" % i, [128, 4, 8, 128], BF16) for i in range(2)]
                yaTb = [sb(ph, "yaTb%d" % i, [128, 4, 512], BF16) for i in range(2)]
                ybTb = [sb(ph, "ybTb%d" % i, [128, 4, 512], BF16) for i in range(2)]
                sga = [sb(ph, "sga%d" % i, [128, 512], F32) for i in range(2)]
                sgb = [sb(ph, "sgb%d" % i, [128, 512], F32) for i in range(2)]
                ta = [sb(ph, "ta%d" % i, [128, 512], F32) for i in range(2)]
                tb = [sb(ph, "tb%d" % i, [128, 512], F32) for i in range(2)]
                mT = [sb(ph, "mT%d" % i, [128, 8, 512], BF16) for i in range(2)]
                xt = [sb(ph, "xt5_%d" % i, [128, D], F32) for i in range(2)]
                tmp = sb(ph, "tmp5", [128, D], F32)
                x1 = [sb(ph, "x1_%d" % i, [128, D], F32) for i in range(2)]
                tmp2 = sb(ph, "tmp5b", [128, D], F32)
                junk = tmp2
                h2 = [sb(ph, "h2_%d" % i, [128, D], BF16) for i in range(2)]
                h2Tt = [sb(ph, "h2Tt%d" % i, [128, 8, 128], BF16) for i in range(2)]
                s2 = [sb(ph, "s5a%d" % i, [128, 2], F32) for i in range(2)]
                s3 = [sb(ph, "s5b%d" % i, [128, 1], F32) for i in range(2)]
                st5 = [sb(ph, "st5_%d" % i, [128, 1], F32) for i in range(2)]
                lg = [sb(ph, "lg%d" % i, [128, NE], F32) for i in range(2)]
                t8 = [sb(ph, "t8_%d" % i, [128, 8], F32) for i in range(2)]
                msk = [sb(ph, "msk%d" % i, [128, NE], F32) for i in range(2)]
                ex = [sb(ph, "ex%d" % i, [128, NE], F32) for i in range(2)]
                gs = [sb(ph, "gs%d" % i, [128, 2], F32) for i in range(2)]
                pG = [ps(ph, "pG%d" % i, [128, 512], F32) for i in range(2)]
                pP = [ps(ph, "pP%d" % i, [128, 512], F32) for i in range(2)]
                pY = [ps(ph, "pY%d" % i, [128, 512], F32) for i in range(2)]
                pT5 = ps(ph, "pT5", [128, 8, 128], BF16)
                pR = ps(ph, "pR", [128, 512], F32)

                def ld5(j):
                    P.op("sp", lambda e: e.dma_start(out=hTb[j % 2][:], in_=HT[32 + j * 4:32 + (j + 1) * 4].rearrange("t p k j -> p t k j")),
                         writes=[hTb[j % 2]], dma=True)
                    P.op("sp", lambda e: e.dma_start(out=yaTb[j % 2][:], in_=YAT[:, :, j * 512:(j + 1) * 512].rearrange("h p t -> p h t")),
                         writes=[yaTb[j % 2]], dma=True)
                    P.op("sp", lambda e: e.dma_start(out=ybTb[j % 2][:], in_=YBT[:, :, j * 512:(j + 1) * 512].rearrange("h p t -> p h t")),
                         writes=[ybTb[j % 2]], dma=True)

                ld5(0)
                cntb = [0, 0]

                def gate_part(j, dcs):
                    hb_ = hTb[j % 2]
                    ya_ = yaTb[j % 2]
                    yb_ = ybTb[j % 2]
                    m_ = mT[j % 2]
                    cnt = cntb[0]
                    for dc in dcs:
                        for (wg_, wbr_, y_, sg_, t_, tag) in ((wga, wa, ya_, sga, ta, 0), (wgb, wb, yb_, sgb, tb, 1)):
                            g_p = pG[cnt % 2]
                            p_p = pP[cnt % 2]
                            s_ = sg_[dc % 2]
                            u_ = t_[dc % 2]
                            cnt += 1
                            for kc in range(8):
                                P.op("pe", lambda e: e.matmul(g_p[:].rearrange("p (t j) -> p t j", j=128), lhsT=wg_[:, kc, dc * 128:(dc + 1) * 128],
                                                              rhs=hb_[:, :, kc, :], start=(kc == 0), stop=(kc == 7)), reads=[wg_, hb_], writes=[g_p])
                            P.op("act", lambda e: e.activation(out=s_[:], in_=g_p[:], func=AF.Exp, scale=-1.0), reads=[g_p], writes=[s_])
                            P.op("act", lambda e: e.activation(out=s_[:], in_=s_[:], func=AF.Ln, bias=1.0, scale=1.0), reads=[s_], writes=[s_])
                            P.op("act", lambda e: e.activation(out=s_[:], in_=s_[:], func=AF.Exp, scale=-1.0), reads=[s_], writes=[s_])
                            for h in range(4):
                                P.op("pe", lambda e: e.matmul(p_p[:], lhsT=wbr_[:, h, dc * 128:(dc + 1) * 128], rhs=y_[:, h, :],
                                                              start=(h == 0), stop=(h == 3)), reads=[wbr_, y_], writes=[p_p])
                            P.op("dve", lambda e: e.tensor_tensor(out=u_[:], in0=p_p[:], in1=s_[:], op=ALU.mult), reads=[p_p, s_], writes=[u_])
                        P.op("dve", lambda e: e.tensor_tensor(out=m_[:, dc, :], in0=ta[dc % 2][:], in1=tb[dc % 2][:], op=ALU.add),
                             reads=[ta[dc % 2], tb[dc % 2]], writes=[m_])
                    cntb[0] = cnt

                def sub_A(j, sub):
                    m_ = mT[j % 2]
                    tl = j * 4 + sub
                    if True:
                        x_ = xt[tl % 2]
                        x1_ = x1[tl % 2]
                        h2_ = h2[tl % 2]
                        hT_ = h2Tt[tl % 2]
                        sa = s2[tl % 2]
                        sb_ = s3[tl % 2]
                        st_ = st5[tl % 2]
                        lg_ = lg[tl % 2]
                        t8_ = t8[tl % 2]
                        mk_ = msk[tl % 2]
                        ex_ = ex[tl % 2]
                        gs_ = gs[tl % 2]
                        P.op("sp", lambda e: e.dma_start(out=x_[:], in_=xall[TOWN + tl * 128:TOWN + (tl + 1) * 128, :]), writes=[x_], dma=True)
                        for half in range(2):
                            for kc in range(8):
                                P.op("pe", lambda e: e.matmul(pY[half][:], lhsT=m_[:, kc, sub * 128:(sub + 1) * 128], rhs=wo[:, kc, half * 512:(half + 1) * 512],
                                                              start=(kc == 0), stop=(kc == 7)), reads=[m_, wo], writes=[pY[half]])
                            P.op("act", lambda e: e.activation(out=junk[:, half * 512:(half + 1) * 512], in_=pY[half][:], func=AF.Square,
                                                               accum_out=sa[:, half:half + 1]), reads=[pY[half]], writes=[junk, sa])
                        P.op("dve", lambda e: e.tensor_tensor(out=sb_[:], in0=sa[:, 0:1], in1=sa[:, 1:2], op=ALU.add), reads=[sa], writes=[sb_])
                        rstd_from_ss(sb_, D, st_)
                        for half in range(2):
                            P.op("dve", lambda e: e.scalar_tensor_tensor(out=tmp[:, half * 512:(half + 1) * 512], in0=pY[half][:], scalar=sb_[:, 0:1],
                                                                         in1=modb[:, 2 * D + half * 512:2 * D + (half + 1) * 512], op0=ALU.mult, op1=ALU.mult),
                                 reads=[pY[half], sb_, modb], writes=[tmp])
                        P.op("dve", lambda e: e.tensor_tensor(out=x1_[:], in0=tmp[:], in1=x_[:], op=ALU.add), reads=[tmp, x_], writes=[x1_])
                        P.op("sp", lambda e: e.dma_start(out=X1[tl * 128:(tl + 1) * 128, :], in_=x1_[:]), reads=[x1_], dma=True)
                        P.op("act", lambda e: e.activation(out=junk[:], in_=x1_[:], func=AF.Square, accum_out=sb_[:, 0:1]), reads=[x1_], writes=[junk, sb_])
                        rstd_from_ss(sb_, D, st_)
                        P.op("dve", lambda e: e.scalar_tensor_tensor(out=tmp2[:], in0=x1_[:], scalar=sb_[:, 0:1], in1=A2(), op0=ALU.mult, op1=ALU.mult),
                             reads=[x1_, sb_, modb], writes=[tmp2])
                        P.op("dve", lambda e: e.tensor_tensor(out=h2_[:], in0=tmp2[:], in1=B2(), op=ALU.add), reads=[tmp2, modb], writes=[h2_])

                def sub_B(j, sub):
                    tl = j * 4 + sub
                    if True:
                        x_ = xt[tl % 2]
                        x1_ = x1[tl % 2]
                        h2_ = h2[tl % 2]
                        hT_ = h2Tt[tl % 2]
                        sa = s2[tl % 2]
                        sb_ = s3[tl % 2]
                        st_ = st5[tl % 2]
                        lg_ = lg[tl % 2]
                        t8_ = t8[tl % 2]
                        mk_ = msk[tl % 2]
                        ex_ = ex[tl % 2]
                        gs_ = gs[tl % 2]
                        for kc in range(8):
                            P.op("pe", lambda e: e.transpose(out=pT5[:, kc, :], in_=h2_[:, kc * 128:(kc + 1) * 128], identity=ident[:]),
                                 reads=[h2_, ident], writes=[pT5])
                        P.op("act", lambda e: e.copy(out=hT_[:], in_=pT5[:]), reads=[pT5], writes=[hT_])
                        P.op("sp", lambda e: e.dma_start(out=H2TM[tl * 128:(tl + 1) * 128, :], in_=h2_[:]), reads=[h2_], dma=True)
                        for kc in range(8):
                            P.op("pe", lambda e: e.matmul(pR[:, 0:NE], lhsT=hT_[:, kc, :], rhs=rw[:, kc, :], start=(kc == 0), stop=(kc == 7)),
                                 reads=[hT_, rw], writes=[pR])
                        P.op("dve", lambda e: e.tensor_tensor(out=lg_[:], in0=pR[:, 0:NE], in1=rbb[:], op=ALU.add), reads=[pR, rbb], writes=[lg_])
                        P.op("dve", lambda e: e.max(out=t8_[:], in_=lg_[:]), reads=[lg_], writes=[t8_])
                        P.op("dve", lambda e: e.tensor_scalar(out=mk_[:], in0=lg_[:], scalar1=t8_[:, 3:4], scalar2=None, op0=ALU.is_ge), reads=[lg_, t8_], writes=[mk_])
                        P.op("dve", lambda e: e.tensor_scalar(out=gs_[:, 0:1], in0=t8_[:, 0:1], scalar1=-1.0, scalar2=None, op0=ALU.mult), reads=[t8_], writes=[gs_])
                        P.op("act", lambda e: e.activation(out=ex_[:], in_=lg_[:], func=AF.Exp, bias=gs_[:, 0:1], scale=1.0), reads=[lg_, gs_], writes=[ex_])
                        P.op("dve", lambda e: e.tensor_tensor(out=ex_[:], in0=ex_[:], in1=mk_[:], op=ALU.mult), reads=[ex_, mk_], writes=[ex_])
                        P.op("dve", lambda e: e.reduce_sum(out=gs_[:, 1:2], in_=ex_[:], axis=mybir.AxisListType.X), reads=[ex_], writes=[gs_])
                        P.op("dve", lambda e: e.reciprocal(out=gs_[:, 1:2], in_=gs_[:, 1:2]), reads=[gs_], writes=[gs_])
                        P.op("dve", lambda e: e.tensor_scalar(out=Gall[:, tl, :], in0=ex_[:], scalar1=gs_[:, 1:2], scalar2=None, op0=ALU.mult),
                             reads=[ex_, gs_], writes=[Gall])

                gate_part(0, range(8))
                for j in range(8):
                    if j + 1 < 8:
                        ld5(j + 1)
                    for sub in range(4):
                        sub_A(j, sub)
                        if sub > 0:
                            sub_B(j, sub - 1)
                        if j + 1 < 8:
                            gate_part(j + 1, (2 * sub, 2 * sub + 1))
                    sub_B(j, 3)
                if dbg:
                    P.op("sp", lambda e: e.dma_start(out=GDBG[:, :, :], in_=Gall[:]), reads=[Gall], dma=True)
                P.barrier()

        if upto >= 5:
            P.op("dve", lambda e: e.tensor_copy(out=G2t[:], in_=G2()), reads=[modb], writes=[G2t])
            with ExitStack() as ph:
                maskf = sb(ph, "maskf", [128, 32, NE], F32)
                maskb = sb(ph, "maskb", [128, 32 * NE], BF16)
                Umat = sb(ph, "Umat", [128, 128], BF16)
                onesb = sb(ph, "onesb", [128, 128], BF16)
                cnt = sb(ph, "cnt", [128, 32, NE], F32)
                tot = sb(ph, "tot", [128, 32, NE], F32)
                base = sb(ph, "base", [128, 32, NE], F32)
                key = sb(ph, "key", [128, 32, NE], F32)
                ntot = sb(ph, "ntot", [128, NE], F32)
                nbi = sb(ph, "nbi", [128, NE], I32)
                nbf = sb(ph, "nbf", [128, NE], F32)
                sbe = sb(ph, "sbe", [128, NE], F32)
                starts = sb(ph, "starts", [128, NE], F32)
                t8r = [sb(ph, "t8r%d" % i, [128, 8], F32) for i in range(2)]
                eqr = [sb(ph, "eqr%d" % i, [128, NE], F32) for i in range(4)]
                dest4f = sb(ph, "dest4f", [128, 32 * 4], F32)
                jidx_i = sb(ph, "jidx_i", [128, NBLK], I32)
                jidx = sb(ph, "jidx", [128, NBLK], F32)
                pidx_i = sb(ph, "pidx_i", [128, 1], I32)
                pidx = sb(ph, "pidx", [128, 1], F32)
                cmp = sb(ph, "cmp", [128, NBLK, NE], F32)
                Ej = sb(ph, "Ej", [128, NBLK], F32)
                bw = sb(ph, "bw", [128, NBLK], F32)
                offwf = sb(ph, "offwf", [128, NBLK, 8], F32)
                offbf = sb(ph, "offbf", [128, NBLK], F32)
                pc = [ps(ph, "pc%d" % i, [128, 512], F32) for i in range(2)]
                ptt = [ps(ph, "ptt%d" % i, [128, 512], F32) for i in range(2)]
                P.op("dve", lambda e: e.tensor_scalar(out=maskf[:], in0=Gall[:], scalar1=0.0, scalar2=None, op0=ALU.is_gt), reads=[Gall], writes=[maskf])
                P.op("dve", lambda e: e.tensor_copy(out=maskb[:], in_=maskf[:].rearrange("p t e -> p (t e)")), reads=[maskf], writes=[maskb])
                P.op("pool", lambda e: e.memset(Umat[:], 1.0), writes=[Umat])
                P.op("pool", lambda e: e.affine_select(out=Umat[:], in_=Umat[:], pattern=[[1, 128]], compare_op=ALU.is_gt, fill=0.0, base=0,
                                                       channel_multiplier=-1), reads=[Umat], writes=[Umat])
                P.op("pool", lambda e: e.memset(onesb[:], 1.0), writes=[onesb])
                for half in range(2):
                    P.op("pe", lambda e: e.matmul(pc[half][:], lhsT=Umat[:], rhs=maskb[:, half * 512:(half + 1) * 512], start=True, stop=True),
                         reads=[Umat, maskb], writes=[pc[half]])
                    P.op("pe", lambda e: e.matmul(ptt[half][:], lhsT=onesb[:], rhs=maskb[:, half * 512:(half + 1) * 512], start=True, stop=True),
                         reads=[onesb, maskb], writes=[ptt[half]])
                    P.op("dve", lambda e: e.tensor_copy(out=cnt[:, half * 16:(half + 1) * 16, :], in_=pc[half][:].rearrange("p (t e) -> p t e", e=NE)),
                         reads=[pc[half]], writes=[cnt])
                    P.op("dve", lambda e: e.tensor_copy(out=tot[:, half * 16:(half + 1) * 16, :], in_=ptt[half][:].rearrange("p (t e) -> p t e", e=NE)),
                         reads=[ptt[half]], writes=[tot])
                P.op("dve", lambda e: e.memset(base[:, 0, :], 0.0), writes=[base])
                for t in range(1, 32):
                    P.op("dve", lambda e: e.tensor_tensor(out=base[:, t, :], in0=base[:, t - 1, :], in1=tot[:, t - 1, :], op=ALU.add), reads=[base, tot], writes=[base])
                P.op("dve", lambda e: e.tensor_tensor(out=ntot[:], in0=base[:, 31, :], in1=tot[:, 31, :], op=ALU.add), reads=[base, tot], writes=[ntot])
                P.op("dve", lambda e: e.tensor_scalar(out=ntot[:], in0=ntot[:], scalar1=float(BLK - 1), scalar2=None, op0=ALU.add), reads=[ntot], writes=[ntot])
                P.op("dve", lambda e: e.tensor_copy(out=nbi[:], in_=ntot[:]), reads=[ntot], writes=[nbi])
                P.op("dve", lambda e: e.tensor_scalar(out=nbi[:], in0=nbi[:], scalar1=9, scalar2=None, op0=ALU.arith_shift_right), reads=[nbi], writes=[nbi])
                P.op("dve", lambda e: e.tensor_copy(out=nbf[:], in_=nbi[:]), reads=[nbi], writes=[nbf])
                P.op("dve", lambda e: e.memset(sbe[:, 0:1], 0.0), writes=[sbe])
                for ex_i in range(1, NE):
                    P.op("dve", lambda e: e.tensor_tensor(out=sbe[:, ex_i:ex_i + 1], in0=sbe[:, ex_i - 1:ex_i], in1=nbf[:, ex_i - 1:ex_i], op=ALU.add),
                         reads=[sbe, nbf], writes=[sbe])
                P.op("dve", lambda e: e.tensor_scalar(out=starts[:], in0=sbe[:], scalar1=float(BLK), scalar2=None, op0=ALU.mult), reads=[sbe], writes=[starts])
                P.op("dve", lambda e: e.tensor_tensor(out=key[:], in0=cnt[:], in1=base[:], op=ALU.add), reads=[cnt, base], writes=[key])
                P.op("dve", lambda e: e.tensor_tensor(out=key[:], in0=key[:], in1=starts[:].unsqueeze(1).to_broadcast([128, 32, NE]), op=ALU.add),
                     reads=[key, starts], writes=[key])
                P.op("dve", lambda e: e.scalar_tensor_tensor(out=key[:], in0=key[:], scalar=1.0, in1=maskf[:], op0=ALU.add, op1=ALU.mult),
                     reads=[key, maskf], writes=[key])
                t8all = sb(ph, "t8all", [128, 32, 8], F32)
                eqb = sb(ph, "eqb", [128, 32, NE], F32)
                for t in range(32):
                    P.op("dve", lambda e: e.max(out=t8all[:, t, :], in_=key[:, t, :]), reads=[key], writes=[t8all])
                P.op("dve", lambda e: e.tensor_scalar(out=dest4f[:].rearrange("p (t k) -> p t k", k=4), in0=t8all[:, :, 0:4], scalar1=-1.0, scalar2=None, op0=ALU.add),
                     reads=[t8all], writes=[dest4f])
                for k in range(4):
                    P.op("dve", lambda e: e.tensor_tensor(out=eqb[:], in0=key[:], in1=t8all[:, :, k:k + 1].to_broadcast([128, 32, NE]), op=ALU.is_equal),
                         reads=[key, t8all], writes=[eqb])
                    P.op("dve", lambda e: e.tensor_tensor(out=eqb[:], in0=eqb[:], in1=Gall[:], op=ALU.mult), reads=[eqb, Gall], writes=[eqb])
                    P.op("dve", lambda e: e.reduce_sum(out=G4[:, :, k], in_=eqb[:], axis=mybir.AxisListType.X), reads=[eqb], writes=[G4])
                P.op("dve", lambda e: e.tensor_copy(out=dest4u[:], in_=dest4f[:]), reads=[dest4f], writes=[dest4u])
                P.op("pool", lambda e: e.iota(jidx_i[:], pattern=[[1, NBLK]], base=0, channel_multiplier=0), writes=[jidx_i])
                P.op("dve", lambda e: e.tensor_copy(out=jidx[:], in_=jidx_i[:]), reads=[jidx_i], writes=[jidx])
                P.op("pool", lambda e: e.iota(pidx_i[:], pattern=[[0, 1]], base=0, channel_multiplier=1), writes=[pidx_i])
                P.op("dve", lambda e: e.tensor_copy(out=pidx[:], in_=pidx_i[:]), reads=[pidx_i], writes=[pidx])
                P.op("dve", lambda e: e.tensor_tensor(out=cmp[:], in0=sbe[:].unsqueeze(1).to_broadcast([128, NBLK, NE]),
                                                      in1=jidx[:].unsqueeze(2).to_broadcast([128, NBLK, NE]), op=ALU.is_le), reads=[sbe, jidx], writes=[cmp])
                P.op("dve", lambda e: e.reduce_sum(out=Ej[:], in_=cmp[:], axis=mybir.AxisListType.X), reads=[cmp], writes=[Ej])
                P.op("dve", lambda e: e.tensor_scalar(out=Ej[:], in0=Ej[:], scalar1=-1.0, scalar2=None, op0=ALU.add), reads=[Ej], writes=[Ej])
                P.op("dve", lambda e: e.tensor_scalar(out=bw[:], in0=Ej[:], scalar1=float(D), scalar2=pidx[:, 0:1], op0=ALU.mult, op1=ALU.add),
                     reads=[Ej, pidx], writes=[bw])
                for kc in range(8):
                    P.op("dve", lambda e: e.tensor_scalar(out=offwf[:, :, kc], in0=bw[:], scalar1=float(kc * 128), scalar2=None, op0=ALU.add),
                         reads=[bw], writes=[offwf])
                P.op("dve", lambda e: e.tensor_copy(out=OFFW[:], in_=offwf[:]), reads=[offwf], writes=[OFFW])
                P.op("dve", lambda e: e.tensor_scalar(out=offbf[:], in0=Ej[:], scalar1=128.0, scalar2=pidx[:, 0:1], op0=ALU.mult, op1=ALU.add),
                     reads=[Ej, pidx], writes=[offbf])
                P.op("dve", lambda e: e.tensor_copy(out=OFFB[:], in_=offbf[:]), reads=[offbf], writes=[OFFB])
                P.op("dve", lambda e: e.tensor_copy(out=OFFD[:], in_=Ej[:]), reads=[Ej], writes=[OFFD])
                if dbg:
                    P.op("sp", lambda e: e.dma_start(out=RDBG[:, 0:128], in_=dest4f[:]), reads=[dest4f], dma=True)
                    P.op("sp", lambda e: e.dma_start(out=RDBG[:, 128:256], in_=G4[:].rearrange("p t k -> p (t k)")), reads=[G4], dma=True)
                    P.op("sp", lambda e: e.dma_start(out=RDBG[:, 256:256 + NBLK], in_=Ej[:]), reads=[Ej], dma=True)
                h2t = [sb(ph, "h2t%d" % i, [128, D], BF16) for i in range(3)]
                for t in range(32):
                    h_ = h2t[t % 3]
                    P.op("sp", lambda e: e.dma_start(out=h_[:], in_=H2TM[t * 128:(t + 1) * 128, :]), writes=[h_], dma=True)
                    for k in range(4):
                        P.op("pool", lambda e: e.indirect_dma_start(out=XS[:, :], out_offset=bass.IndirectOffsetOnAxis(ap=dest4u[:, t * 4 + k:t * 4 + k + 1], axis=0),
                                                                    in_=h_[:], in_offset=None), reads=[dest4u, h_], dma=True)
                P.barrier()

        mes.close()
        if upto >= 6:
            with ExitStack() as ph:
                wgb_ = [sb(ph, "wgub%d" % i, [128, 8, 2 * D], BF16) for i in range(2)]
                wdb_ = [sb(ph, "wdnb%d" % i, [128, 8, D], BF16) for i in range(2)]
                bgb_ = [sb(ph, "bgub%d" % i, [128, 16], F32) for i in range(2)]
                bdb_ = [sb(ph, "bdb%d" % i, [128, D], F32) for i in range(2)]
                xtok = [sb(ph, "xtok%d" % i, [128, 4, D], BF16) for i in range(1)]
                xT = [sb(ph, "xT%d" % i, [128, 8, 512], BF16) for i in range(2)]
                actT = [sb(ph, "actT%d" % i, [128, 8, 512], BF16) for i in range(2)]
                gbt = [sb(ph, "gbt%d" % i, [128, 512], F32) for i in range(2)]
                sig = [sb(ph, "sig%d" % i, [128, 512], F32) for i in range(2)]
                ubt = [sb(ph, "ubt%d" % i, [128, 512], F32) for i in range(2)]
                ys = [sb(ph, "ys%d" % i, [128, 512], F32) for i in range(12)]
                pTx = [ps(ph, "pTx%d" % i, [128, 8, 128], BF16) for i in range(2)]
                pg = [ps(ph, "pg%d" % i, [128, 512], F32) for i in range(2)]
                pu = [ps(ph, "pu%d" % i, [128, 512], F32) for i in range(2)]
                py = [ps(ph, "py%d" % i, [128, 512], F32) for i in range(2)]

                def ldblk(j):
                    b = j % 2
                    P.op("pool", lambda e: e.indirect_dma_start(out=wgb_[b][:].rearrange("p k n -> p (k n)"), out_offset=None,
                                                                in_=WGUB.rearrange("(r k) n -> r (k n)", k=8),
                                                                in_offset=bass.IndirectOffsetOnAxis(ap=OFFB[:, j:j + 1], axis=0)),
                         reads=[OFFB], writes=[wgb_[b]], dma=True)
                    P.op("pool", lambda e: e.indirect_dma_start(out=wdb_[b][:].rearrange("p k n -> p (k n)"), out_offset=None,
                                                                in_=WDNB.rearrange("(r k) n -> r (k n)", k=8),
                                                                in_offset=bass.IndirectOffsetOnAxis(ap=OFFB[:, j:j + 1], axis=0)),
                         reads=[OFFB], writes=[wdb_[b]], dma=True)
                    P.op("pool", lambda e: e.indirect_dma_start(out=bgb_[b][:], out_offset=None, in_=bgu_d[:, :],
                                                                in_offset=bass.IndirectOffsetOnAxis(ap=OFFB[:, j:j + 1], axis=0)),
                         reads=[OFFB], writes=[bgb_[b]], dma=True)
                    P.op("pool", lambda e: e.indirect_dma_start(out=bdb_[b][:], out_offset=None, in_=b_dn[:, :],
                                                                in_offset=bass.IndirectOffsetOnAxis(ap=OFFD[:, j:j + 1], axis=0)),
                         reads=[OFFD], writes=[bdb_[b]], dma=True)

                def ldx(j):
                    P.op("sp", lambda e: e.dma_start(out=xtok[0][:], in_=XS[j * BLK:(j + 1) * BLK, :].rearrange("(s p) d -> p s d", p=128)),
                         writes=[xtok[0]], dma=True)

                ldblk(0)
                ldx(0)
                fcn = 0
                yc = 0
                tcx = [0]

                def xpose(jj):
                    xk_ = xtok[0]
                    for sub in range(4):
                        p_ = pTx[tcx[0] % 2]
                        tcx[0] += 1
                        for kc in range(8):
                            P.op("pe", lambda e: e.transpose(out=p_[:, kc, :], in_=xk_[:, sub, kc * 128:(kc + 1) * 128], identity=ident[:]),
                                 reads=[xk_, ident], writes=[p_])
                        P.op("act", lambda e: e.copy(out=xT[jj % 2][:, :, sub * 128:(sub + 1) * 128], in_=p_[:]), reads=[p_], writes=[xT[jj % 2]])
                    if jj + 1 < NBLK:
                        ldx(jj + 1)
                for j in range(NBLK):
                    if j + 1 < NBLK:
                        ldblk(j + 1)
                    b = j % 2
                    wg_ = wgb_[b]
                    wd_ = wdb_[b]
                    bg_ = bgb_[b]
                    bd_ = bdb_[b]
                    xT_ = xT[b]
                    a_ = actT[b]
                    if j == 0:
                        xpose(0)
                    for fc in range(8):
                        g_p = pg[fcn % 2]
                        u_p = pu[fcn % 2]
                        gb_ = gbt[fcn % 2]
                        sg_ = sig[fcn % 2]
                        ub_ = ubt[fcn % 2]
                        fcn += 1
                        for kc in range(8):
                            P.op("pe", lambda e: e.matmul(g_p[:], lhsT=wg_[:, kc, fc * 128:(fc + 1) * 128], rhs=xT_[:, kc, :], start=(kc == 0), stop=(kc == 7)),
                                 reads=[wg_, xT_], writes=[g_p])
                        for kc in range(8):
                            P.op("pe", lambda e: e.matmul(u_p[:], lhsT=wg_[:, kc, D + fc * 128:D + (fc + 1) * 128], rhs=xT_[:, kc, :], start=(kc == 0), stop=(kc == 7)),
                                 reads=[wg_, xT_], writes=[u_p])
                        P.op("act", lambda e: e.activation(out=ub_[:], in_=u_p[:], func=AF.Identity, bias=bg_[:, 8 + fc:9 + fc], scale=1.0),
                             reads=[u_p, bg_], writes=[ub_])
                        P.op("dve", lambda e: e.tensor_scalar(out=gb_[:], in0=g_p[:], scalar1=bg_[:, fc:fc + 1], scalar2=7.0, op0=ALU.add, op1=ALU.min),
                             reads=[g_p, bg_], writes=[gb_])
                        P.op("act", lambda e: e.activation(out=sg_[:], in_=gb_[:], func=AF.Sigmoid, scale=1.702), reads=[gb_], writes=[sg_])
                        P.op("dve", lambda e: e.tensor_scalar(out=ub_[:], in0=ub_[:], scalar1=7.0, scalar2=-7.0, op0=ALU.min, op1=ALU.max), reads=[ub_], writes=[ub_])
                        P.op("dve", lambda e: e.tensor_tensor(out=gb_[:], in0=gb_[:], in1=sg_[:], op=ALU.mult), reads=[gb_, sg_], writes=[gb_])
                        P.op("dve", lambda e: e.scalar_tensor_tensor(out=a_[:, fc, :], in0=ub_[:], scalar=1.0, in1=gb_[:], op0=ALU.add, op1=ALU.mult),
                             reads=[ub_, gb_], writes=[a_])
                    if j + 1 < NBLK:
                        xpose(j + 1)
                    for sub in range(4):
                        for half in range(2):
                            y_p = py[yc % 2]
                            y_ = ys[yc % 12]
                            yc += 1
                            for fc in range(8):
                                P.op("pe", lambda e: e.matmul(y_p[:], lhsT=a_[:, fc, sub * 128:(sub + 1) * 128], rhs=wd_[:, fc, half * 512:(half + 1) * 512],
                                                              start=(fc == 0), stop=(fc == 7)), reads=[a_, wd_], writes=[y_p])
                            P.op("dve", lambda e: e.tensor_tensor(out=y_[:], in0=y_p[:], in1=bd_[:, half * 512:(half + 1) * 512], op=ALU.add),
                                 reads=[y_p, bd_], writes=[y_])
                            P.op("sp", lambda e: e.dma_start(out=YS[j * BLK + sub * 128:j * BLK + (sub + 1) * 128, half * 512:(half + 1) * 512], in_=y_[:]),
                                 reads=[y_], dma=True)
                P.barrier()

        if upto >= 6:
            with ExitStack() as ph:
                yk = [[sb(ph, "yk%d_%d" % (i, k), [128, D], F32) for k in range(4)] for i in range(2)]
                xt = [sb(ph, "xt7_%d" % i, [128, D], F32) for i in range(2)]
                acc = [sb(ph, "acc7_%d" % i, [128, D], F32) for i in range(2)]
                ot = [sb(ph, "ot7_%d" % i, [128, D], F32) for i in range(2)]
                s7 = [sb(ph, "s7_%d" % i, [128, 1], F32) for i in range(2)]
                st7 = [sb(ph, "st7_%d" % i, [128, 1], F32) for i in range(2)]

                def ld7(t):
                    for k in range(4):
                        P.op("pool", lambda e: e.indirect_dma_start(out=yk[t % 2][k][:], out_offset=None, in_=YS[:, :],
                                                                    in_offset=bass.IndirectOffsetOnAxis(ap=dest4u[:, t * 4 + k:t * 4 + k + 1], axis=0)),
                             reads=[dest4u], writes=[yk[t % 2][k]], dma=True)
                    P.op("sp", lambda e: e.dma_start(out=xt[t % 2][:], in_=X1[t * 128:(t + 1) * 128, :]), writes=[xt[t % 2]], dma=True)

                ld7(0)
                for t in range(32):
                    if t + 1 < 32:
                        ld7(t + 1)
                    y4 = yk[t % 2]
                    a_ = acc[t % 2]
                    o_ = ot[t % 2]
                    x_ = xt[t % 2]
                    s_ = s7[t % 2]
                    st_ = st7[t % 2]
                    P.op("dve", lambda e: e.tensor_scalar(out=a_[:], in0=y4[0][:], scalar1=G4[:, t, 0:1], scalar2=None, op0=ALU.mult), reads=[y4[0], G4], writes=[a_])
                    for k in range(1, 4):
                        P.op("dve", lambda e: e.scalar_tensor_tensor(out=a_[:], in0=y4[k][:], scalar=G4[:, t, k:k + 1], in1=a_[:], op0=ALU.mult, op1=ALU.add),
                             reads=[y4[k], G4, a_], writes=[a_])
                    P.op("act", lambda e: e.activation(out=o_[:], in_=a_[:], func=AF.Square, accum_out=s_[:, 0:1]), reads=[a_], writes=[o_, s_])
                    rstd_from_ss(s_, D, st_)
                    P.op("dve", lambda e: e.scalar_tensor_tensor(out=o_[:], in0=a_[:], scalar=s_[:, 0:1], in1=G2t[:], op0=ALU.mult, op1=ALU.mult),
                         reads=[a_, s_, G2t], writes=[o_])
                    P.op("dve", lambda e: e.tensor_tensor(out=o_[:], in0=o_[:], in1=x_[:], op=ALU.add), reads=[o_, x_], writes=[o_])
                    P.op("sp", lambda e: e.dma_start(out=y_out[t * 128:(t + 1) * 128, :], in_=o_[:]), reads=[o_], dma=True)
                P.barrier()
        P.barrier()
        print("ops", P.nops, "waits", P.nwaits, flush=True)
    return nc


def _host_inputs(inputs):
    x = np.asarray(inputs["x"], np.float32)
    c = np.asarray(inputs["c"], np.float32)
    inv = (10000.0 ** (-np.arange(0, 64, 2, dtype=np.float32) / np.float32(64))).astype(np.float32)
    shared = {
        "w_mod": np.ascontiguousarray(inputs["w_mod"][0], np.float32),
        "b_mod": np.ascontiguousarray(inputs["b_mod"][0:1], np.float32),
        "norm_pre_mix": np.ascontiguousarray(inputs["norm_pre_mix"][0:1], np.float32),
        "norm_post_mix": np.ascontiguousarray(inputs["norm_post_mix"][0:1], np.float32),
        "w_in": np.ascontiguousarray(inputs["w_in"][0], np.float32),
        "da_lambda_q1": np.ascontiguousarray(inputs["da_lambda_q1"][0:1], np.float32),
        "da_lambda_k1": np.ascontiguousarray(inputs["da_lambda_k1"][0:1], np.float32),
        "da_lambda_q2": np.ascontiguousarray(inputs["da_lambda_q2"][0:1], np.float32),
        "da_lambda_k2": np.ascontiguousarray(inputs["da_lambda_k2"][0:1], np.float32),
        "da_subln": np.ascontiguousarray(inputs["da_subln"][0:1], np.float32),
        "subln_col": np.ascontiguousarray(np.asarray(inputs["da_subln"], np.float32)[0].reshape(128, 1)),
        "lbl": np.ascontiguousarray(np.asarray(inputs["hg_lb_logits"], np.float32).reshape(2, 4, 128).transpose(2, 0, 1)),
        "hg_norm": np.ascontiguousarray(inputs["hg_norm"][0:1], np.float32),
        "w_branch_a": np.ascontiguousarray(inputs["w_branch_a"][0], np.float32),
        "w_branch_b": np.ascontiguousarray(inputs["w_branch_b"][0], np.float32),
        "w_out": np.ascontiguousarray(inputs["w_out"][0], np.float32),
        "norm_pre_ffn": np.ascontiguousarray(inputs["norm_pre_ffn"][0:1], np.float32),
        "norm_post_ffn": np.ascontiguousarray(inputs["norm_post_ffn"][0:1], np.float32),
        "router_w": np.ascontiguousarray(inputs["router_w"][0], np.float32),
        "router_b": np.ascontiguousarray(inputs["router_b"][0:1], np.float32),
        "w_gate_up": np.ascontiguousarray(np.asarray(inputs["w_gate_up"][0], np.float32).reshape(NE * D, 2 * D)),
        "bgu": np.ascontiguousarray(np.asarray(inputs["b_gate_up"][0], np.float32).reshape(NE, 16, 128).transpose(0, 2, 1).reshape(NE * 128, 16)),
        "w_down": np.ascontiguousarray(np.asarray(inputs["w_down"][0], np.float32).reshape(NE * D, D)),
        "b_down": np.ascontiguousarray(inputs["b_down"][0], np.float32),
    }
    in_maps = []
    p = np.arange(128)
    sign = np.where((p % 64) < 32, -1.0, 1.0).astype(np.float32)[:, None]
    for core in range(8):
        b, hf = core // 2, core % 2
        if hf == 1:
            xall = x[b]
            pos = np.arange(SEQ, dtype=np.float32)
        else:
            xall = np.concatenate([x[b, :TOWN], x[b, :TOWN]], axis=0)
            pos = np.concatenate([np.arange(TOWN), np.arange(TOWN)]).astype(np.float32)
        ang = (pos[None, :] * inv[p % 32][:, None]).astype(np.float32)
        m = dict(shared)
        m["xall"] = np.ascontiguousarray(xall)
        m["cosT"] = np.ascontiguousarray(np.cos(ang).astype(np.float32))
        m["sinT"] = np.ascontiguousarray((np.sin(ang) * sign).astype(np.float32))
        m["flag"] = np.full((128, 1), float(hf), np.float32)
        m["c2"] = np.ascontiguousarray(c[b].reshape(8, 128).T)
        in_maps.append(m)
    return in_maps


_NC_CACHE = {}


def kernel(**inputs):
    in_maps = _host_inputs(inputs)
    if "nc" not in _NC_CACHE:
        _NC_CACHE["nc"] = build_nc()
    nc = _NC_CACHE["nc"]
    res = run_bass_kernel_spmd(nc, in_maps, core_ids=list(range(8)))
    out = np.empty((NB, SEQ, D), np.float32)
    for core in range(8):
        b, hf = core // 2, core % 2
        out[b, hf * TOWN:(hf + 1) * TOWN] = res.results[core]["y"]
    return out
```

```python
import numpy as np
from contextlib import ExitStack
import concourse.bass as bass
import concourse.mybir as mybir
from concourse.bass_utils import run_bass_kernel_spmd

F32 = mybir.dt.float32
BF16 = mybir.dt.bfloat16
U32 = mybir.dt.uint32
I32 = mybir.dt.int32
ALU = mybir.AluOpType
AF = mybir.ActivationFunctionType

D = 1024
SEQ = 8192
NB = 4
TOWN = 4096
NT = 64
NE = 32
BLK = 512
NBLK = 63
NSLOT = NBLK * BLK
EPS = 1e-6
IN_COLS = 5632


class Buf:
    __slots__ = ("writers", "readers")

    def __init__(self):
        self.writers = {}
        self.readers = {}


class T:
    def __init__(self, t):
        self.t = t
        self.b = Buf()

    def __getitem__(self, k):
        return self.t[k]


class Prog:
    def __init__(self, nc, es, n_dma_sems=32):
        self.nc = nc
        self.eng = {"pe": nc.tensor, "act": nc.scalar, "dve": nc.vector, "pool": nc.gpsimd, "sp": nc.sync}
        self.sem = {}
        self.cnt = {}
        for k in self.eng:
            self.sem[k] = es.enter_context(nc.semaphore("sem_" + k))
            self.cnt[k] = 0
        self.rings = {}
        for rn, n in (("main", n_dma_sems), ("pre", 8), ("sw", 24)):
            self.rings[rn] = {"sem": [es.enter_context(nc.semaphore("dsem_%s%d" % (rn, i))) for i in range(n)], "cnt": [0] * n, "next": 0}
        self.seen = {k: {} for k in self.eng}
        self.nwaits = 0
        self.nops = 0

    def _wait(self, eng, tok):
        sem, val = tok
        key = id(sem)
        if self.seen[eng].get(key, 0) >= val:
            return
        self.eng[eng].wait_ge(sem, val)
        self.seen[eng][key] = val
        self.nwaits += 1

    def op(self, eng, fn, reads=(), writes=(), dma=False, ring="main"):
        pe_sem = self.sem["pe"]
        for t in reads:
            for tok in t.b.writers.values():
                if eng == "pe" and tok[0] is pe_sem:
                    continue
                self._wait(eng, tok)
        for t in writes:
            for tok in t.b.writers.values():
                if eng == "pe" and tok[0] is pe_sem:
                    continue
                self._wait(eng, tok)
            for tok in t.b.readers.values():
                if eng == "pe" and tok[0] is pe_sem:
                    continue
                self._wait(eng, tok)
        if dma:
            if eng == "pool" and ring == "main":
                ring = "sw"
            rg = self.rings[ring]
            i = rg["next"]
            rg["next"] = (i + 1) % len(rg["sem"])
            sem = rg["sem"][i]
            if rg["cnt"][i] > 0:
                self._wait(eng, (sem, rg["cnt"][i]))
            ins = fn(self.eng[eng])
            rg["cnt"][i] += 16
            ins.then_inc(sem, 16)
            tok = (sem, rg["cnt"][i])
        else:
            ins = fn(self.eng[eng])
            self.cnt[eng] += 1
            ins.then_inc(self.sem[eng], 1)
            tok = (self.sem[eng], self.cnt[eng])
        self.nops += 1
        k = id(tok[0])
        for t in reads:
            t.b.readers[k] = tok
        for t in writes:
            t.b.writers[k] = tok
            t.b.readers = {}
        return tok

    def barrier(self, engines=None):
        engines = engines or list(self.eng)
        for e in engines:
            for o in self.eng:
                if o != e and self.cnt[o] > 0:
                    self._wait(e, (self.sem[o], self.cnt[o]))
            for rg in self.rings.values():
                for i, s in enumerate(rg["sem"]):
                    if rg["cnt"][i] > 0:
                        self._wait(e, (s, rg["cnt"][i]))


def build_nc(upto=99, dbg=False):
    nc = bass.Bass("TRN2", target_bir_lowering=False)

    def din(name, shape, dt=F32):
        return nc.dram_tensor(name, list(shape), dt, kind="ExternalInput").ap()

    skind = "ExternalOutput" if dbg else "Internal"

    def dscr(name, shape, dt):
        return nc.dram_tensor(name, list(shape), dt, kind=skind).ap()

    xall = din("xall", [SEQ, D])
    cosT = din("cosT", [128, SEQ])
    sinT = din("sinT", [128, SEQ])
    flag_d = din("flag", [128, 1])
    c2_d = din("c2", [128, 8])
    w_mod = din("w_mod", [D, 6 * D])
    b_mod = din("b_mod", [1, 6 * D])
    n_pre_mix = din("norm_pre_mix", [1, D])
    n_post_mix = din("norm_post_mix", [1, D])
    w_in = din("w_in", [D, IN_COLS])
    lq1 = din("da_lambda_q1", [1, 64])
    lk1 = din("da_lambda_k1", [1, 64])
    lq2 = din("da_lambda_q2", [1, 64])
    lk2 = din("da_lambda_k2", [1, 64])
    da_subln = din("da_subln", [1, 128])
    subln_col = din("subln_col", [128, 1])
    lbl_d = din("lbl", [128, 2, 4])
    hg_norm = din("hg_norm", [1, 128])
    w_ba = din("w_branch_a", [512, D])
    w_bb = din("w_branch_b", [512, D])
    w_out = din("w_out", [D, D])
    n_pre_ffn = din("norm_pre_ffn", [1, D])
    n_post_ffn = din("norm_post_ffn", [1, D])
    router_w = din("router_w", [D, NE])
    router_b = din("router_b", [1, NE])
    if upto >= 6:
        w_gu = din("w_gate_up", [NE * D, 2 * D])
        bgu_d = din("bgu", [NE * 128, 16])
        w_dn = din("w_down", [NE * D, D])
        b_dn = din("b_down", [NE, D])
    y_out = nc.dram_tensor("y", [TOWN, D], F32, kind="ExternalOutput").ap()

    HT = dscr("HT", [NT, 128, 8, 128], BF16)
    KT = dscr("KT", [4, 128, SEQ], BF16)
    QT = dscr("QT", [4, 128, TOWN], BF16)
    VS = dscr("VS", [4, 128, NT, 130], BF16)
    YAT = dscr("YAT", [4, 128, TOWN], BF16)
    YBT = dscr("YBT", [4, 128, TOWN], BF16)
    X1 = dscr("X1", [TOWN, D], F32)
    H2TM = dscr("H2TM", [TOWN, D], BF16)
    XS = dscr("XS", [NSLOT, D], BF16)
    YS = dscr("YS", [NSLOT, D], F32)
    WGUB = nc.dram_tensor("WGUB", [NE * D, 2 * D], BF16).ap()
    WDNB = nc.dram_tensor("WDNB", [NE * D, D], BF16).ap()
    if dbg:
        GDBG = dscr("GDBG", [128, 32, NE], F32)
        RDBG = dscr("RDBG", [128, 32 * 4 + 32 * 4 + 64], F32)

    w_in_v = w_in.rearrange("(kc p) n -> p kc n", p=128)

    es = ExitStack()
    with es:
        P = Prog(nc, es)

        def sb(stack, name, shape, dt):
            return T(stack.enter_context(nc.sbuf_tensor(name, list(shape), dt)))

        def ps(stack, name, shape, dt):
            return T(stack.enter_context(nc.psum_tensor(name, list(shape), dt)))

        def rstd_from_ss(ss, n, tmp):
            P.op("dve", lambda e: e.tensor_scalar(out=tmp[:, 0:1], in0=ss[:, 0:1], scalar1=1.0 / n, scalar2=EPS,
                                                  op0=ALU.mult, op1=ALU.add), reads=[ss], writes=[tmp])
            P.op("act", lambda e: e.activation(out=tmp[:, 0:1], in_=tmp[:, 0:1], func=AF.Ln), reads=[tmp], writes=[tmp])
            P.op("act", lambda e: e.activation(out=ss[:, 0:1], in_=tmp[:, 0:1], func=AF.Exp, scale=-0.5), reads=[tmp], writes=[ss])

        ident = sb(es, "ident", [128, 128], BF16)
        P.op("pool", lambda e: e.memset(ident[:], 1.0), writes=[ident])
        P.op("pool", lambda e: e.affine_select(out=ident[:], in_=ident[:], pattern=[[-1, 128]], compare_op=ALU.is_equal,
                                               fill=0.0, base=0, channel_multiplier=1), reads=[ident], writes=[ident])
        flag = sb(es, "flag_t", [128, 1], F32)
        P.op("sp", lambda e: e.dma_start(out=flag[:], in_=flag_d[:, :]), writes=[flag], dma=True)
        nlam = sb(es, "nlam", [128, 1], F32)
        subln_b = sb(es, "subln_b", [128, 128], F32)
        hgn_b = sb(es, "hgn_b", [128, 128], F32)
        lb = sb(es, "lb", [128, 4], F32)
        oml = sb(es, "oml", [128, 4], F32)
        Gall = sb(es, "Gall", [128, 32, NE], F32)
        dest4u = sb(es, "dest4u", [128, 32 * 4], U32)
        G4 = sb(es, "G4", [128, 32, 4], F32)
        OFFW = sb(es, "OFFW", [128, NBLK, 8], U32)
        OFFB = sb(es, "OFFB", [128, NBLK], U32)
        OFFD = sb(es, "OFFD", [128, NBLK], U32)
        G2t = sb(es, "G2t", [128, D], F32)
        mes = ExitStack()
        modb = sb(mes, "modb", [128, 6 * D], F32)
        B1 = lambda: modb[:, 0:D]
        A1 = lambda: modb[:, D:2 * D]
        G1 = lambda: modb[:, 2 * D:3 * D]
        B2 = lambda: modb[:, 3 * D:4 * D]
        A2 = lambda: modb[:, 4 * D:5 * D]
        G2 = lambda: modb[:, 5 * D:6 * D]

        st0 = ExitStack()
        if True:
            ph = st0
            c2 = sb(ph, "c2t", [128, 8], F32)
            cb = sb(ph, "cb", [128, 8, 128], F32)
            ones1 = sb(ph, "ones1", [1, 128], F32)
            wm = [sb(ph, "wm%d" % i, [128, 8, 512], F32) for i in range(2)]
            bm = [sb(ph, "bm%d" % i, [1, 512], F32) for i in range(2)]
            pmod = [ps(ph, "pmod%d" % i, [128, 512], F32) for i in range(2)]
            nb4 = [sb(ph, "nb%d" % i, [128, D], F32) for i in range(4)]
            l4 = sb(ph, "l4", [128, 4, 64], F32)
            lt = sb(ph, "lt", [128, 2, 64], F32)
            ls = sb(ph, "ls", [128, 2], F32)
            lbl = sb(ph, "lblt", [128, 2, 4], F32)
            P.op("sp", lambda e: e.dma_start(out=c2[:], in_=c2_d[:, :]), writes=[c2], dma=True)
            P.op("act", lambda e: e.activation(out=c2[:], in_=c2[:], func=AF.Silu), reads=[c2], writes=[c2])
            P.op("dve", lambda e: e.tensor_copy(out=cb[:], in_=c2[:].unsqueeze(2).to_broadcast([128, 8, 128])), reads=[c2], writes=[cb])
            P.op("pool", lambda e: e.memset(ones1[:], 1.0), writes=[ones1])
            w_mod_v = w_mod.rearrange("(kc p) n -> p kc n", p=128)
            for ci in range(12):
                w_ = wm[ci % 2]
                b_ = bm[ci % 2]
                pm_ = pmod[ci % 2]
                P.op("sp", lambda e: e.dma_start(out=w_[:], in_=w_mod_v[:, :, ci * 512:(ci + 1) * 512]), writes=[w_], dma=True)
                P.op("sp", lambda e: e.dma_start(out=b_[:], in_=b_mod[0:1, ci * 512:(ci + 1) * 512]), writes=[b_], dma=True)
                for kc in range(8):
                    P.op("pe", lambda e: e.matmul(pm_[:], lhsT=cb[:, kc, :], rhs=w_[:, kc, :], start=(kc == 0), stop=False),
                         reads=[cb, w_], writes=[pm_])
                P.op("pe", lambda e: e.matmul(pm_[:], lhsT=ones1[0:1, :], rhs=b_[0:1, :], start=False, stop=True),
                     reads=[ones1, b_], writes=[pm_])
                P.op("dve", lambda e: e.tensor_copy(out=modb[:, ci * 512:(ci + 1) * 512], in_=pm_[:]), reads=[pm_], writes=[modb])
            for i, src in enumerate([n_pre_mix, n_post_mix, n_pre_ffn, n_post_ffn]):
                P.op("sp", lambda e: e.dma_start(out=nb4[i][:], in_=src[0:1, :].partition_broadcast(128)), writes=[nb4[i]], dma=True)
            P.op("dve", lambda e: e.scalar_tensor_tensor(out=A1(), in0=A1(), scalar=1.0, in1=nb4[0][:], op0=ALU.add, op1=ALU.mult),
                 reads=[modb, nb4[0]], writes=[modb])
            P.op("dve", lambda e: e.tensor_tensor(out=G1(), in0=G1(), in1=nb4[1][:], op=ALU.mult), reads=[modb, nb4[1]], writes=[modb])
            P.op("dve", lambda e: e.scalar_tensor_tensor(out=A2(), in0=A2(), scalar=1.0, in1=nb4[2][:], op0=ALU.add, op1=ALU.mult),
                 reads=[modb, nb4[2]], writes=[modb])
            P.op("dve", lambda e: e.tensor_tensor(out=G2(), in0=G2(), in1=nb4[3][:], op=ALU.mult), reads=[modb, nb4[3]], writes=[modb])
            for i, src in enumerate([lq1, lk1, lq2, lk2]):
                P.op("sp", lambda e: e.dma_start(out=l4[:, i, :], in_=src[0:1, :].partition_broadcast(128)), writes=[l4], dma=True)
            P.op("dve", lambda e: e.tensor_tensor(out=lt[:, 0, :], in0=l4[:, 0, :], in1=l4[:, 1, :], op=ALU.mult), reads=[l4], writes=[lt])
            P.op("dve", lambda e: e.tensor_tensor(out=lt[:, 1, :], in0=l4[:, 2, :], in1=l4[:, 3, :], op=ALU.mult), reads=[l4], writes=[lt])
            P.op("dve", lambda e: e.reduce_sum(out=ls[:], in_=lt[:], axis=mybir.AxisListType.X), reads=[lt], writes=[ls])
            P.op("act", lambda e: e.activation(out=ls[:], in_=ls[:], func=AF.Exp), reads=[ls], writes=[ls])
            P.op("dve", lambda e: e.tensor_tensor(out=nlam[:], in0=ls[:, 1:2], in1=ls[:, 0:1], op=ALU.subtract), reads=[ls], writes=[nlam])
            P.op("dve", lambda e: e.tensor_scalar(out=nlam[:], in0=nlam[:], scalar1=-0.2, scalar2=None, op0=ALU.add), reads=[nlam], writes=[nlam])
            P.op("sp", lambda e: e.dma_start(out=subln_b[:], in_=da_subln[0:1, :].partition_broadcast(128)), writes=[subln_b], dma=True)
            P.op("dve", lambda e: e.tensor_scalar(out=subln_b[:], in0=subln_b[:], scalar1=0.8, scalar2=None, op0=ALU.mult), reads=[subln_b], writes=[subln_b])
            P.op("sp", lambda e: e.dma_start(out=hgn_b[:], in_=hg_norm[0:1, :].partition_broadcast(128)), writes=[hgn_b], dma=True)
            P.op("sp", lambda e: e.dma_start(out=lbl[:], in_=lbl_d[:, :, :]), writes=[lbl], dma=True)
            P.op("dve", lambda e: e.tensor_tensor(out=lb[:], in0=lbl[:, 0, :], in1=lbl[:, 1, :], op=ALU.subtract), reads=[lbl], writes=[lb])
            P.op("act", lambda e: e.activation(out=lb[:], in_=lb[:], func=AF.Sigmoid), reads=[lb], writes=[lb])
            P.op("dve", lambda e: e.tensor_scalar(out=oml[:], in0=lb[:], scalar1=-1.0, scalar2=1.0, op0=ALU.mult, op1=ALU.add), reads=[lb], writes=[oml])

        if upto >= 1:
            with ExitStack() as ph:
                xt = [sb(ph, "xt%d" % i, [128, D], F32) for i in range(3)]
                junk = sb(ph, "junk", [128, D], F32)
                tmp = sb(ph, "tmp", [128, D], F32)
                hb = [sb(ph, "hb%d" % i, [128, D], BF16) for i in range(2)]
                hTt = [sb(ph, "hTt%d" % i, [128, 8, 128], BF16) for i in range(2)]
                ss = [sb(ph, "ss%d" % i, [128, 1], F32) for i in range(2)]
                st = [sb(ph, "st%d" % i, [128, 1], F32) for i in range(2)]
                pT = [ps(ph, "pT%d" % i, [128, 8, 128], BF16) for i in range(2)]

                def ld(i):
                    P.op("sp", lambda e: e.dma_start(out=xt[i % 3][:], in_=xall[i * 128:(i + 1) * 128, :]), writes=[xt[i % 3]], dma=True)

                ld(0)
                ld(1)

                def stA(i):
                    x_ = xt[i % 3]
                    s_ = ss[i % 2]
                    t_ = st[i % 2]
                    h_ = hb[i % 2]
                    P.op("act", lambda e: e.activation(out=junk[:], in_=x_[:], func=AF.Square, accum_out=s_[:, 0:1]), reads=[x_], writes=[junk, s_])
                    rstd_from_ss(s_, D, t_)
                    P.op("dve", lambda e: e.scalar_tensor_tensor(out=tmp[:], in0=x_[:], scalar=s_[:, 0:1], in1=A1(), op0=ALU.mult, op1=ALU.mult),
                         reads=[x_, s_, modb], writes=[tmp])
                    P.op("dve", lambda e: e.tensor_tensor(out=h_[:], in0=tmp[:], in1=B1(), op=ALU.add), reads=[tmp, modb], writes=[h_])

                stA(0)
                for i in range(NT):
                    if i + 2 < NT:
                        ld(i + 2)
                    if i + 1 < NT:
                        stA(i + 1)
                    h_ = hb[i % 2]
                    p_ = pT[i % 2]
                    o_ = hTt[i % 2]
                    for kc in range(8):
                        P.op("pe", lambda e: e.transpose(out=p_[:, kc, :], in_=h_[:, kc * 128:(kc + 1) * 128], identity=ident[:]),
                             reads=[h_, ident], writes=[p_])
                    P.op("act", lambda e: e.copy(out=o_[:], in_=p_[:]), reads=[p_], writes=[o_])
                    P.op("sp", lambda e: e.dma_start(out=HT[i], in_=o_[:]), reads=[o_], dma=True)
                P.barrier()

        st0.close()

        if upto >= 2:
            with ExitStack() as ph:
                wq = sb(ph, "wq", [128, 8, 512], BF16)
                wk = sb(ph, "wk", [128, 8, 512], BF16)
                wv = sb(ph, "wv", [128, 8, 512], BF16)
                wqs = sb(ph, "wqs", [128, 8, 512], BF16)
                wks = sb(ph, "wks", [128, 8, 512], BF16)
                for w_, c0 in ((wq, 0), (wk, 512), (wv, 1024)):
                    for kh in range(2):
                        P.op("pool", lambda e: e.dma_start(out=w_[:, kh * 4:(kh + 1) * 4, :], in_=w_in_v[:, kh * 4:(kh + 1) * 4, c0:c0 + 512]),
                             writes=[w_], dma=True)
                for w_, ws_ in ((wq, wqs), (wk, wks)):
                    src = w_[:].rearrange("p k (g two j) -> p k g two j", two=2, j=32)
                    dst = ws_[:].rearrange("p k (g two j) -> p k g two j", two=2, j=32)
                    for kc in range(8):
                        P.op("dve", lambda e: e.tensor_copy(out=dst[:, kc, :, 0, :], in_=src[:, kc, :, 1, :]), reads=[w_], writes=[ws_])
                        P.op("dve", lambda e: e.tensor_copy(out=dst[:, kc, :, 1, :], in_=src[:, kc, :, 0, :]), reads=[w_], writes=[ws_])
                hTb = [sb(ph, "hTb%d" % i, [128, 4, 8, 128], BF16) for i in range(2)]
                cs = [sb(ph, "cs%d" % i, [128, 512], F32) for i in range(2)]
                sn = [sb(ph, "sn%d" % i, [128, 512], F32) for i in range(2)]
                t1 = [sb(ph, "t1_%d" % i, [128, 512], F32) for i in range(2)]
                t2 = [sb(ph, "t2_%d" % i, [128, 512], F32) for i in range(2)]
                kts = [sb(ph, "kts%d" % i, [128, 4, 512], BF16) for i in range(2)]
                qts = [sb(ph, "qts%d" % i, [128, 4, 512], BF16) for i in range(2)]
                vst = [sb(ph, "vst%d" % i, [128, 4, 4, 130], BF16) for i in range(2)]
                pA = [ps(ph, "pA%d" % i, [128, 512], F32) for i in range(2)]
                pB = [ps(ph, "pB%d" % i, [128, 512], F32) for i in range(2)]
                pV = [ps(ph, "pV%d" % i, [128, 512], F32) for i in range(2)]
                for v_ in vst:
                    P.op("pool", lambda e: e.memset(v_[:], 0.0), writes=[v_])

                def ld2(bi):
                    P.op("sp", lambda e: e.dma_start(out=hTb[bi % 2][:], in_=HT[bi * 4:(bi + 1) * 4].rearrange("t p k j -> p t k j")),
                         writes=[hTb[bi % 2]], dma=True)
                    P.op("sp", lambda e: e.dma_start(out=cs[bi % 2][:], in_=cosT[:, bi * 512:(bi + 1) * 512]), writes=[cs[bi % 2]], dma=True)
                    P.op("sp", lambda e: e.dma_start(out=sn[bi % 2][:], in_=sinT[:, bi * 512:(bi + 1) * 512]), writes=[sn[bi % 2]], dma=True)

                ld2(0)
                cnt = 0
                for bi in range(16):
                    if bi + 1 < 16:
                        ld2(bi + 1)
                    own = bi >= 8
                    hb_ = hTb[bi % 2]
                    cs_ = cs[bi % 2]
                    sn_ = sn[bi % 2]
                    jobs = [(wk, wks, kts[bi % 2])]
                    if own:
                        jobs.append((wq, wqs, qts[bi % 2]))
                    for (w_, ws_, dst_) in jobs:
                        for h in range(4):
                            a_ = pA[cnt % 2]
                            b_ = pB[cnt % 2]
                            u1 = t1[cnt % 2]
                            u2 = t2[cnt % 2]
                            cnt += 1
                            for kc in range(8):
                                P.op("pe", lambda e: e.matmul(a_[:].rearrange("p (t j) -> p t j", j=128), lhsT=w_[:, kc, h * 128:(h + 1) * 128],
                                                              rhs=hb_[:, :, kc, :], start=(kc == 0), stop=(kc == 7)), reads=[w_, hb_], writes=[a_])
                            for kc in range(8):
                                P.op("pe", lambda e: e.matmul(b_[:].rearrange("p (t j) -> p t j", j=128), lhsT=ws_[:, kc, h * 128:(h + 1) * 128],
                                                              rhs=hb_[:, :, kc, :], start=(kc == 0), stop=(kc == 7)), reads=[ws_, hb_], writes=[b_])
                            P.op("dve", lambda e: e.tensor_tensor(out=u1[:], in0=a_[:], in1=cs_[:], op=ALU.mult), reads=[a_, cs_], writes=[u1])
                            P.op("dve", lambda e: e.tensor_tensor(out=u2[:], in0=b_[:], in1=sn_[:], op=ALU.mult), reads=[b_, sn_], writes=[u2])
                            P.op("dve", lambda e: e.tensor_tensor(out=dst_[:, h, :], in0=u1[:], in1=u2[:], op=ALU.add), reads=[u1, u2], writes=[dst_])
                    for h in range(4):
                        P.op("sp", lambda e: e.dma_start(out=KT[h, :, bi * 512:(bi + 1) * 512], in_=kts[bi % 2][:, h, :]), reads=[kts[bi % 2]], dma=True)
                        if own:
                            P.op("sp", lambda e: e.dma_start(out=QT[h, :, (bi - 8) * 512:(bi - 7) * 512], in_=qts[bi % 2][:, h, :]),
                                 reads=[qts[bi % 2]], dma=True)
                    v_ = vst[bi % 2]
                    for sub in range(4):
                        p_ = pV[sub % 2]
                        for kc in range(8):
                            P.op("pe", lambda e: e.matmul(p_[:], lhsT=hb_[:, sub, kc, :], rhs=wv[:, kc, :], start=(kc == 0), stop=(kc == 7)),
                                 reads=[hb_, wv], writes=[p_])
                        pv3 = p_[:].rearrange("p (h v) -> p h v", v=128)
                        if own:
                            P.op("act", lambda e: e.copy(out=v_[:, sub, :, 0:128], in_=pv3), reads=[p_], writes=[v_])
                        else:
                            P.op("dve", lambda e: e.tensor_scalar(out=v_[:, sub, :, 0:128], in0=pv3, scalar1=flag[:, 0:1], scalar2=None, op0=ALU.mult),
                                 reads=[p_, flag], writes=[v_])
                    if own:
                        P.op("pool", lambda e: e.memset(v_[:, :, :, 128:129], 1.0), writes=[v_])
                    else:
                        P.op("dve", lambda e: e.tensor_copy(out=v_[:, :, :, 128:129], in_=flag[:, 0:1].unsqueeze(1).unsqueeze(1).to_broadcast([128, 4, 4, 1])),
                             reads=[flag], writes=[v_])
                    for h in range(4):
                        P.op("sp", lambda e: e.dma_start(out=VS[h, :, bi * 4:(bi + 1) * 4, :], in_=v_[:, :, h, :]), reads=[v_], dma=True)
                P.barrier()

        if upto >= 3:
            with ExitStack() as ph:
                KTh = [sb(ph, "KTh%d" % i, [128, SEQ], BF16) for i in range(2)]
                QTh = [sb(ph, "QTh%d" % i, [128, 2, TOWN], BF16) for i in range(2)]
                for q_ in QTh:
                    P.op("dve", lambda e: e.memset(q_[64:128, 0, :], 0.0), writes=[q_])
                    P.op("dve", lambda e: e.memset(q_[0:64, 1, :], 0.0), writes=[q_])
                Vh = [sb(ph, "Vh%d" % i, [128, NT, 130], BF16) for i in range(2)]
                PT = [sb(ph, "PT%d" % i, [128, 512], BF16) for i in range(6)]
                Sps = [ps(ph, "Sps%d" % i, [128, 512], F32) for i in range(4)]
                OT1 = [ps(ph, "OT_%d" % m, [128, 512], F32) for m in range(2)]
                OT = [OT1, OT1]
                sumP = [ps(ph, "sumP_%d" % m, [128, 512], F32) for m in range(2)]
                pE = OT1[1]
                o0s = sb(ph, "o0s", [128, 512], F32)
                o1s = sb(ph, "o1s", [128, 512], F32)
                racc = [[sb(ph, "racc%d_%d" % (jp, m), [128, 512], F32) for m in range(2)] for jp in range(2)]
                ones32 = sb(ph, "ones32", [128, 128], F32)
                subcol = sb(ph, "subcol", [128, 1], F32)
                rr = [sb(ph, "rr%d" % i, [128, 512], F32) for i in range(2)]
                tq = sb(ph, "tq", [128, 512], F32)
                uq = sb(ph, "uq", [128, 512], F32)
                oq = sb(ph, "oq", [128, 512], F32)
                o2 = sb(ph, "o2", [128, 512], F32)
                rs = sb(ph, "rs", [128, 512], F32)
                yaTs = [sb(ph, "yaTs%d" % i, [128, 512], BF16) for i in range(2)]
                P.op("pool", lambda e: e.memset(ones32[:], 1.0), writes=[ones32])
                onesB = sb(ph, "onesB", [128, 128], BF16)
                onesF = sb(ph, "onesF", [128, 128], BF16)
                P.op("pool", lambda e: e.memset(onesB[:], 1.0), writes=[onesB])
                P.op("dve", lambda e: e.tensor_scalar(out=onesF[:], in0=ones32[:], scalar1=flag[:, 0:1], scalar2=None, op0=ALU.mult),
                     reads=[ones32, flag], writes=[onesF])
                P.op("sp", lambda e: e.dma_start(out=subcol[:], in_=subln_col[:, :]), writes=[subcol], dma=True)
                P.op("dve", lambda e: e.tensor_scalar(out=subcol[:], in0=subcol[:], scalar1=0.8, scalar2=None, op0=ALU.mult), reads=[subcol], writes=[subcol])

                def ld3(h):
                    P.op("sp", lambda e: e.dma_start(out=KTh[h % 2][:], in_=KT[h]), writes=[KTh[h % 2]], dma=True)
                    P.op("sp", lambda e: e.dma_start(out=QTh[h % 2][0:64, 0, :], in_=QT[h, 0:64, :]), writes=[QTh[h % 2]], dma=True)
                    P.op("sp", lambda e: e.dma_start(out=QTh[h % 2][64:128, 1, :], in_=QT[h, 64:128, :]), writes=[QTh[h % 2]], dma=True)
                    P.op("sp", lambda e: e.dma_start(out=Vh[h % 2][:], in_=VS[h]), writes=[Vh[h % 2]], dma=True)

                ld3(0)
                if upto >= 6:
                    stg = [sb(ph, "stg%d" % i, [128, 4, 2 * D], BF16) for i in range(2)]
                    ci = 0
                    WG4 = WGUB.rearrange("(e p k) n -> e p k n", p=128, k=8)
                    WD4 = WDNB.rearrange("(e p k) n -> e p k n", p=128, k=8)
                    for c in range(NE * D // 512):
                        st_ = stg[ci % 2]
                        ci += 1
                        P.op("pool", lambda e: e.dma_start(out=st_[:], in_=w_gu[c * 512:(c + 1) * 512, :].rearrange("(a p) n -> p a n", p=128)),
                             writes=[st_], dma=True, ring="pre")
                        P.op("pool", lambda e: e.dma_start(out=WG4[c // 2, :, (c % 2) * 4:(c % 2) * 4 + 4, :], in_=st_[:]),
                             reads=[st_], dma=True, ring="pre")
                    for c in range(NE):
                        st_ = stg[ci % 2]
                        ci += 1
                        v_ = st_[:].rearrange("p a (b n) -> p (a b) n", b=2)
                        P.op("pool", lambda e: e.dma_start(out=v_, in_=w_dn[c * 1024:(c + 1) * 1024, :].rearrange("(a p) n -> p a n", p=128)),
                             writes=[st_], dma=True, ring="pre")
                        P.op("pool", lambda e: e.dma_start(out=WD4[c, :, :, :], in_=v_), reads=[st_], dma=True, ring="pre")
                gs_ = 0
                for h in range(4):
                    if h + 1 < 4:
                        ld3(h + 1)
                    K_ = KTh[h % 2]
                    Q_ = QTh[h % 2]
                    V_ = Vh[h % 2]
                    steps = []
                    for j in range(8):
                        for m in range(2):
                            kts = list(range(32)) + [32 + t for t in range(4 * j + 4)]
                            for idx, kt in enumerate(kts):
                                steps.append((j, m, idx, kt, idx == len(kts) - 1))

                    def geom(st):
                        j, m, idx, kt, last = st
                        own_t = kt - 32
                        dsub = own_t - 4 * j if own_t >= 4 * j else -1
                        return dsub, max(dsub, 0) * 128

                    def front(si):
                        j, m, idx, kt, last = steps[si]
                        dsub, c0 = geom(steps[si])
                        sp_ = Sps[(gs_ + si) % 4]
                        pt_ = PT[(gs_ + si) % 6]
                        P.op("pe", lambda e: e.matmul(sp_[:, c0:512], lhsT=K_[:, kt * 128:(kt + 1) * 128],
                                                      rhs=Q_[:, m, j * 512 + c0:(j + 1) * 512], start=True, stop=True),
                             reads=[K_, Q_], writes=[sp_])
                        P.op("act", lambda e: e.activation(out=pt_[:, c0:512], in_=sp_[:, c0:512], func=AF.Exp, scale=0.125),
                             reads=[sp_], writes=[pt_])
                        if dsub >= 0:
                            P.op("act", lambda e: e.mul(out=pt_[64:128, c0:c0 + 64], in_=pt_[64:128, c0:c0 + 64], mul=0.0), reads=[pt_], writes=[pt_])

                    def back(si):
                        j, m, idx, kt, last = steps[si]
                        dsub, c0 = geom(steps[si])
                        pt_ = PT[(gs_ + si) % 6]
                        ra = racc[j % 2][m]
                        ot = OT[j % 2][m]
                        sp2 = sumP[m]
                        if idx % 2 == 0:
                            P.op("pe", lambda e: e.matmul(sp2[:, c0:512], lhsT=(onesF if kt < 32 else onesB)[:], rhs=pt_[:, c0:512], start=(idx == 0), stop=False),
                                 reads=[onesF, onesB, pt_], writes=[sp2])
                        elif idx == 1:
                            P.op("dve", lambda e: e.tensor_scalar(out=ra[:], in0=pt_[:], scalar1=flag[:, 0:1], scalar2=None, op0=ALU.mult),
                                 reads=[pt_, flag], writes=[ra])
                        elif kt < 32:
                            P.op("dve", lambda e: e.scalar_tensor_tensor(out=ra[:], in0=pt_[:], scalar=flag[:, 0:1], in1=ra[:], op0=ALU.mult, op1=ALU.add),
                                 reads=[ra, pt_, flag], writes=[ra])
                        else:
                            P.op("dve", lambda e: e.tensor_tensor(out=ra[:, c0:512], in0=ra[:, c0:512], in1=pt_[:, c0:512], op=ALU.add),
                                 reads=[ra, pt_], writes=[ra])
                        P.op("pe", lambda e: e.matmul(ot[:, c0:512], lhsT=V_[:, kt, 0:128], rhs=pt_[:, c0:512], start=(idx == 0), stop=last),
                             reads=[V_, pt_], writes=[ot])
                        if last and m == 1:
                            epi(j)

                    def epi(j):
                        ys_ = yaTs[j % 2]
                        for m in range(2):
                            P.op("pe", lambda e: e.matmul(sumP[m][:], lhsT=ones32[:], rhs=racc[j % 2][m][:], start=False, stop=True),
                                 reads=[ones32, racc[j % 2][m]], writes=[sumP[m]])
                        P.op("act", lambda e: e.copy(out=o0s[:], in_=OT[j % 2][0][:]), reads=[OT[j % 2][0]], writes=[o0s])
                        P.op("dve", lambda e: e.reciprocal(out=rr[0][:], in_=sumP[0][:]), reads=[sumP[0]], writes=[rr[0]])
                        P.op("act", lambda e: e.copy(out=o1s[:], in_=OT[j % 2][1][:]), reads=[OT[j % 2][1]], writes=[o1s])
                        P.op("dve", lambda e: e.reciprocal(out=rr[1][:], in_=sumP[1][:]), reads=[sumP[1]], writes=[rr[1]])
                        P.op("dve", lambda e: e.tensor_tensor(out=tq[:], in0=o0s[:], in1=rr[0][:], op=ALU.mult), reads=[o0s, rr[0]], writes=[tq])
                        P.op("dve", lambda e: e.tensor_scalar(out=rr[1][:], in0=rr[1][:], scalar1=nlam[:, 0:1], scalar2=None, op0=ALU.mult),
                             reads=[rr[1], nlam], writes=[rr[1]])
                        P.op("dve", lambda e: e.tensor_tensor(out=uq[:], in0=o1s[:], in1=rr[1][:], op=ALU.mult), reads=[o1s, rr[1]], writes=[uq])
                        P.op("dve", lambda e: e.tensor_tensor(out=oq[:], in0=tq[:], in1=uq[:], op=ALU.add), reads=[tq, uq], writes=[oq])
                        P.op("dve", lambda e: e.tensor_tensor(out=o2[:], in0=oq[:], in1=oq[:], op=ALU.mult), reads=[oq], writes=[o2])
                        P.op("pe", lambda e: e.matmul(pE[:], lhsT=ones32[:], rhs=o2[:], start=True, stop=True), reads=[ones32, o2], writes=[pE])
                        P.op("act", lambda e: e.activation(out=rs[:], in_=pE[:], func=AF.Ln, bias=EPS, scale=1.0 / 128), reads=[pE], writes=[rs])
                        P.op("act", lambda e: e.activation(out=rs[:], in_=rs[:], func=AF.Exp, scale=-0.5), reads=[rs], writes=[rs])
                        P.op("dve", lambda e: e.scalar_tensor_tensor(out=ys_[:], in0=oq[:], scalar=subcol[:, 0:1], in1=rs[:], op0=ALU.mult, op1=ALU.mult),
                             reads=[oq, subcol, rs], writes=[ys_])
                        P.op("sp", lambda e: e.dma_start(out=YAT[h, :, j * 512:(j + 1) * 512], in_=ys_[:]), reads=[ys_], dma=True)

                    n = len(steps)
                    for si in range(n + 3):
                        if si < n:
                            front(si)
                        if si >= 3:
                            back(si - 3)
                    gs_ += n
                P.barrier()

        if upto >= 4:
            with ExitStack() as ph:
                whq = sb(ph, "whq", [128, 8, 512], BF16)
                whf = sb(ph, "whf", [128, 8, 512], BF16)
                whi = sb(ph, "whi", [128, 8, 512], BF16)
                whg = sb(ph, "whg", [128, 8, 512], BF16)
                for w_, c0 in ((whq, 1536), (whf, 2048), (whi, 2560), (whg, 3072)):
                    for kh in range(2):
                        P.op("pool", lambda e: e.dma_start(out=w_[:, kh * 4:(kh + 1) * 4, :], in_=w_in_v[:, kh * 4:(kh + 1) * 4, c0:c0 + 512]),
                             writes=[w_], dma=True)
                rm = sb(ph, "rm", [128, 512], F32)
                P.op("pool", lambda e: e.memset(rm[:], 1.0), writes=[rm])
                for t in range(4):
                    P.op("pool", lambda e: e.memset(rm[:, t * 128:t * 128 + 1], 0.0), writes=[rm])
                cmask = sb(ph, "cmask", [128, 128], F32)
                P.op("pool", lambda e: e.memset(cmask[:], 1.0), writes=[cmask])
                P.op("pool", lambda e: e.affine_select(out=cmask[:], in_=cmask[:], pattern=[[1, 128]], compare_op=ALU.is_ge,
                                                       fill=0.0, base=0, channel_multiplier=-1), reads=[cmask], writes=[cmask])
                hTb = [sb(ph, "hTb4_%d" % i, [128, 4, 8, 128], BF16) for i in range(2)]
                sg = [sb(ph, "sg%d" % i, [128, 512], F32) for i in range(4)]
                lf = [sb(ph, "lf%d" % i, [128, 512], F32) for i in range(4)]
                kk = [sb(ph, "kk%d" % i, [128, 512], F32) for i in range(4)]
                Gc = [sb(ph, "Gc%d" % i, [128, 512], F32) for i in range(4)]
                eg = [sb(ph, "eg%d" % i, [128, 512], F32) for i in range(4)]
                kt32 = [sb(ph, "kt32_%d" % i, [128, 512], F32) for i in range(4)]
                qs = [sb(ph, "qs%d" % i, [128, 512], F32) for i in range(4)]
                qraw = [sb(ph, "qraw%d" % i, [128, 512], F32) for i in range(4)]
                gex = [sb(ph, "gex%d" % i, [128, 512], F32) for i in range(2)]
                dec_all = sb(ph, "dec_all", [128, 4, NT], F32)
                q_t = sb(ph, "q_t", [128, 4, 512], BF16)
                k_t = sb(ph, "k_t", [128, 4, 512], BF16)
                kdec = sb(ph, "kdec", [128, 4, 512], BF16)
                kdT = sb(ph, "kdT", [128, 4, 4, 128], BF16)
                vtm = sb(ph, "vtm", [128, 4, 512], BF16)
                gsn = sb(ph, "gsn", [128, 4, 512], F32)
                S32 = sb(ph, "S32", [128, 4, 128], F32)
                yb = [sb(ph, "yb%d" % i, [128, 512], BF16) for i in range(2)]
                ybTs = [sb(ph, "ybTs%d" % i, [128, 4, 512], BF16) for i in range(2)]
                pF = [ps(ph, "pF%d" % i, [128, 512], F32) for i in range(2)]
                pAT = [ps(ph, "pAT%d" % i, [128, 4, 128], F32) for i in range(2)]
                pO = [ps(ph, "pO%d" % i, [128, 4, 128], F32) for i in range(2)]
                pS = ps(ph, "pS", [128, 4, 128], F32)
                AT4 = [sb(ph, "AT4_%d" % i, [128, 4, 128], BF16) for i in range(2)]
                Sbf4 = sb(ph, "Sbf4", [128, 4, 128], BF16)
                sq4 = sb(ph, "sq4", [128, 4, 128], F32)
                tm4 = sb(ph, "tm4", [128, 4, 128], F32)
                ss4 = [sb(ph, "ss4_%d" % i, [128, 4], F32) for i in range(2)]
                st4 = [sb(ph, "st4_%d" % i, [128, 4], F32) for i in range(2)]
                P.op("pool", lambda e: e.memset(Sbf4[:], 0.0), writes=[Sbf4])
                P.op("dve", lambda e: e.memset(S32[:], 0.0), writes=[S32])
                pTk16 = ps(ph, "pTk16", [128, 4, 128], BF16)
                if upto >= 5:
                    zt = sb(ph, "zt", [128, 1024], BF16)
                    P.op("pool", lambda e: e.memset(zt[:], 0.0), writes=[zt])
                    for i in range(NSLOT // 128):
                        P.op("pool", lambda e: e.dma_start(out=XS[i * 128:(i + 1) * 128, :], in_=zt[:]), reads=[zt], dma=True, ring="pre")

                def ld4(bi):
                    P.op("sp", lambda e: e.dma_start(out=hTb[bi % 2][:], in_=HT[bi * 4:(bi + 1) * 4].rearrange("t p k j -> p t k j")),
                         writes=[hTb[bi % 2]], dma=True)

                ld4(0)
                cnt = 0
                oc = 0
                for bi in range(16):
                    if bi + 1 < 16:
                        ld4(bi + 1)
                    own = bi >= 8
                    hb_ = hTb[bi % 2]
                    rr3 = lambda ap: ap.rearrange("p (t j) -> p t j", j=128)
                    for h in range(4):
                        p_ = pF[cnt % 2]
                        cnt += 1
                        for kc in range(8):
                            P.op("pe", lambda e: e.matmul(rr3(p_[:]), lhsT=whf[:, kc, h * 128:(h + 1) * 128],
                                                          rhs=hb_[:, :, kc, :], start=(kc == 0), stop=(kc == 7)), reads=[whf, hb_], writes=[p_])
                        P.op("act", lambda e: e.activation(out=sg[h][:], in_=p_[:], func=AF.Exp, scale=-1.0), reads=[p_], writes=[sg[h]])
                    if own:
                        for h in range(4):
                            p_ = pF[cnt % 2]
                            cnt += 1
                            for kc in range(8):
                                P.op("pe", lambda e: e.matmul(rr3(p_[:]), lhsT=whq[:, kc, h * 128:(h + 1) * 128],
                                                              rhs=hb_[:, :, kc, :], start=(kc == 0), stop=(kc == 7)), reads=[whq, hb_], writes=[p_])
                            P.op("act", lambda e: e.copy(out=qraw[h][:], in_=p_[:]), reads=[p_], writes=[qraw[h]])
                            P.op("act", lambda e: e.activation(out=qs[h][:], in_=p_[:], func=AF.Exp, scale=-1.0), reads=[p_], writes=[qs[h]])
                    for h in range(4):
                        P.op("act", lambda e: e.activation(out=sg[h][:], in_=sg[h][:], func=AF.Ln, bias=1.0, scale=1.0), reads=[sg[h]], writes=[sg[h]])
                        P.op("act", lambda e: e.activation(out=sg[h][:], in_=sg[h][:], func=AF.Exp, scale=-1.0), reads=[sg[h]], writes=[sg[h]])
                        P.op("dve", lambda e: e.tensor_scalar(out=sg[h][:], in0=sg[h][:], scalar1=oml[:, h:h + 1], scalar2=lb[:, h:h + 1],
                                                              op0=ALU.mult, op1=ALU.add), reads=[sg[h], oml, lb], writes=[sg[h]])
                    for h in range(4):
                        P.op("act", lambda e: e.activation(out=lf[h][:], in_=sg[h][:], func=AF.Ln), reads=[sg[h]], writes=[lf[h]])
                    for h in range(4):
                        P.op("dve", lambda e: e.tensor_scalar(out=kk[h][:], in0=sg[h][:], scalar1=-1.0, scalar2=1.0, op0=ALU.mult, op1=ALU.add),
                             reads=[sg[h]], writes=[kk[h]])
                        P.op("dve", lambda e: e.tensor_tensor_scan(out=Gc[h][:], data0=rm[:], data1=lf[h][:], initial=0.0, op0=ALU.mult, op1=ALU.add),
                             reads=[rm, lf[h]], writes=[Gc[h]])
                    for h in range(4):
                        P.op("act", lambda e: e.activation(out=eg[h][:], in_=Gc[h][:], func=AF.Exp, scale=-1.0), reads=[Gc[h]], writes=[eg[h]])
                        P.op("act", lambda e: e.activation(out=dec_all[:, h, bi * 4:(bi + 1) * 4], in_=rr3(Gc[h][:])[:, :, 127], func=AF.Exp),
                             reads=[Gc[h]], writes=[dec_all])
                    for h in range(4):
                        P.op("dve", lambda e: e.tensor_tensor(out=kt32[h][:], in0=kk[h][:], in1=eg[h][:], op=ALU.mult), reads=[kk[h], eg[h]], writes=[kt32[h]])
                        P.op("dve", lambda e: e.tensor_tensor(out=rr3(kdec[:, h, :]), in0=rr3(kt32[h][:]),
                                                              in1=dec_all[:, h, bi * 4:(bi + 1) * 4].unsqueeze(2).to_broadcast([128, 4, 128]),
                                                              op=ALU.mult), reads=[kt32[h], dec_all], writes=[kdec])
                    for h in range(4):
                        for sub in range(4):
                            P.op("pe", lambda e: e.transpose(out=pTk16[:, sub, :], in_=kdec[:, h, sub * 128:(sub + 1) * 128], identity=ident[:]),
                                 reads=[kdec, ident], writes=[pTk16])
                        P.op("act", lambda e: e.copy(out=kdT[:, :, h, :], in_=pTk16[:]), reads=[pTk16], writes=[kdT])
                    if own:
                        for h in range(4):
                            P.op("act", lambda e: e.copy(out=k_t[:, h, :], in_=kt32[h][:]), reads=[kt32[h]], writes=[k_t])
                            P.op("act", lambda e: e.activation(out=eg[h][:], in_=Gc[h][:], func=AF.Exp), reads=[Gc[h]], writes=[eg[h]])
                        for h in range(4):
                            P.op("act", lambda e: e.activation(out=qs[h][:], in_=qs[h][:], func=AF.Ln, bias=1.0, scale=1.0), reads=[qs[h]], writes=[qs[h]])
                            P.op("act", lambda e: e.activation(out=qs[h][:], in_=qs[h][:], func=AF.Exp, scale=-1.0), reads=[qs[h]], writes=[qs[h]])
                            P.op("dve", lambda e: e.tensor_tensor(out=qs[h][:], in0=qs[h][:], in1=qraw[h][:], op=ALU.mult), reads=[qs[h], qraw[h]], writes=[qs[h]])
                            P.op("dve", lambda e: e.tensor_tensor(out=q_t[:, h, :], in0=qs[h][:], in1=eg[h][:], op=ALU.mult), reads=[qs[h], eg[h]], writes=[q_t])
                    for sub in range(4):
                        p_ = pF[cnt % 2]
                        cnt += 1
                        for kc in range(8):
                            P.op("pe", lambda e: e.matmul(p_[:], lhsT=hb_[:, sub, kc, :], rhs=whi[:, kc, :], start=(kc == 0), stop=(kc == 7)),
                                 reads=[hb_, whi], writes=[p_])
                        if own:
                            P.op("act", lambda e: e.copy(out=vtm[:, sub, :], in_=p_[:]), reads=[p_], writes=[vtm])
                        else:
                            P.op("dve", lambda e: e.tensor_scalar(out=vtm[:, sub, :], in0=p_[:], scalar1=flag[:, 0:1], scalar2=None, op0=ALU.mult),
                                 reads=[p_, flag], writes=[vtm])
                        if own:
                            p2 = pF[cnt % 2]
                            cnt += 1
                            gx = gex[sub % 2]
                            for kc in range(8):
                                P.op("pe", lambda e: e.matmul(p2[:], lhsT=hb_[:, sub, kc, :], rhs=whg[:, kc, :], start=(kc == 0), stop=(kc == 7)),
                                     reads=[hb_, whg], writes=[p2])
                            P.op("act", lambda e: e.activation(out=gx[:], in_=p2[:], func=AF.Exp, scale=-1.0), reads=[p2], writes=[gx])
                            P.op("act", lambda e: e.copy(out=gsn[:, sub, :], in_=p2[:]), reads=[p2], writes=[gsn])
                            P.op("act", lambda e: e.activation(out=gx[:], in_=gx[:], func=AF.Ln, bias=1.0, scale=1.0), reads=[gx], writes=[gx])
                            P.op("act", lambda e: e.activation(out=gx[:], in_=gx[:], func=AF.Exp, scale=-1.0), reads=[gx], writes=[gx])
                            P.op("dve", lambda e: e.tensor_tensor(out=gsn[:, sub, :], in0=gsn[:, sub, :], in1=gx[:], op=ALU.mult), reads=[gsn, gx], writes=[gsn])
                            P.op("dve", lambda e: e.tensor_tensor(out=gsn[:, sub, :].rearrange("p (h v) -> p h v", v=128),
                                                                  in0=gsn[:, sub, :].rearrange("p (h v) -> p h v", v=128),
                                                                  in1=hgn_b[:].unsqueeze(1).to_broadcast([128, 4, 128]), op=ALU.mult),
                                 reads=[gsn, hgn_b], writes=[gsn])
                    for sub in range(4):
                        tile = bi * 4 + sub
                        yb_ = yb[sub % 2]
                        cols = slice(sub * 128, (sub + 1) * 128)
                        if own:
                            at_p = pAT[oc % 2]
                            at_s = AT4[oc % 2]
                            o_p = pO[oc % 2]
                            s_ = ss4[oc % 2]
                            st_ = st4[oc % 2]
                            oc += 1
                            for h in range(4):
                                P.op("pe", lambda e: e.matmul(at_p[:, h, :], lhsT=k_t[:, h, cols], rhs=q_t[:, h, cols], start=True, stop=True),
                                     reads=[k_t, q_t], writes=[at_p])
                            P.op("dve", lambda e: e.tensor_tensor(out=at_s[:], in0=at_p[:], in1=cmask[:].unsqueeze(1).to_broadcast([128, 4, 128]), op=ALU.mult),
                                 reads=[at_p, cmask], writes=[at_s])
                            for h in range(4):
                                P.op("pe", lambda e: e.matmul(o_p[:, h, :], lhsT=at_s[:, h, :], rhs=vtm[:, sub, h * 128:(h + 1) * 128], start=True, stop=False),
                                     reads=[at_s, vtm], writes=[o_p])
                                P.op("pe", lambda e: e.matmul(o_p[:, h, :], lhsT=q_t[:, h, cols], rhs=Sbf4[:, h, :], start=False, stop=True),
                                     reads=[q_t, Sbf4], writes=[o_p])
                            P.op("act", lambda e: e.activation(out=sq4[:], in_=o_p[:], func=AF.Square), reads=[o_p], writes=[sq4])
                            P.op("dve", lambda e: e.reduce_sum(out=s_[:], in_=sq4[:], axis=mybir.AxisListType.X), reads=[sq4], writes=[s_])
                            P.op("dve", lambda e: e.tensor_scalar(out=st_[:], in0=s_[:], scalar1=1.0 / 128, scalar2=EPS, op0=ALU.mult, op1=ALU.add),
                                 reads=[s_], writes=[st_])
                            P.op("act", lambda e: e.activation(out=st_[:], in_=st_[:], func=AF.Ln), reads=[st_], writes=[st_])
                            P.op("act", lambda e: e.activation(out=s_[:], in_=st_[:], func=AF.Exp, scale=-0.5), reads=[st_], writes=[s_])
                            P.op("dve", lambda e: e.tensor_tensor(out=tm4[:], in0=o_p[:], in1=s_[:].unsqueeze(2).to_broadcast([128, 4, 128]), op=ALU.mult),
                                 reads=[o_p, s_], writes=[tm4])
                            P.op("dve", lambda e: e.tensor_tensor(out=yb_[:], in0=tm4[:].rearrange("p h v -> p (h v)"), in1=gsn[:, sub, :], op=ALU.mult),
                                 reads=[tm4, gsn], writes=[yb_])
                        for h in range(4):
                            P.op("pe", lambda e: e.matmul(pS[:, h, :], lhsT=kdT[:, sub, h, :], rhs=vtm[:, sub, h * 128:(h + 1) * 128], start=True, stop=True),
                                 reads=[kdT, vtm], writes=[pS])
                        P.op("dve", lambda e: e.tensor_tensor(out=S32[:], in0=S32[:], in1=dec_all[:, :, tile:tile + 1].to_broadcast([128, 4, 128]), op=ALU.mult),
                             reads=[S32, dec_all], writes=[S32])
                        P.op("dve", lambda e: e.tensor_tensor(out=S32[:], in0=S32[:], in1=pS[:], op=ALU.add), reads=[S32, pS], writes=[S32])
                        P.op("act", lambda e: e.copy(out=Sbf4[:], in_=S32[:]), reads=[S32], writes=[Sbf4])
                        if own:
                            for h in range(4):
                                P.op("pe", lambda e: e.transpose(out=pTk16[:, h, :], in_=yb_[:, h * 128:(h + 1) * 128], identity=ident[:]),
                                     reads=[yb_, ident], writes=[pTk16])
                            P.op("act", lambda e: e.copy(out=ybTs[bi % 2][:, :, sub * 128:(sub + 1) * 128], in_=pTk16[:]), reads=[pTk16], writes=[ybTs[bi % 2]])
                    if own:
                        for h in range(4):
                            P.op("sp", lambda e: e.dma_start(out=YBT[h, :, (bi - 8) * 512:(bi - 7) * 512], in_=ybTs[bi % 2][:, h, :]),
                                 reads=[ybTs[bi % 2]], dma=True)
                P.barrier()

        if upto >= 5:
            with ExitStack() as ph:
                wga = sb(ph, "wga", [128, 8, D], BF16)
                wgb = sb(ph, "wgb", [128, 8, D], BF16)
                wa = sb(ph, "wa", [128, 4, D], BF16)
                wb = sb(ph, "wb", [128, 4, D], BF16)
                wo = sb(ph, "wo", [128, 8, D], BF16)
                rw = sb(ph, "rw", [128, 8, NE], BF16)
                rbb = sb(ph, "rbb", [128, NE], F32)
                for w_, c0 in ((wga, 3584), (wgb, 4608)):
                    for kh in range(4):
                        P.op("pool", lambda e: e.dma_start(out=w_[:, kh * 2:(kh + 1) * 2, :], in_=w_in_v[:, kh * 2:(kh + 1) * 2, c0:c0 + D]),
                             writes=[w_], dma=True)
                P.op("pool", lambda e: e.dma_start(out=wa[:], in_=w_ba.rearrange("(h p) n -> p h n", p=128)), writes=[wa], dma=True)
                P.op("pool", lambda e: e.dma_start(out=wb[:], in_=w_bb.rearrange("(h p) n -> p h n", p=128)), writes=[wb], dma=True)
                for kh in range(4):
                    P.op("pool", lambda e: e.dma_start(out=wo[:, kh * 2:(kh + 1) * 2, :], in_=w_out.rearrange("(kc p) n -> p kc n", p=128)[:, kh * 2:(kh + 1) * 2, :]),
                         writes=[wo], dma=True)
                P.op("pool", lambda e: e.dma_start(out=rw[:], in_=router_w.rearrange("(kc p) n -> p kc n", p=128)), writes=[rw], dma=True)
                P.op("sp", lambda e: e.dma_start(out=rbb[:], in_=router_b[0:1, :].partition_broadcast(128)), writes=[rbb], dma=True)
                hTb = [sb(ph, "hTb5_%d" % i, [128, 4, 8, 128], BF16) for i in range(2)]
                yaTb = [sb(ph, "yaTb%d" % i, [128, 4, 512], BF16) for i in range(2)]
                ybTb = [sb(ph, "ybTb%d" % i, [128, 4, 512], BF16) for i in range(2)]
                sga = [sb(ph, "sga%d" % i, [128, 512], F32) for i in range(2)]
                sgb = [sb(ph, "sgb%d" % i, [128, 512], F32) for i in range(2)]
                ta = [sb(ph, "ta%d" % i, [128, 512], F32) for i in range(2)]
                tb = [sb(ph, "tb%d" % i, [128, 512], F32) for i in range(2)]
                mT = [sb(ph, "mT%d" % i, [128, 8, 512], BF16) for i in range(2)]
                xt = [sb(ph, "xt5_%d" % i, [128, D], F32) for i in range(2)]
                tmp = sb(ph, "tmp5", [128, D], F32)
                x1 = [sb(ph, "x1_%d" % i, [128, D], F32) for i in range(2)]
                tmp2 = sb(ph, "tmp5b", [128, D], F32)
                junk = tmp2
                h2 = [sb(ph, "h2_%d" % i, [128, D], BF16) for i in range(2)]
                h2Tt = [sb(ph, "h2Tt%d" % i, [128, 8, 128], BF16) for i in range(2)]
                s2 = [sb(ph, "s5a%d" % i, [128, 2], F32) for i in range(2)]
                s3 = [sb(ph, "s5b%d" % i, [128, 1], F32) for i in range(2)]
                st5 = [sb(ph, "st5_%d" % i, [128, 1], F32) for i in range(2)]
                lg = [sb(ph, "lg%d" % i, [128, NE], F32) for i in range(2)]
                t8 = [sb(ph, "t8_%d" % i, [128, 8], F32) for i in range(2)]
                msk = [sb(ph, "msk%d" % i, [128, NE], F32) for i in range(2)]
                ex = [sb(ph, "ex%d" % i, [128, NE], F32) for i in range(2)]
                gs = [sb(ph, "gs%d" % i, [128, 2], F32) for i in range(2)]
                pG = [ps(ph, "pG%d" % i, [128, 512], F32) for i in range(2)]
                pP = [ps(ph, "pP%d" % i, [128, 512], F32) for i in range(2)]
                pY = [ps(ph, "pY%d" % i, [128, 512], F32) for i in range(2)]
                pT5 = ps(ph, "pT5", [128, 8, 128], BF16)
                pR = ps(ph, "pR", [128, 512], F32)

                def ld5(j):
                    P.op("sp", lambda e: e.dma_start(out=hTb[j % 2][:], in_=HT[32 + j * 4:32 + (j + 1) * 4].rearrange("t p k j -> p t k j")),
                         writes=[hTb[j % 2]], dma=True)
                    P.op("sp", lambda e: e.dma_start(out=yaTb[j % 2][:], in_=YAT[:, :, j * 512:(j + 1) * 512].rearrange("h p t -> p h t")),
                         writes=[yaTb[j % 2]], dma=True)
                    P.op("sp", lambda e: e.dma_start(out=ybTb[j % 2][:], in_=YBT[:, :, j * 512:(j + 1) * 512].rearrange("h p t -> p h t")),
                         writes=[ybTb[j % 2]], dma=True)

                ld5(0)
                cntb = [0, 0]

                def gate_part(j, dcs):
                    hb_ = hTb[j % 2]
                    ya_ = yaTb[j % 2]
                    yb_ = ybTb[j % 2]
                    m_ = mT[j % 2]
                    cnt = cntb[0]
                    for dc in dcs:
                        for (wg_, wbr_, y_, sg_, t_, tag) in ((wga, wa, ya_, sga, ta, 0), (wgb, wb, yb_, sgb, tb, 1)):
                            g_p = pG[cnt % 2]
                            p_p = pP[cnt % 2]
                            s_ = sg_[dc % 2]
                            u_ = t_[dc % 2]
                            cnt += 1
                            for kc in range(8):
                                P.op("pe", lambda e: e.matmul(g_p[:].rearrange("p (t j) -> p t j", j=128), lhsT=wg_[:, kc, dc * 128:(dc + 1) * 128],
                                                              rhs=hb_[:, :, kc, :], start=(kc == 0), stop=(kc == 7)), reads=[wg_, hb_], writes=[g_p])
                            P.op("act", lambda e: e.activation(out=s_[:], in_=g_p[:], func=AF.Exp, scale=-1.0), reads=[g_p], writes=[s_])
                            P.op("act", lambda e: e.activation(out=s_[:], in_=s_[:], func=AF.Ln, bias=1.0, scale=1.0), reads=[s_], writes=[s_])
                            P.op("act", lambda e: e.activation(out=s_[:], in_=s_[:], func=AF.Exp, scale=-1.0), reads=[s_], writes=[s_])
                            for h in range(4):
                                P.op("pe", lambda e: e.matmul(p_p[:], lhsT=wbr_[:, h, dc * 128:(dc + 1) * 128], rhs=y_[:, h, :],
                                                              start=(h == 0), stop=(h == 3)), reads=[wbr_, y_], writes=[p_p])
                            P.op("dve", lambda e: e.tensor_tensor(out=u_[:], in0=p_p[:], in1=s_[:], op=ALU.mult), reads=[p_p, s_], writes=[u_])
                        P.op("dve", lambda e: e.tensor_tensor(out=m_[:, dc, :], in0=ta[dc % 2][:], in1=tb[dc % 2][:], op=ALU.add),
                             reads=[ta[dc % 2], tb[dc % 2]], writes=[m_])
                    cntb[0] = cnt

                def sub_A(j, sub):
                    m_ = mT[j % 2]
                    tl = j * 4 + sub
                    if True:
                        x_ = xt[tl % 2]
                        x1_ = x1[tl % 2]
                        h2_ = h2[tl % 2]
                        hT_ = h2Tt[tl % 2]
                        sa = s2[tl % 2]
                        sb_ = s3[tl % 2]
                        st_ = st5[tl % 2]
                        lg_ = lg[tl % 2]
                        t8_ = t8[tl % 2]
                        mk_ = msk[tl % 2]
                        ex_ = ex[tl % 2]
                        gs_ = gs[tl % 2]
                        P.op("sp", lambda e: e.dma_start(out=x_[:], in_=xall[TOWN + tl * 128:TOWN + (tl + 1) * 128, :]), writes=[x_], dma=True)
                        for half in range(2):
                            for kc in range(8):
                                P.op("pe", lambda e: e.matmul(pY[half][:], lhsT=m_[:, kc, sub * 128:(sub + 1) * 128], rhs=wo[:, kc, half * 512:(half + 1) * 512],
                                                              start=(kc == 0), stop=(kc == 7)), reads=[m_, wo], writes=[pY[half]])
                            P.op("act", lambda e: e.activation(out=junk[:, half * 512:(half + 1) * 512], in_=pY[half][:], func=AF.Square,
                                                               accum_out=sa[:, half:half + 1]), reads=[pY[half]], writes=[junk, sa])
                        P.op("dve", lambda e: e.tensor_tensor(out=sb_[:], in0=sa[:, 0:1], in1=sa[:, 1:2], op=ALU.add), reads=[sa], writes=[sb_])
                        rstd_from_ss(sb_, D, st_)
                        for half in range(2):
                            P.op("dve", lambda e: e.scalar_tensor_tensor(out=tmp[:, half * 512:(half + 1) * 512], in0=pY[half][:], scalar=sb_[:, 0:1],
                                                                         in1=modb[:, 2 * D + half * 512:2 * D + (half + 1) * 512], op0=ALU.mult, op1=ALU.mult),
                                 reads=[pY[half], sb_, modb], writes=[tmp])
                        P.op("dve", lambda e: e.tensor_tensor(out=x1_[:], in0=tmp[:], in1=x_[:], op=ALU.add), reads=[tmp, x_], writes=[x1_])
                        P.op("sp", lambda e: e.dma_start(out=X1[tl * 128:(tl + 1) * 128, :], in_=x1_[:]), reads=[x1_], dma=True)
                        P.op("act", lambda e: e.activation(out=junk[:], in_=x1_[:], func=AF.Square, accum_out=sb_[:, 0:1]), reads=[x1_], writes=[junk, sb_])
                        rstd_from_ss(sb_, D, st_)
                        P.op("dve", lambda e: e.scalar_tensor_tensor(out=tmp2[:], in0=x1_[:], scalar=sb_[:, 0:1], in1=A2(), op0=ALU.mult, op1=ALU.mult),
                             reads=[x1_, sb_, modb], writes=[tmp2])
                        P.op("dve", lambda e: e.tensor_tensor(out=h2_[:], in0=tmp2[:], in1=B2(), op=ALU.add), reads=[tmp2, modb], writes=[h2_])

                def sub_B(j, sub):
                    tl = j * 4 + sub
                    if True:
                        x_ = xt[tl % 2]
                        x1_ = x1[tl % 2]
                        h2_ = h2[tl % 2]
                        hT_ = h2Tt[tl % 2]
                        sa = s2[tl % 2]
                        sb_ = s3[tl % 2]
                        st_ = st5[tl % 2]
                        lg_ = lg[tl % 2]
                        t8_ = t8[tl % 2]
                        mk_ = msk[tl % 2]
                        ex_ = ex[tl % 2]
                        gs_ = gs[tl % 2]
                        for kc in range(8):
                            P.op("pe", lambda e: e.transpose(out=pT5[:, kc, :], in_=h2_[:, kc * 128:(kc + 1) * 128], identity=ident[:]),
                                 reads=[h2_, ident], writes=[pT5])
                        P.op("act", lambda e: e.copy(out=hT_[:], in_=pT5[:]), reads=[pT5], writes=[hT_])
                        P.op("sp", lambda e: e.dma_start(out=H2TM[tl * 128:(tl + 1) * 128, :], in_=h2_[:]), reads=[h2_], dma=True)
                        for kc in range(8):
                            P.op("pe", lambda e: e.matmul(pR[:, 0:NE], lhsT=hT_[:, kc, :], rhs=rw[:, kc, :], start=(kc == 0), stop=(kc == 7)),
                                 reads=[hT_, rw], writes=[pR])
                        P.op("dve", lambda e: e.tensor_tensor(out=lg_[:], in0=pR[:, 0:NE], in1=rbb[:], op=ALU.add), reads=[pR, rbb], writes=[lg_])
                        P.op("dve", lambda e: e.max(out=t8_[:], in_=lg_[:]), reads=[lg_], writes=[t8_])
                        P.op("dve", lambda e: e.tensor_scalar(out=mk_[:], in0=lg_[:], scalar1=t8_[:, 3:4], scalar2=None, op0=ALU.is_ge), reads=[lg_, t8_], writes=[mk_])
                        P.op("dve", lambda e: e.tensor_scalar(out=gs_[:, 0:1], in0=t8_[:, 0:1], scalar1=-1.0, scalar2=None, op0=ALU.mult), reads=[t8_], writes=[gs_])
                        P.op("act", lambda e: e.activation(out=ex_[:], in_=lg_[:], func=AF.Exp, bias=gs_[:, 0:1], scale=1.0), reads=[lg_, gs_], writes=[ex_])
                        P.op("dve", lambda e: e.tensor_tensor(out=ex_[:], in0=ex_[:], in1=mk_[:], op=ALU.mult), reads=[ex_, mk_], writes=[ex_])
                        P.op("dve", lambda e: e.reduce_sum(out=gs_[:, 1:2], in_=ex_[:], axis=mybir.AxisListType.X), reads=[ex_], writes=[gs_])
                        P.op("dve", lambda e: e.reciprocal(out=gs_[:, 1:2], in_=gs_[:, 1:2]), reads=[gs_], writes=[gs_])
                        P.op("dve", lambda e: e.tensor_scalar(out=Gall[:, tl, :], in0=ex_[:], scalar1=gs_[:, 1:2], scalar2=None, op0=ALU.mult),
                             reads=[ex_, gs_], writes=[Gall])

                gate_part(0, range(8))
                for j in range(8):
                    if j + 1 < 8:
                        ld5(j + 1)
                    for sub in range(4):
                        sub_A(j, sub)
                        if sub > 0:
                            sub_B(j, sub - 1)
                        if j + 1 < 8:
                            gate_part(j + 1, (2 * sub, 2 * sub + 1))
                    sub_B(j, 3)
                if dbg:
                    P.op("sp", lambda e: e.dma_start(out=GDBG[:, :, :], in_=Gall[:]), reads=[Gall], dma=True)
                P.barrier()

        if upto >= 5:
            P.op("dve", lambda e: e.tensor_copy(out=G2t[:], in_=G2()), reads=[modb], writes=[G2t])
            with ExitStack() as ph:
                maskf = sb(ph, "maskf", [128, 32, NE], F32)
                maskb = sb(ph, "maskb", [128, 32 * NE], BF16)
                Umat = sb(ph, "Umat", [128, 128], BF16)
                onesb = sb(ph, "onesb", [128, 128], BF16)
                cnt = sb(ph, "cnt", [128, 32, NE], F32)
                tot = sb(ph, "tot", [128, 32, NE], F32)
                base = sb(ph, "base", [128, 32, NE], F32)
                key = sb(ph, "key", [128, 32, NE], F32)
                ntot = sb(ph, "ntot", [128, NE], F32)
                nbi = sb(ph, "nbi", [128, NE], I32)
                nbf = sb(ph, "nbf", [128, NE], F32)
                sbe = sb(ph, "sbe", [128, NE], F32)
                starts = sb(ph, "starts", [128, NE], F32)
                t8r = [sb(ph, "t8r%d" % i, [128, 8], F32) for i in range(2)]
                eqr = [sb(ph, "eqr%d" % i, [128, NE], F32) for i in range(4)]
                dest4f = sb(ph, "dest4f", [128, 32 * 4], F32)
                jidx_i = sb(ph, "jidx_i", [128, NBLK], I32)
                jidx = sb(ph, "jidx", [128, NBLK], F32)
                pidx_i = sb(ph, "pidx_i", [128, 1], I32)
                pidx = sb(ph, "pidx", [128, 1], F32)
                cmp = sb(ph, "cmp", [128, NBLK, NE], F32)
                Ej = sb(ph, "Ej", [128, NBLK], F32)
                bw = sb(ph, "bw", [128, NBLK], F32)
                offwf = sb(ph, "offwf", [128, NBLK, 8], F32)
                offbf = sb(ph, "offbf", [128, NBLK], F32)
                pc = [ps(ph, "pc%d" % i, [128, 512], F32) for i in range(2)]
                ptt = [ps(ph, "ptt%d" % i, [128, 512], F32) for i in range(2)]
                P.op("dve", lambda e: e.tensor_scalar(out=maskf[:], in0=Gall[:], scalar1=0.0, scalar2=None, op0=ALU.is_gt), reads=[Gall], writes=[maskf])
                P.op("dve", lambda e: e.tensor_copy(out=maskb[:], in_=maskf[:].rearrange("p t e -> p (t e)")), reads=[maskf], writes=[maskb])
                P.op("pool", lambda e: e.memset(Umat[:], 1.0), writes=[Umat])
                P.op("pool", lambda e: e.affine_select(out=Umat[:], in_=Umat[:], pattern=[[1, 128]], compare_op=ALU.is_gt, fill=0.0, base=0,
                                                       channel_multiplier=-1), reads=[Umat], writes=[Umat])
                P.op("pool", lambda e: e.memset(onesb[:], 1.0), writes=[onesb])
                for half in range(2):
                    P.op("pe", lambda e: e.matmul(pc[half][:], lhsT=Umat[:], rhs=maskb[:, half * 512:(half + 1) * 512], start=True, stop=True),
                         reads=[Umat, maskb], writes=[pc[half]])
                    P.op("pe", lambda e: e.matmul(ptt[half][:], lhsT=onesb[:], rhs=maskb[:, half * 512:(half + 1) * 512], start=True, stop=True),
                         reads=[onesb, maskb], writes=[ptt[half]])
                    P.op("dve", lambda e: e.tensor_copy(out=cnt[:, half * 16:(half + 1) * 16, :], in_=pc[half][:].rearrange("p (t e) -> p t e", e=NE)),
                         reads=[pc[half]], writes=[cnt])
                    P.op("dve", lambda e: e.tensor_copy(out=tot[:, half * 16:(half + 1) * 16, :], in_=ptt[half][:].rearrange("p (t e) -> p t e", e=NE)),
                         reads=[ptt[half]], writes=[tot])
                P.op("dve", lambda e: e.memset(base[:, 0, :], 0.0), writes=[base])
                for t in range(1, 32):
                    P.op("dve", lambda e: e.tensor_tensor(out=base[:, t, :], in0=base[:, t - 1, :], in1=tot[:, t - 1, :], op=ALU.add), reads=[base, tot], writes=[base])
                P.op("dve", lambda e: e.tensor_tensor(out=ntot[:], in0=base[:, 31, :], in1=tot[:, 31, :], op=ALU.add), reads=[base, tot], writes=[ntot])
                P.op("dve", lambda e: e.tensor_scalar(out=ntot[:], in0=ntot[:], scalar1=float(BLK - 1), scalar2=None, op0=ALU.add), reads=[ntot], writes=[ntot])
                P.op("dve", lambda e: e.tensor_copy(out=nbi[:], in_=ntot[:]), reads=[ntot], writes=[nbi])
                P.op("dve", lambda e: e.tensor_scalar(out=nbi[:], in0=nbi[:], scalar1=9, scalar2=None, op0=ALU.arith_shift_right), reads=[nbi], writes=[nbi])
                P.op("dve", lambda e: e.tensor_copy(out=nbf[:], in_=nbi[:]), reads=[nbi], writes=[nbf])
                P.op("dve", lambda e: e.memset(sbe[:, 0:1], 0.0), writes=[sbe])
                for ex_i in range(1, NE):
                    P.op("dve", lambda e: e.tensor_tensor(out=sbe[:, ex_i:ex_i + 1], in0=sbe[:, ex_i - 1:ex_i], in1=nbf[:, ex_i - 1:ex_i], op=ALU.add),
                         reads=[sbe, nbf], writes=[sbe])
                P.op("dve", lambda e: e.tensor_scalar(out=starts[:], in0=sbe[:], scalar1=float(BLK), scalar2=None, op0=ALU.mult), reads=[sbe], writes=[starts])
                P.op("dve", lambda e: e.tensor_tensor(out=key[:], in0=cnt[:], in1=base[:], op=ALU.add), reads=[cnt, base], writes=[key])
                P.op("dve", lambda e: e.tensor_tensor(out=key[:], in0=key[:], in1=starts[:].unsqueeze(1).to_broadcast([128, 32, NE]), op=ALU.add),
                     reads=[key, starts], writes=[key])
                P.op("dve", lambda e: e.scalar_tensor_tensor(out=key[:], in0=key[:], scalar=1.0, in1=maskf[:], op0=ALU.add, op1=ALU.mult),
                     reads=[key, maskf], writes=[key])
                t8all = sb(ph, "t8all", [128, 32, 8], F32)
                eqb = sb(ph, "eqb", [128, 32, NE], F32)
                for t in range(32):
                    P.op("dve", lambda e: e.max(out=t8all[:, t, :], in_=key[:, t, :]), reads=[key], writes=[t8all])
                P.op("dve", lambda e: e.tensor_scalar(out=dest4f[:].rearrange("p (t k) -> p t k", k=4), in0=t8all[:, :, 0:4], scalar1=-1.0, scalar2=None, op0=ALU.add),
                     reads=[t8all], writes=[dest4f])
                for k in range(4):
                    P.op("dve", lambda e: e.tensor_tensor(out=eqb[:], in0=key[:], in1=t8all[:, :, k:k + 1].to_broadcast([128, 32, NE]), op=ALU.is_equal),
                         reads=[key, t8all], writes=[eqb])
                    P.op("dve", lambda e: e.tensor_tensor(out=eqb[:], in0=eqb[:], in1=Gall[:], op=ALU.mult), reads=[eqb, Gall], writes=[eqb])
                    P.op("dve", lambda e: e.reduce_sum(out=G4[:, :, k], in_=eqb[:], axis=mybir.AxisListType.X), reads=[eqb], writes=[G4])
                P.op("dve", lambda e: e.tensor_copy(out=dest4u[:], in_=dest4f[:]), reads=[dest4f], writes=[dest4u])
                P.op("pool", lambda e: e.iota(jidx_i[:], pattern=[[1, NBLK]], base=0, channel_multiplier=0), writes=[jidx_i])
                P.op("dve", lambda e: e.tensor_copy(out=jidx[:], in_=jidx_i[:]), reads=[jidx_i], writes=[jidx])
                P.op("pool", lambda e: e.iota(pidx_i[:], pattern=[[0, 1]], base=0, channel_multiplier=1), writes=[pidx_i])
                P.op("dve", lambda e: e.tensor_copy(out=pidx[:], in_=pidx_i[:]), reads=[pidx_i], writes=[pidx])
                P.op("dve", lambda e: e.tensor_tensor(out=cmp[:], in0=sbe[:].unsqueeze(1).to_broadcast([128, NBLK, NE]),
                                                      in1=jidx[:].unsqueeze(2).to_broadcast([128, NBLK, NE]), op=ALU.is_le), reads=[sbe, jidx], writes=[cmp])
                P.op("dve", lambda e: e.reduce_sum(out=Ej[:], in_=cmp[:], axis=mybir.AxisListType.X), reads=[cmp], writes=[Ej])
                P.op("dve", lambda e: e.tensor_scalar(out=Ej[:], in0=Ej[:], scalar1=-1.0, scalar2=None, op0=ALU.add), reads=[Ej], writes=[Ej])
                P.op("dve", lambda e: e.tensor_scalar(out=bw[:], in0=Ej[:], scalar1=float(D), scalar2=pidx[:, 0:1], op0=ALU.mult, op1=ALU.add),
                     reads=[Ej, pidx], writes=[bw])
                for kc in range(8):
                    P.op("dve", lambda e: e.tensor_scalar(out=offwf[:, :, kc], in0=bw[:], scalar1=float(kc * 128), scalar2=None, op0=ALU.add),
                         reads=[bw], writes=[offwf])
                P.op("dve", lambda e: e.tensor_copy(out=OFFW[:], in_=offwf[:]), reads=[offwf], writes=[OFFW])
                P.op("dve", lambda e: e.tensor_scalar(out=offbf[:], in0=Ej[:], scalar1=128.0, scalar2=pidx[:, 0:1], op0=ALU.mult, op1=ALU.add),
                     reads=[Ej, pidx], writes=[offbf])
                P.op("dve", lambda e: e.tensor_copy(out=OFFB[:], in_=offbf[:]), reads=[offbf], writes=[OFFB])
                P.op("dve", lambda e: e.tensor_copy(out=OFFD[:], in_=Ej[:]), reads=[Ej], writes=[OFFD])
                if dbg:
                    P.op("sp", lambda e: e.dma_start(out=RDBG[:, 0:128], in_=dest4f[:]), reads=[dest4f], dma=True)
                    P.op("sp", lambda e: e.dma_start(out=RDBG[:, 128:256], in_=G4[:].rearrange("p t k -> p (t k)")), reads=[G4], dma=True)
                    P.op("sp", lambda e: e.dma_start(out=RDBG[:, 256:256 + NBLK], in_=Ej[:]), reads=[Ej], dma=True)
                h2t = [sb(ph, "h2t%d" % i, [128, D], BF16) for i in range(3)]
                for t in range(32):
                    h_ = h2t[t % 3]
                    P.op("sp", lambda e: e.dma_start(out=h_[:], in_=H2TM[t * 128:(t + 1) * 128, :]), writes=[h_], dma=True)
                    for k in range(4):
                        P.op("pool", lambda e: e.indirect_dma_start(out=XS[:, :], out_offset=bass.IndirectOffsetOnAxis(ap=dest4u[:, t * 4 + k:t * 4 + k + 1], axis=0),
                                                                    in_=h_[:], in_offset=None), reads=[dest4u, h_], dma=True)
                P.barrier()

        mes.close()
        if upto >= 6:
            with ExitStack() as ph:
                wgb_ = [sb(ph, "wgub%d" % i, [128, 8, 2 * D], BF16) for i in range(2)]
                wdb_ = [sb(ph, "wdnb%d" % i, [128, 8, D], BF16) for i in range(2)]
                bgb_ = [sb(ph, "bgub%d" % i, [128, 16], F32) for i in range(2)]
                bdb_ = [sb(ph, "bdb%d" % i, [128, D], F32) for i in range(2)]
                xtok = [sb(ph, "xtok%d" % i, [128, 4, D], BF16) for i in range(1)]
                xT = [sb(ph, "xT%d" % i, [128, 8, 512], BF16) for i in range(2)]
                actT = [sb(ph, "actT%d" % i, [128, 8, 512], BF16) for i in range(2)]
                gbt = [sb(ph, "gbt%d" % i, [128, 512], F32) for i in range(2)]
                sig = [sb(ph, "sig%d" % i, [128, 512], F32) for i in range(2)]
                ubt = [sb(ph, "ubt%d" % i, [128, 512], F32) for i in range(2)]
                ys = [sb(ph, "ys%d" % i, [128, 512], F32) for i in range(12)]
                pTx = [ps(ph, "pTx%d" % i, [128, 8, 128], BF16) for i in range(2)]
                pg = [ps(ph, "pg%d" % i, [128, 512], F32) for i in range(2)]
                pu = [ps(ph, "pu%d" % i, [128, 512], F32) for i in range(2)]
                py = [ps(ph, "py%d" % i, [128, 512], F32) for i in range(2)]

                def ldblk(j):
                    b = j % 2
                    P.op("pool", lambda e: e.indirect_dma_start(out=wgb_[b][:].rearrange("p k n -> p (k n)"), out_offset=None,
                                                                in_=WGUB.rearrange("(r k) n -> r (k n)", k=8),
                                                                in_offset=bass.IndirectOffsetOnAxis(ap=OFFB[:, j:j + 1], axis=0)),
                         reads=[OFFB], writes=[wgb_[b]], dma=True)
                    P.op("pool", lambda e: e.indirect_dma_start(out=wdb_[b][:].rearrange("p k n -> p (k n)"), out_offset=None,
                                                                in_=WDNB.rearrange("(r k) n -> r (k n)", k=8),
                                                                in_offset=bass.IndirectOffsetOnAxis(ap=OFFB[:, j:j + 1], axis=0)),
                         reads=[OFFB], writes=[wdb_[b]], dma=True)
                    P.op("pool", lambda e: e.indirect_dma_start(out=bgb_[b][:], out_offset=None, in_=bgu_d[:, :],
                                                                in_offset=bass.IndirectOffsetOnAxis(ap=OFFB[:, j:j + 1], axis=0)),
                         reads=[OFFB], writes=[bgb_[b]], dma=True)
                    P.op("pool", lambda e: e.indirect_dma_start(out=bdb_[b][:], out_offset=None, in_=b_dn[:, :],
                                                                in_offset=bass.IndirectOffsetOnAxis(ap=OFFD[:, j:j + 1], axis=0)),
                         reads=[OFFD], writes=[bdb_[b]], dma=True)

                def ldx(j):
                    P.op("sp", lambda e: e.dma_start(out=xtok[0][:], in_=XS[j * BLK:(j + 1) * BLK, :].rearrange("(s p) d -> p s d", p=128)),
                         writes=[xtok[0]], dma=True)

                ldblk(0)
                ldx(0)
                fcn = 0
                yc = 0
                tcx = [0]

                def xpose(jj):
                    xk_ = xtok[0]
                    for sub in range(4):
                        p_ = pTx[tcx[0] % 2]
                        tcx[0] += 1
                        for kc in range(8):
                            P.op("pe", lambda e: e.transpose(out=p_[:, kc, :], in_=xk_[:, sub, kc * 128:(kc + 1) * 128], identity=ident[:]),
                                 reads=[xk_, ident], writes=[p_])
                        P.op("act", lambda e: e.copy(out=xT[jj % 2][:, :, sub * 128:(sub + 1) * 128], in_=p_[:]), reads=[p_], writes=[xT[jj % 2]])
                    if jj + 1 < NBLK:
                        ldx(jj + 1)
                for j in range(NBLK):
                    if j + 1 < NBLK:
                        ldblk(j + 1)
                    b = j % 2
                    wg_ = wgb_[b]
                    wd_ = wdb_[b]
                    bg_ = bgb_[b]
                    bd_ = bdb_[b]
                    xT_ = xT[b]
                    a_ = actT[b]
                    if j == 0:
                        xpose(0)
                    for fc in range(8):
                        g_p = pg[fcn % 2]
                        u_p = pu[fcn % 2]
                        gb_ = gbt[fcn % 2]
                        sg_ = sig[fcn % 2]
                        ub_ = ubt[fcn % 2]
                        fcn += 1
                        for kc in range(8):
                            P.op("pe", lambda e: e.matmul(g_p[:], lhsT=wg_[:, kc, fc * 128:(fc + 1) * 128], rhs=xT_[:, kc, :], start=(kc == 0), stop=(kc == 7)),
                                 reads=[wg_, xT_], writes=[g_p])
                        for kc in range(8):
                            P.op("pe", lambda e: e.matmul(u_p[:], lhsT=wg_[:, kc, D + fc * 128:D + (fc + 1) * 128], rhs=xT_[:, kc, :], start=(kc == 0), stop=(kc == 7)),
                                 reads=[wg_, xT_], writes=[u_p])
                        P.op("act", lambda e: e.activation(out=ub_[:], in_=u_p[:], func=AF.Identity, bias=bg_[:, 8 + fc:9 + fc], scale=1.0),
                             reads=[u_p, bg_], writes=[ub_])
                        P.op("dve", lambda e: e.tensor_scalar(out=gb_[:], in0=g_p[:], scalar1=bg_[:, fc:fc + 1], scalar2=7.0, op0=ALU.add, op1=ALU.min),
                             reads=[g_p, bg_], writes=[gb_])
                        P.op("act", lambda e: e.activation(out=sg_[:], in_=gb_[:], func=AF.Sigmoid, scale=1.702), reads=[gb_], writes=[sg_])
                        P.op("dve", lambda e: e.tensor_scalar(out=ub_[:], in0=ub_[:], scalar1=7.0, scalar2=-7.0, op0=ALU.min, op1=ALU.max), reads=[ub_], writes=[ub_])
                        P.op("dve", lambda e: e.tensor_tensor(out=gb_[:], in0=gb_[:], in1=sg_[:], op=ALU.mult), reads=[gb_, sg_], writes=[gb_])
                        P.op("dve", lambda e: e.scalar_tensor_tensor(out=a_[:, fc, :], in0=ub_[:], scalar=1.0, in1=gb_[:], op0=ALU.add, op1=ALU.mult),
                             reads=[ub_, gb_], writes=[a_])
                    if j + 1 < NBLK:
                        xpose(j + 1)
                    for sub in range(4):
                        for half in range(2):
                            y_p = py[yc % 2]
                            y_ = ys[yc % 12]
                            yc += 1
                            for fc in range(8):
                                P.op("pe", lambda e: e.matmul(y_p[:], lhsT=a_[:, fc, sub * 128:(sub + 1) * 128], rhs=wd_[:, fc, half * 512:(half + 1) * 512],
                                                              start=(fc == 0), stop=(fc == 7)), reads=[a_, wd_], writes=[y_p])
                            P.op("dve", lambda e: e.tensor_tensor(out=y_[:], in0=y_p[:], in1=bd_[:, half * 512:(half + 1) * 512], op=ALU.add),
                                 reads=[y_p, bd_], writes=[y_])
                            P.op("sp", lambda e: e.dma_start(out=YS[j * BLK + sub * 128:j * BLK + (sub + 1) * 128, half * 512:(half + 1) * 512], in_=y_[:]),
                                 reads=[y_], dma=True)
                P.barrier()

        if upto >= 6:
            with ExitStack() as ph:
                yk = [[sb(ph, "yk%d_%d" % (i, k), [128, D], F32) for k in range(4)] for i in range(2)]
                xt = [sb(ph, "xt7_%d" % i, [128, D], F32) for i in range(2)]
                acc = [sb(ph, "acc7_%d" % i, [128, D], F32) for i in range(2)]
                ot = [sb(ph, "ot7_%d" % i, [128, D], F32) for i in range(2)]
                s7 = [sb(ph, "s7_%d" % i, [128, 1], F32) for i in range(2)]
                st7 = [sb(ph, "st7_%d" % i, [128, 1], F32) for i in range(2)]

                def ld7(t):
                    for k in range(4):
                        P.op("pool", lambda e: e.indirect_dma_start(out=yk[t % 2][k][:], out_offset=None, in_=YS[:, :],
                                                                    in_offset=bass.IndirectOffsetOnAxis(ap=dest4u[:, t * 4 + k:t * 4 + k + 1], axis=0)),
                             reads=[dest4u], writes=[yk[t % 2][k]], dma=True)
                    P.op("sp", lambda e: e.dma_start(out=xt[t % 2][:], in_=X1[t * 128:(t + 1) * 128, :]), writes=[xt[t % 2]], dma=True)

                ld7(0)
                for t in range(32):
                    if t + 1 < 32:
                        ld7(t + 1)
                    y4 = yk[t % 2]
                    a_ = acc[t % 2]
                    o_ = ot[t % 2]
                    x_ = xt[t % 2]
                    s_ = s7[t % 2]
                    st_ = st7[t % 2]
                    P.op("dve", lambda e: e.tensor_scalar(out=a_[:], in0=y4[0][:], scalar1=G4[:, t, 0:1], scalar2=None, op0=ALU.mult), reads=[y4[0], G4], writes=[a_])
                    for k in range(1, 4):
                        P.op("dve", lambda e: e.scalar_tensor_tensor(out=a_[:], in0=y4[k][:], scalar=G4[:, t, k:k + 1], in1=a_[:], op0=ALU.mult, op1=ALU.add),
                             reads=[y4[k], G4, a_], writes=[a_])
                    P.op("act", lambda e: e.activation(out=o_[:], in_=a_[:], func=AF.Square, accum_out=s_[:, 0:1]), reads=[a_], writes=[o_, s_])
                    rstd_from_ss(s_, D, st_)
                    P.op("dve", lambda e: e.scalar_tensor_tensor(out=o_[:], in0=a_[:], scalar=s_[:, 0:1], in1=G2t[:], op0=ALU.mult, op1=ALU.mult),
                         reads=[a_, s_, G2t], writes=[o_])
                    P.op("dve", lambda e: e.tensor_tensor(out=o_[:], in0=o_[:], in1=x_[:], op=ALU.add), reads=[o_, x_], writes=[o_])
                    P.op("sp", lambda e: e.dma_start(out=y_out[t * 128:(t + 1) * 128, :], in_=o_[:]), reads=[o_], dma=True)
                P.barrier()
        P.barrier()
        print("ops", P.nops, "waits", P.nwaits, flush=True)
    return nc


def _host_inputs(inputs):
    x = np.asarray(inputs["x"], np.float32)
    c = np.asarray(inputs["c"], np.float32)
    inv = (10000.0 ** (-np.arange(0, 64, 2, dtype=np.float32) / np.float32(64))).astype(np.float32)
    shared = {
        "w_mod": np.ascontiguousarray(inputs["w_mod"][0], np.float32),
        "b_mod": np.ascontiguousarray(inputs["b_mod"][0:1], np.float32),
        "norm_pre_mix": np.ascontiguousarray(inputs["norm_pre_mix"][0:1], np.float32),
        "norm_post_mix": np.ascontiguousarray(inputs["norm_post_mix"][0:1], np.float32),
        "w_in": np.ascontiguousarray(inputs["w_in"][0], np.float32),
        "da_lambda_q1": np.ascontiguousarray(inputs["da_lambda_q1"][0:1], np.float32),
        "da_lambda_k1": np.ascontiguousarray(inputs["da_lambda_k1"][0:1], np.float32),
        "da_lambda_q2": np.ascontiguousarray(inputs["da_lambda_q2"][0:1], np.float32),
        "da_lambda_k2": np.ascontiguousarray(inputs["da_lambda_k2"][0:1], np.float32),
        "da_subln": np.ascontiguousarray(inputs["da_subln"][0:1], np.float32),
        "subln_col": np.ascontiguousarray(np.asarray(inputs["da_subln"], np.float32)[0].reshape(128, 1)),
        "lbl": np.ascontiguousarray(np.asarray(inputs["hg_lb_logits"], np.float32).reshape(2, 4, 128).transpose(2, 0, 1)),
        "hg_norm": np.ascontiguousarray(inputs["hg_norm"][0:1], np.float32),
        "w_branch_a": np.ascontiguousarray(inputs["w_branch_a"][0], np.float32),
        "w_branch_b": np.ascontiguousarray(inputs["w_branch_b"][0], np.float32),
        "w_out": np.ascontiguousarray(inputs["w_out"][0], np.float32),
        "norm_pre_ffn": np.ascontiguousarray(inputs["norm_pre_ffn"][0:1], np.float32),
        "norm_post_ffn": np.ascontiguousarray(inputs["norm_post_ffn"][0:1], np.float32),
        "router_w": np.ascontiguousarray(inputs["router_w"][0], np.float32),
        "router_b": np.ascontiguousarray(inputs["router_b"][0:1], np.float32),
        "w_gate_up": np.ascontiguousarray(np.asarray(inputs["w_gate_up"][0], np.float32).reshape(NE * D, 2 * D)),
        "bgu": np.ascontiguousarray(np.asarray(inputs["b_gate_up"][0], np.float32).reshape(NE, 16, 128).transpose(0, 2, 1).reshape(NE * 128, 16)),
        "w_down": np.ascontiguousarray(np.asarray(inputs["w_down"][0], np.float32).reshape(NE * D, D)),
        "b_down": np.ascontiguousarray(inputs["b_down"][0], np.float32),
    }
    in_maps = []
    p = np.arange(128)
    sign = np.where((p % 64) < 32, -1.0, 1.0).astype(np.float32)[:, None]
    for core in range(8):
        b, hf = core // 2, core % 2
        if hf == 1:
            xall = x[b]
            pos = np.arange(SEQ, dtype=np.float32)
        else:
            xall = np.concatenate([x[b, :TOWN], x[b, :TOWN]], axis=0)
            pos = np.concatenate([np.arange(TOWN), np.arange(TOWN)]).astype(np.float32)
        ang = (pos[None, :] * inv[p % 32][:, None]).astype(np.float32)
        m = dict(shared)
        m["xall"] = np.ascontiguousarray(xall)
        m["cosT"] = np.ascontiguousarray(np.cos(ang).astype(np.float32))
        m["sinT"] = np.ascontiguousarray((np.sin(ang) * sign).astype(np.float32))
        m["flag"] = np.full((128, 1), float(hf), np.float32)
        m["c2"] = np.ascontiguousarray(c[b].reshape(8, 128).T)
        in_maps.append(m)
    return in_maps


_NC_CACHE = {}


def kernel(**inputs):
    in_maps = _host_inputs(inputs)
    if "nc" not in _NC_CACHE:
        _NC_CACHE["nc"] = build_nc()
    nc = _NC_CACHE["nc"]
    res = run_bass_kernel_spmd(nc, in_maps, core_ids=list(range(8)))
    out = np.empty((NB, SEQ, D), np.float32)
    for core in range(8):
        b, hf = core // 2, core % 2
        out[b, hf * TOWN:(hf + 1) * TOWN] = res.results[core]["y"]
    return out
```

```python
import numpy as np
from contextlib import ExitStack
import concourse.bass as bass
import concourse.mybir as mybir
from concourse.bass_utils import run_bass_kernel_spmd

F32 = mybir.dt.float32
BF16 = mybir.dt.bfloat16
U32 = mybir.dt.uint32
I32 = mybir.dt.int32
ALU = mybir.AluOpType
AF = mybir.ActivationFunctionType

D = 1024
SEQ = 8192
NB = 4
TOWN = 4096
NT = 64
NE = 32
BLK = 512
NBLK = 63
NSLOT = NBLK * BLK
EPS = 1e-6
IN_COLS = 5632


class Buf:
    __slots__ = ("writers", "readers")

    def __init__(self):
        self.writers = {}
        self.readers = {}


class T:
    def __init__(self, t):
        self.t = t
        self.b = Buf()

    def __getitem__(self, k):
        return self.t[k]


class Prog:
    def __init__(self, nc, es, n_dma_sems=32):
        self.nc = nc
        self.eng = {"pe": nc.tensor, "act": nc.scalar, "dve": nc.vector, "pool": nc.gpsimd, "sp": nc.sync}
        self.sem = {}
        self.cnt = {}
        for k in self.eng:
            self.sem[k] = es.enter_context(nc.semaphore("sem_" + k))
            self.cnt[k] = 0
        self.rings = {}
        for rn, n in (("main", n_dma_sems), ("pre", 8), ("sw", 24)):
            self.rings[rn] = {"sem": [es.enter_context(nc.semaphore("dsem_%s%d" % (rn, i))) for i in range(n)], "cnt": [0] * n, "next": 0}
        self.seen = {k: {} for k in self.eng}
        self.nwaits = 0
        self.nops = 0

    def _wait(self, eng, tok):
        sem, val = tok
        key = id(sem)
        if self.seen[eng].get(key, 0) >= val:
            return
        self.eng[eng].wait_ge(sem, val)
        self.seen[eng][key] = val
        self.nwaits += 1

    def op(self, eng, fn, reads=(), writes=(), dma=False, ring="main"):
        pe_sem = self.sem["pe"]
        for t in reads:
            for tok in t.b.writers.values():
                if eng == "pe" and tok[0] is pe_sem:
                    continue
                self._wait(eng, tok)
        for t in writes:
            for tok in t.b.writers.values():
                if eng == "pe" and tok[0] is pe_sem:
                    continue
                self._wait(eng, tok)
            for tok in t.b.readers.values():
                if eng == "pe" and tok[0] is pe_sem:
                    continue
                self._wait(eng, tok)
        if dma:
            if eng == "pool" and ring == "main":
                ring = "sw"
            rg = self.rings[ring]
            i = rg["next"]
            rg["next"] = (i + 1) % len(rg["sem"])
            sem = rg["sem"][i]
            if rg["cnt"][i] > 0:
                self._wait(eng, (sem, rg["cnt"][i]))
            ins = fn(self.eng[eng])
            rg["cnt"][i] += 16
            ins.then_inc(sem, 16)
            tok = (sem, rg["cnt"][i])
        else:
            ins = fn(self.eng[eng])
            self.cnt[eng] += 1
            ins.then_inc(self.sem[eng], 1)
            tok = (self.sem[eng], self.cnt[eng])
        self.nops += 1
        k = id(tok[0])
        for t in reads:
            t.b.readers[k] = tok
        for t in writes:
            t.b.writers[k] = tok
            t.b.readers = {}
        return tok

    def barrier(self, engines=None):
        engines = engines or list(self.eng)
        for e in engines:
            for o in self.eng:
                if o != e and self.cnt[o] > 0:
                    self._wait(e, (self.sem[o], self.cnt[o]))
            for rg in self.rings.values():
                for i, s in enumerate(rg["sem"]):
                    if rg["cnt"][i] > 0:
                        self._wait(e, (s, rg["cnt"][i]))


def build_nc(upto=99, dbg=False):
    nc = bass.Bass("TRN2", target_bir_lowering=False)

    def din(name, shape, dt=F32):
        return nc.dram_tensor(name, list(shape), dt, kind="ExternalInput").ap()

    skind = "ExternalOutput" if dbg else "Internal"

    def dscr(name, shape, dt):
        return nc.dram_tensor(name, list(shape), dt, kind=skind).ap()

    xall = din("xall", [SEQ, D])
    cosT = din("cosT", [128, SEQ])
    sinT = din("sinT", [128, SEQ])
    flag_d = din("flag", [128, 1])
    c2_d = din("c2", [128, 8])
    w_mod = din("w_mod", [D, 6 * D])
    b_mod = din("b_mod", [1, 6 * D])
    n_pre_mix = din("norm_pre_mix", [1, D])
    n_post_mix = din("norm_post_mix", [1, D])
    w_in = din("w_in", [D, IN_COLS])
    lq1 = din("da_lambda_q1", [1, 64])
    lk1 = din("da_lambda_k1", [1, 64])
    lq2 = din("da_lambda_q2", [1, 64])
    lk2 = din("da_lambda_k2", [1, 64])
    da_subln = din("da_subln", [1, 128])
    subln_col = din("subln_col", [128, 1])
    lbl_d = din("lbl", [128, 2, 4])
    hg_norm = din("hg_norm", [1, 128])
    w_ba = din("w_branch_a", [512, D])
    w_bb = din("w_branch_b", [512, D])
    w_out = din("w_out", [D, D])
    n_pre_ffn = din("norm_pre_ffn", [1, D])
    n_post_ffn = din("norm_post_ffn", [1, D])
    router_w = din("router_w", [D, NE])
    router_b = din("router_b", [1, NE])
    if upto >= 6:
        w_gu = din("w_gate_up", [NE * D, 2 * D])
        bgu_d = din("bgu", [NE * 128, 16])
        w_dn = din("w_down", [NE * D, D])
        b_dn = din("b_down", [NE, D])
    y_out = nc.dram_tensor("y", [TOWN, D], F32, kind="ExternalOutput").ap()

    HT = dscr("HT", [NT, 128, 8, 128], BF16)
    KT = dscr("KT", [4, 128, SEQ], BF16)
    QT = dscr("QT", [4, 128, TOWN], BF16)
    VS = dscr("VS", [4, 128, NT, 130], BF16)
    YAT = dscr("YAT", [4, 128, TOWN], BF16)
    YBT = dscr("YBT", [4, 128, TOWN], BF16)
    X1 = dscr("X1", [TOWN, D], F32)
    H2TM = dscr("H2TM", [TOWN, D], BF16)
    XS = dscr("XS", [NSLOT, D], BF16)
    YS = dscr("YS", [NSLOT, D], F32)
    WGUB = nc.dram_tensor("WGUB", [NE * D, 2 * D], BF16).ap()
    WDNB = nc.dram_tensor("WDNB", [NE * D, D], BF16).ap()
    if dbg:
        GDBG = dscr("GDBG", [128, 32, NE], F32)
        RDBG = dscr("RDBG", [128, 32 * 4 + 32 * 4 + 64], F32)

    w_in_v = w_in.rearrange("(kc p) n -> p kc n", p=128)

    es = ExitStack()
    with es:
        P = Prog(nc, es)

        def sb(stack, name, shape, dt):
            return T(stack.enter_context(nc.sbuf_tensor(name, list(shape), dt)))

        def ps(stack, name, shape, dt):
            return T(stack.enter_context(nc.psum_tensor(name, list(shape), dt)))

        def rstd_from_ss(ss, n, tmp):
            P.op("dve", lambda e: e.tensor_scalar(out=tmp[:, 0:1], in0=ss[:, 0:1], scalar1=1.0 / n, scalar2=EPS,
                                                  op0=ALU.mult, op1=ALU.add), reads=[ss], writes=[tmp])
            P.op("act", lambda e: e.activation(out=tmp[:, 0:1], in_=tmp[:, 0:1], func=AF.Ln), reads=[tmp], writes=[tmp])
            P.op("act", lambda e: e.activation(out=ss[:, 0:1], in_=tmp[:, 0:1], func=AF.Exp, scale=-0.5), reads=[tmp], writes=[ss])

        ident = sb(es, "ident", [128, 128], BF16)
        P.op("pool", lambda e: e.memset(ident[:], 1.0), writes=[ident])
        P.op("pool", lambda e: e.affine_select(out=ident[:], in_=ident[:], pattern=[[-1, 128]], compare_op=ALU.is_equal,
                                               fill=0.0, base=0, channel_multiplier=1), reads=[ident], writes=[ident])
        flag = sb(es, "flag_t", [128, 1], F32)
        P.op("sp", lambda e: e.dma_start(out=flag[:], in_=flag_d[:, :]), writes=[flag], dma=True)
        nlam = sb(es, "nlam", [128, 1], F32)
        subln_b = sb(es, "subln_b", [128, 128], F32)
        hgn_b = sb(es, "hgn_b", [128, 128], F32)
        lb = sb(es, "lb", [128, 4], F32)
        oml = sb(es, "oml", [128, 4], F32)
        Gall = sb(es, "Gall", [128, 32, NE], F32)
        dest4u = sb(es, "dest4u", [128, 32 * 4], U32)
        G4 = sb(es, "G4", [128, 32, 4], F32)
        OFFW = sb(es, "OFFW", [128, NBLK, 8], U32)
        OFFB = sb(es, "OFFB", [128, NBLK], U32)
        OFFD = sb(es, "OFFD", [128, NBLK], U32)
        G2t = sb(es, "G2t", [128, D], F32)
        mes = ExitStack()
        modb = sb(mes, "modb", [128, 6 * D], F32)
        B1 = lambda: modb[:, 0:D]
        A1 = lambda: modb[:, D:2 * D]
        G1 = lambda: modb[:, 2 * D:3 * D]
        B2 = lambda: modb[:, 3 * D:4 * D]
        A2 = lambda: modb[:, 4 * D:5 * D]
        G2 = lambda: modb[:, 5 * D:6 * D]

        st0 = ExitStack()
        if True:
            ph = st0
            c2 = sb(ph, "c2t", [128, 8], F32)
            cb = sb(ph, "cb", [128, 8, 128], F32)
            ones1 = sb(ph, "ones1", [1, 128], F32)
            wm = [sb(ph, "wm%d" % i, [128, 8, 512], F32) for i in range(2)]
            bm = [sb(ph, "bm%d" % i, [1, 512], F32) for i in range(2)]
            pmod = [ps(ph, "pmod%d" % i, [128, 512], F32) for i in range(2)]
            nb4 = [sb(ph, "nb%d" % i, [128, D], F32) for i in range(4)]
            l4 = sb(ph, "l4", [128, 4, 64], F32)
            lt = sb(ph, "lt", [128, 2, 64], F32)
            ls = sb(ph, "ls", [128, 2], F32)
            lbl = sb(ph, "lblt", [128, 2, 4], F32)
            P.op("sp", lambda e: e.dma_start(out=c2[:], in_=c2_d[:, :]), writes=[c2], dma=True)
            P.op("act", lambda e: e.activation(out=c2[:], in_=c2[:], func=AF.Silu), reads=[c2], writes=[c2])
            P.op("dve", lambda e: e.tensor_copy(out=cb[:], in_=c2[:].unsqueeze(2).to_broadcast([128, 8, 128])), reads=[c2], writes=[cb])
            P.op("pool", lambda e: e.memset(ones1[:], 1.0), writes=[ones1])
            w_mod_v = w_mod.rearrange("(kc p) n -> p kc n", p=128)
            for ci in range(12):
                w_ = wm[ci % 2]
                b_ = bm[ci % 2]
                pm_ = pmod[ci % 2]
                P.op("sp", lambda e: e.dma_start(out=w_[:], in_=w_mod_v[:, :, ci * 512:(ci + 1) * 512]), writes=[w_], dma=True)
                P.op("sp", lambda e: e.dma_start(out=b_[:], in_=b_mod[0:1, ci * 512:(ci + 1) * 512]), writes=[b_], dma=True)
                for kc in range(8):
                    P.op("pe", lambda e: e.matmul(pm_[:], lhsT=cb[:, kc, :], rhs=w_[:, kc, :], start=(kc == 0), stop=False),
                         reads=[cb, w_], writes=[pm_])
                P.op("pe", lambda e: e.matmul(pm_[:], lhsT=ones1[0:1, :], rhs=b_[0:1, :], start=False, stop=True),
                     reads=[ones1, b_], writes=[pm_])
                P.op("dve", lambda e: e.tensor_copy(out=modb[:, ci * 512:(ci + 1) * 512], in_=pm_[:]), reads=[pm_], writes=[modb])
            for i, src in enumerate([n_pre_mix, n_post_mix, n_pre_ffn, n_post_ffn]):
                P.op("sp", lambda e: e.dma_start(out=nb4[i][:], in_=src[0:1, :].partition_broadcast(128)), writes=[nb4[i]], dma=True)
            P.op("dve", lambda e: e.scalar_tensor_tensor(out=A1(), in0=A1(), scalar=1.0, in1=nb4[0][:], op0=ALU.add, op1=ALU.mult),
                 reads=[modb, nb4[0]], writes=[modb])
            P.op("dve", lambda e: e.tensor_tensor(out=G1(), in0=G1(), in1=nb4[1][:], op=ALU.mult), reads=[modb, nb4[1]], writes=[modb])
            P.op("dve", lambda e: e.scalar_tensor_tensor(out=A2(), in0=A2(), scalar=1.0, in1=nb4[2][:], op0=ALU.add, op1=ALU.mult),
                 reads=[modb, nb4[2]], writes=[modb])
            P.op("dve", lambda e: e.tensor_tensor(out=G2(), in0=G2(), in1=nb4[3][:], op=ALU.mult), reads=[modb, nb4[3]], writes=[modb])
            for i, src in enumerate([lq1, lk1, lq2, lk2]):
                P.op("sp", lambda e: e.dma_start(out=l4[:, i, :], in_=src[0:1, :].partition_broadcast(128)), writes=[l4], dma=True)
            P.op("dve", lambda e: e.tensor_tensor(out=lt[:, 0, :], in0=l4[:, 0, :], in1=l4[:, 1, :], op=ALU.mult), reads=[l4], writes=[lt])
            P.op("dve", lambda e: e.tensor_tensor(out=lt[:, 1, :], in0=l4[:, 2, :], in1=l4[:, 3, :], op=ALU.mult), reads=[l4], writes=[lt])
            P.op("dve", lambda e: e.reduce_sum(out=ls[:], in_=lt[:], axis=mybir.AxisListType.X), reads=[lt], writes=[ls])
            P.op("act", lambda e: e.activation(out=ls[:], in_=ls[:], func=AF.Exp), reads=[ls], writes=[ls])
            P.op("dve", lambda e: e.tensor_tensor(out=nlam[:], in0=ls[:, 1:2], in1=ls[:, 0:1], op=ALU.subtract), reads=[ls], writes=[nlam])
            P.op("dve", lambda e: e.tensor_scalar(out=nlam[:], in0=nlam[:], scalar1=-0.2, scalar2=None, op0=ALU.add), reads=[nlam], writes=[nlam])
            P.op("sp", lambda e: e.dma_start(out=subln_b[:], in_=da_subln[0:1, :].partition_broadcast(128)), writes=[subln_b], dma=True)
            P.op("dve", lambda e: e.tensor_scalar(out=subln_b[:], in0=subln_b[:], scalar1=0.8, scalar2=None, op0=ALU.mult), reads=[subln_b], writes=[subln_b])
            P.op("sp", lambda e: e.dma_start(out=hgn_b[:], in_=hg_norm[0:1, :].partition_broadcast(128)), writes=[hgn_b], dma=True)
            P.op("sp", lambda e: e.dma_start(out=lbl[:], in_=lbl_d[:, :, :]), writes=[lbl], dma=True)
            P.op("dve", lambda e: e.tensor_tensor(out=lb[:], in0=lbl[:, 0, :], in1=lbl[:, 1, :], op=ALU.subtract), reads=[lbl], writes=[lb])
            P.op("act", lambda e: e.activation(out=lb[:], in_=lb[:], func=AF.Sigmoid), reads=[lb], writes=[lb])
            P.op("dve", lambda e: e.tensor_scalar(out=oml[:], in0=lb[:], scalar1=-1.0, scalar2=1.0, op0=ALU.mult, op1=ALU.add), reads=[lb], writes=[oml])

        if upto >= 1:
            with ExitStack() as ph:
                xt = [sb(ph, "xt%d" % i, [128, D], F32) for i in range(3)]
                junk = sb(ph, "junk", [128, D], F32)
                tmp = sb(ph, "tmp", [128, D], F32)
                hb = [sb(ph, "hb%d" % i, [128, D], BF16) for i in range(2)]
                hTt = [sb(ph, "hTt%d" % i, [128, 8, 128], BF16) for i in range(2)]
                ss = [sb(ph, "ss%d" % i, [128, 1], F32) for i in range(2)]
                st = [sb(ph, "st%d" % i, [128, 1], F32) for i in range(2)]
                pT = [ps(ph, "pT%d" % i, [128, 8, 128], BF16) for i in range(2)]

                def ld(i):
                    P.op("sp", lambda e: e.dma_start(out=xt[i % 3][:], in_=xall[i * 128:(i + 1) * 128, :]), writes=[xt[i % 3]], dma=True)

                ld(0)
                ld(1)

                def stA(i):
                    x_ = xt[i % 3]
                    s_ = ss[i % 2]
                    t_ = st[i % 2]
                    h_ = hb[i % 2]
                    P.op("act", lambda e: e.activation(out=junk[:], in_=x_[:], func=AF.Square, accum_out=s_[:, 0:1]), reads=[x_], writes=[junk, s_])
                    rstd_from_ss(s_, D, t_)
                    P.op("dve", lambda e: e.scalar_tensor_tensor(out=tmp[:], in0=x_[:], scalar=s_[:, 0:1], in1=A1(), op0=ALU.mult, op1=ALU.mult),
                         reads=[x_, s_, modb], writes=[tmp])
                    P.op("dve", lambda e: e.tensor_tensor(out=h_[:], in0=tmp[:], in1=B1(), op=ALU.add), reads=[tmp, modb], writes=[h_])

                stA(0)
                for i in range(NT):
                    if i + 2 < NT:
                        ld(i + 2)
                    if i + 1 < NT:
                        stA(i + 1)
                    h_ = hb[i % 2]
                    p_ = pT[i % 2]
                    o_ = hTt[i % 2]
                    for kc in range(8):
                        P.op("pe", lambda e: e.transpose(out=p_[:, kc, :], in_=h_[:, kc * 128:(kc + 1) * 128], identity=ident[:]),
                             reads=[h_, ident], writes=[p_])
                    P.op("act", lambda e: e.copy(out=o_[:], in_=p_[:]), reads=[p_], writes=[o_])
                    P.op("sp", lambda e: e.dma_start(out=HT[i], in_=o_[:]), reads=[o_], dma=True)
                P.barrier()

        st0.close()

        if upto >= 2:
            with ExitStack() as ph:
                wq = sb(ph, "wq", [128, 8, 512], BF16)
                wk = sb(ph, "wk", [128, 8, 512], BF16)
                wv = sb(ph, "wv", [128, 8, 512], BF16)
                wqs = sb(ph, "wqs", [128, 8, 512], BF16)
                wks = sb(ph, "wks", [128, 8, 512], BF16)
                for w_, c0 in ((wq, 0), (wk, 512), (wv, 1024)):
                    for kh in range(2):
                        P.op("pool", lambda e: e.dma_start(out=w_[:, kh * 4:(kh + 1) * 4, :], in_=w_in_v[:, kh * 4:(kh + 1) * 4, c0:c0 + 512]),
                             writes=[w_], dma=True)
                for w_, ws_ in ((wq, wqs), (wk, wks)):
                    src = w_[:].rearrange("p k (g two j) -> p k g two j", two=2, j=32)
                    dst = ws_[:].rearrange("p k (g two j) -> p k g two j", two=2, j=32)
                    for kc in range(8):
                        P.op("dve", lambda e: e.tensor_copy(out=dst[:, kc, :, 0, :], in_=src[:, kc, :, 1, :]), reads=[w_], writes=[ws_])
                        P.op("dve", lambda e: e.tensor_copy(out=dst[:, kc, :, 1, :], in_=src[:, kc, :, 0, :]), reads=[w_], writes=[ws_])
                hTb = [sb(ph, "hTb%d" % i, [128, 4, 8, 128], BF16) for i in range(2)]
                cs = [sb(ph, "cs%d" % i, [128, 512], F32) for i in range(2)]
                sn = [sb(ph, "sn%d" % i, [128, 512], F32) for i in range(2)]
                t1 = [sb(ph, "t1_%d" % i, [128, 512], F32) for i in range(2)]
                t2 = [sb(ph, "t2_%d" % i, [128, 512], F32) for i in range(2)]
                kts = [sb(ph, "kts%d" % i, [128, 4, 512], BF16) for i in range(2)]
                qts = [sb(ph, "qts%d" % i, [128, 4, 512], BF16) for i in range(2)]
                vst = [sb(ph, "vst%d" % i, [128, 4, 4, 130], BF16) for i in range(2)]
                pA = [ps(ph, "pA%d" % i, [128, 512], F32) for i in range(2)]
                pB = [ps(ph, "pB%d" % i, [128, 512], F32) for i in range(2)]
                pV = [ps(ph, "pV%d" % i, [128, 512], F32) for i in range(2)]
                for v_ in vst:
                    P.op("pool", lambda e: e.memset(v_[:], 0.0), writes=[v_])

                def ld2(bi):
                    P.op("sp", lambda e: e.dma_start(out=hTb[bi % 2][:], in_=HT[bi * 4:(bi + 1) * 4].rearrange("t p k j -> p t k j")),
                         writes=[hTb[bi % 2]], dma=True)
                    P.op("sp", lambda e: e.dma_start(out=cs[bi % 2][:], in_=cosT[:, bi * 512:(bi + 1) * 512]), writes=[cs[bi % 2]], dma=True)
                    P.op("sp", lambda e: e.dma_start(out=sn[bi % 2][:], in_=sinT[:, bi * 512:(bi + 1) * 512]), writes=[sn[bi % 2]], dma=True)

                ld2(0)
                cnt = 0
                for bi in range(16):
                    if bi + 1 < 16:
                        ld2(bi + 1)
                    own = bi >= 8
                    hb_ = hTb[bi % 2]
                    cs_ = cs[bi % 2]
                    sn_ = sn[bi % 2]
                    jobs = [(wk, wks, kts[bi % 2])]
                    if own:
                        jobs.append((wq, wqs, qts[bi % 2]))
                    for (w_, ws_, dst_) in jobs:
                        for h in range(4):
                            a_ = pA[cnt % 2]
                            b_ = pB[cnt % 2]
                            u1 = t1[cnt % 2]
                            u2 = t2[cnt % 2]
                            cnt += 1
                            for kc in range(8):
                                P.op("pe", lambda e: e.matmul(a_[:].rearrange("p (t j) -> p t j", j=128), lhsT=w_[:, kc, h * 128:(h + 1) * 128],
                                                              rhs=hb_[:, :, kc, :], start=(kc == 0), stop=(kc == 7)), reads=[w_, hb_], writes=[a_])
                            for kc in range(8):
                                P.op("pe", lambda e: e.matmul(b_[:].rearrange("p (t j) -> p t j", j=128), lhsT=ws_[:, kc, h * 128:(h + 1) * 128],
                                                              rhs=hb_[:, :, kc, :], start=(kc == 0), stop=(kc == 7)), reads=[ws_, hb_], writes=[b_])
                            P.op("dve", lambda e: e.tensor_tensor(out=u1[:], in0=a_[:], in1=cs_[:], op=ALU.mult), reads=[a_, cs_], writes=[u1])
                            P.op("dve", lambda e: e.tensor_tensor(out=u2[:], in0=b_[:], in1=sn_[:], op=ALU.mult), reads=[b_, sn_], writes=[u2])
                            P.op("dve", lambda e: e.tensor_tensor(out=dst_[:, h, :], in0=u1[:], in1=u2[:], op=ALU.add), reads=[u1, u2], writes=[dst_])
                    for h in range(4):
                        P.op("sp", lambda e: e.dma_start(out=KT[h, :, bi * 512:(bi + 1) * 512], in_=kts[bi % 2][:, h, :]), reads=[kts[bi % 2]], dma=True)
                        if own:
                            P.op("sp", lambda e: e.dma_start(out=QT[h, :, (bi - 8) * 512:(bi - 7) * 512], in_=qts[bi % 2][:, h, :]),
                                 reads=[qts[bi % 2]], dma=True)
                    v_ = vst[bi % 2]
                    for sub in range(4):
                        p_ = pV[sub % 2]
                        for kc in range(8):
                            P.op("pe", lambda e: e.matmul(p_[:], lhsT=hb_[:, sub, kc, :], rhs=wv[:, kc, :], start=(kc == 0), stop=(kc == 7)),
                                 reads=[hb_, wv], writes=[p_])
                        pv3 = p_[:].rearrange("p (h v) -> p h v", v=128)
                        if own:
                            P.op("act", lambda e: e.copy(out=v_[:, sub, :, 0:128], in_=pv3), reads=[p_], writes=[v_])
                        else:
                            P.op("dve", lambda e: e.tensor_scalar(out=v_[:, sub, :, 0:128], in0=pv3, scalar1=flag[:, 0:1], scalar2=None, op0=ALU.mult),
                                 reads=[p_, flag], writes=[v_])
                    if own:
                        P.op("pool", lambda e: e.memset(v_[:, :, :, 128:129], 1.0), writes=[v_])
                    else:
                        P.op("dve", lambda e: e.tensor_copy(out=v_[:, :, :, 128:129], in_=flag[:, 0:1].unsqueeze(1).unsqueeze(1).to_broadcast([128, 4, 4, 1])),
                             reads=[flag], writes=[v_])
                    for h in range(4):
                        P.op("sp", lambda e: e.dma_start(out=VS[h, :, bi * 4:(bi + 1) * 4, :], in_=v_[:, :, h, :]), reads=[v_], dma=True)
                P.barrier()

        if upto >= 3:
            with ExitStack() as ph:
                KTh = [sb(ph, "KTh%d" % i, [128, SEQ], BF16) for i in range(2)]
                QTh = [sb(ph, "QTh%d" % i, [128, 2, TOWN], BF16) for i in range(2)]
                for q_ in QTh:
                    P.op("dve", lambda e: e.memset(q_[64:128, 0, :], 0.0), writes=[q_])
                    P.op("dve", lambda e: e.memset(q_[0:64, 1, :], 0.0), writes=[q_])
                Vh = [sb(ph, "Vh%d" % i, [128, NT, 130], BF16) for i in range(2)]
                PT = [sb(ph, "PT%d" % i, [128, 512], BF16) for i in range(6)]
                Sps = [ps(ph, "Sps%d" % i, [128, 512], F32) for i in range(4)]
                OT1 = [ps(ph, "OT_%d" % m, [128, 512], F32) for m in range(2)]
                OT = [OT1, OT1]
                sumP = [ps(ph, "sumP_%d" % m, [128, 512], F32) for m in range(2)]
                pE = OT1[1]
                o0s = sb(ph, "o0s", [128, 512], F32)
                o1s = sb(ph, "o1s", [128, 512], F32)
                racc = [[sb(ph, "racc%d_%d" % (jp, m), [128, 512], F32) for m in range(2)] for jp in range(2)]
                ones32 = sb(ph, "ones32", [128, 128], F32)
                subcol = sb(ph, "subcol", [128, 1], F32)
                rr = [sb(ph, "rr%d" % i, [128, 512], F32) for i in range(2)]
                tq = sb(ph, "tq", [128, 512], F32)
                uq = sb(ph, "uq", [128, 512], F32)
                oq = sb(ph, "oq", [128, 512], F32)
                o2 = sb(ph, "o2", [128, 512], F32)
                rs = sb(ph, "rs", [128, 512], F32)
                yaTs = [sb(ph, "yaTs%d" % i, [128, 512], BF16) for i in range(2)]
                P.op("pool", lambda e: e.memset(ones32[:], 1.0), writes=[ones32])
                onesB = sb(ph, "onesB", [128, 128], BF16)
                onesF = sb(ph, "onesF", [128, 128], BF16)
                P.op("pool", lambda e: e.memset(onesB[:], 1.0), writes=[onesB])
                P.op("dve", lambda e: e.tensor_scalar(out=onesF[:], in0=ones32[:], scalar1=flag[:, 0:1], scalar2=None, op0=ALU.mult),
                     reads=[ones32, flag], writes=[onesF])
                P.op("sp", lambda e: e.dma_start(out=subcol[:], in_=subln_col[:, :]), writes=[subcol], dma=True)
                P.op("dve", lambda e: e.tensor_scalar(out=subcol[:], in0=subcol[:], scalar1=0.8, scalar2=None, op0=ALU.mult), reads=[subcol], writes=[subcol])

                def ld3(h):
                    P.op("sp", lambda e: e.dma_start(out=KTh[h % 2][:], in_=KT[h]), writes=[KTh[h % 2]], dma=True)
                    P.op("sp", lambda e: e.dma_start(out=QTh[h % 2][0:64, 0, :], in_=QT[h, 0:64, :]), writes=[QTh[h % 2]], dma=True)
                    P.op("sp", lambda e: e.dma_start(out=QTh[h % 2][64:128, 1, :], in_=QT[h, 64:128, :]), writes=[QTh[h % 2]], dma=True)
                    P.op("sp", lambda e: e.dma_start(out=Vh[h % 2][:], in_=VS[h]), writes=[Vh[h % 2]], dma=True)

                ld3(0)
                if upto >= 6:
                    stg = [sb(ph, "stg%d" % i, [128, 4, 2 * D], BF16) for i in range(2)]
                    ci = 0
                    WG4 = WGUB.rearrange("(e p k) n -> e p k n", p=128, k=8)
                    WD4 = WDNB.rearrange("(e p k) n -> e p k n", p=128, k=8)
                    for c in range(NE * D // 512):
                        st_ = stg[ci % 2]
                        ci += 1
                        P.op("pool", lambda e: e.dma_start(out=st_[:], in_=w_gu[c * 512:(c + 1) * 512, :].rearrange("(a p) n -> p a n", p=128)),
                             writes=[st_], dma=True, ring="pre")
                        P.op("pool", lambda e: e.dma_start(out=WG4[c // 2, :, (c % 2) * 4:(c % 2) * 4 + 4, :], in_=st_[:]),
                             reads=[st_], dma=True, ring="pre")
                    for c in range(NE):
                        st_ = stg[ci % 2]
                        ci += 1
                        v_ = st_[:].rearrange("p a (b n) -> p (a b) n", b=2)
                        P.op("pool", lambda e: e.dma_start(out=v_, in_=w_dn[c * 1024:(c + 1) * 1024, :].rearrange("(a p) n -> p a n", p=128)),
                             writes=[st_], dma=True, ring="pre")
                        P.op("pool", lambda e: e.dma_start(out=WD4[c, :, :, :], in_=v_), reads=[st_], dma=True, ring="pre")
                gs_ = 0
                for h in range(4):
                    if h + 1 < 4:
                        ld3(h + 1)
                    K_ = KTh[h % 2]
                    Q_ = QTh[h % 2]
                    V_ = Vh[h % 2]
                    steps = []
                    for j in range(8):
                        for m in range(2):
                            kts = list(range(32)) + [32 + t for t in range(4 * j + 4)]
                            for idx, kt in enumerate(kts):
                                steps.append((j, m, idx, kt, idx == len(kts) - 1))

                    def geom(st):
                        j, m, idx, kt, last = st
                        own_t = kt - 32
                        dsub = own_t - 4 * j if own_t >= 4 * j else -1
                        return dsub, max(dsub, 0) * 128

                    def front(si):
                        j, m, idx, kt, last = steps[si]
                        dsub, c0 = geom(steps[si])
                        sp_ = Sps[(gs_ + si) % 4]
                        pt_ = PT[(gs_ + si) % 6]
                        P.op("pe", lambda e: e.matmul(sp_[:, c0:512], lhsT=K_[:, kt * 128:(kt + 1) * 128],
                                                      rhs=Q_[:, m, j * 512 + c0:(j + 1) * 512], start=True, stop=True),
                             reads=[K_, Q_], writes=[sp_])
                        P.op("act", lambda e: e.activation(out=pt_[:, c0:512], in_=sp_[:, c0:512], func=AF.Exp, scale=0.125),
                             reads=[sp_], writes=[pt_])
                        if dsub >= 0:
                            P.op("act", lambda e: e.mul(out=pt_[64:128, c0:c0 + 64], in_=pt_[64:128, c0:c0 + 64], mul=0.0), reads=[pt_], writes=[pt_])

                    def back(si):
                        j, m, idx, kt, last = steps[si]
                        dsub, c0 = geom(steps[si])
                        pt_ = PT[(gs_ + si) % 6]
                        ra = racc[j % 2][m]
                        ot = OT[j % 2][m]
                        sp2 = sumP[m]
                        if idx % 2 == 0:
                            P.op("pe", lambda e: e.matmul(sp2[:, c0:512], lhsT=(onesF if kt < 32 else onesB)[:], rhs=pt_[:, c0:512], start=(idx == 0), stop=False),
                                 reads=[onesF, onesB, pt_], writes=[sp2])
                        elif idx == 1:
                            P.op("dve", lambda e: e.tensor_scalar(out=ra[:], in0=pt_[:], scalar1=flag[:, 0:1], scalar2=None, op0=ALU.mult),
                                 reads=[pt_, flag], writes=[ra])
                        elif kt < 32:
                            P.op("dve", lambda e: e.scalar_tensor_tensor(out=ra[:], in0=pt_[:], scalar=flag[:, 0:1], in1=ra[:], op0=ALU.mult, op1=ALU.add),
                                 reads=[ra, pt_, flag], writes=[ra])
                        else:
                            P.op("dve", lambda e: e.tensor_tensor(out=ra[:, c0:512], in0=ra[:, c0:512], in1=pt_[:, c0:512], op=ALU.add),
                                 reads=[ra, pt_], writes=[ra])
                        P.op("pe", lambda e: e.matmul(ot[:, c0:512], lhsT=V_[:, kt, 0:128], rhs=pt_[:, c0:512], start=(idx == 0), stop=last),
                             reads=[V_, pt_], writes=[ot])
                        if last and m == 1:
                            epi(j)

                    def epi(j):
                        ys_ = yaTs[j % 2]
                        for m in range(2):
                            P.op("pe", lambda e: e.matmul(sumP[m][:], lhsT=ones32[:], rhs=racc[j % 2][m][:], start=False, stop=True),
                                 reads=[ones32, racc[j % 2][m]], writes=[sumP[m]])
                        P.op("act", lambda e: e.copy(out=o0s[:], in_=OT[j % 2][0][:]), reads=[OT[j % 2][0]], writes=[o0s])
                        P.op("dve", lambda e: e.reciprocal(out=rr[0][:], in_=sumP[0][:]), reads=[sumP[0]], writes=[rr[0]])
                        P.op("act", lambda e: e.copy(out=o1s[:], in_=OT[j % 2][1][:]), reads=[OT[j % 2][1]], writes=[o1s])
                        P.op("dve", lambda e: e.reciprocal(out=rr[1][:], in_=sumP[1][:]), reads=[sumP[1]], writes=[rr[1]])
                        P.op("dve", lambda e: e.tensor_tensor(out=tq[:], in0=o0s[:], in1=rr[0][:], op=ALU.mult), reads=[o0s, rr[0]], writes=[tq])
                        P.op("dve", lambda e: e.tensor_scalar(out=rr[1][:], in0=rr[1][:], scalar1=nlam[:, 0:1], scalar2=None, op0=ALU.mult),
                             reads=[rr[1], nlam], writes=[rr[1]])
                        P.op("dve", lambda e: e.tensor_tensor(out=uq[:], in0=o1s[:], in1=rr[1][:], op=ALU.mult), reads=[o1s, rr[1]], writes=[uq])
                        P.op("dve", lambda e: e.tensor_tensor(out=oq[:], in0=tq[:], in1=uq[:], op=ALU.add), reads=[tq, uq], writes=[oq])
                        P.op("dve", lambda e: e.tensor_tensor(out=o2[:], in0=oq[:], in1=oq[:], op=ALU.mult), reads=[oq], writes=[o2])
                        P.op("pe", lambda e: e.matmul(pE[:], lhsT=ones32[:], rhs=o2[:], start=True, stop=True), reads=[ones32, o2], writes=[pE])
                        P.op("act", lambda e: e.activation(out=rs[:], in_=pE[:], func=AF.Ln, bias=EPS, scale=1.0 / 128), reads=[pE], writes=[rs])
                        P.op("act", lambda e: e.activation(out=rs[:], in_=rs[:], func=AF.Exp, scale=-0.5), reads=[rs], writes=[rs])
                        P.op("dve", lambda e: e.scalar_tensor_tensor(out=ys_[:], in0=oq[:], scalar=subcol[:, 0:1], in1=rs[:], op0=ALU.mult, op1=ALU.mult),
                             reads=[oq, subcol, rs], writes=[ys_])
                        P.op("sp", lambda e: e.dma_start(out=YAT[h, :, j * 512:(j + 1) * 512], in_=ys_[:]), reads=[ys_], dma=True)

                    n = len(steps)
                    for si in range(n + 3):
                        if si < n:
                            front(si)
                        if si >= 3:
                            back(si - 3)
                    gs_ += n
                P.barrier()

        if upto >= 4:
            with ExitStack() as ph:
                whq = sb(ph, "whq", [128, 8, 512], BF16)
                whf = sb(ph, "whf", [128, 8, 512], BF16)
                whi = sb(ph, "whi", [128, 8, 512], BF16)
                whg = sb(ph, "whg", [128, 8, 512], BF16)
                for w_, c0 in ((whq, 1536), (whf, 2048), (whi, 2560), (whg, 3072)):
                    for kh in range(2):
                        P.op("pool", lambda e: e.dma_start(out=w_[:, kh * 4:(kh + 1) * 4, :], in_=w_in_v[:, kh * 4:(kh + 1) * 4, c0:c0 + 512]),
                             writes=[w_], dma=True)
                rm = sb(ph, "rm", [128, 512], F32)
                P.op("pool", lambda e: e.memset(rm[:], 1.0), writes=[rm])
                for t in range(4):
                    P.op("pool", lambda e: e.memset(rm[:, t * 128:t * 128 + 1], 0.0), writes=[rm])
                cmask = sb(ph, "cmask", [128, 128], F32)
                P.op("pool", lambda e: e.memset(cmask[:], 1.0), writes=[cmask])
                P.op("pool", lambda e: e.affine_select(out=cmask[:], in_=cmask[:], pattern=[[1, 128]], compare_op=ALU.is_ge,
                                                       fill=0.0, base=0, channel_multiplier=-1), reads=[cmask], writes=[cmask])
                hTb = [sb(ph, "hTb4_%d" % i, [128, 4, 8, 128], BF16) for i in range(2)]
                sg = [sb(ph, "sg%d" % i, [128, 512], F32) for i in range(4)]
                lf = [sb(ph, "lf%d" % i, [128, 512], F32) for i in range(4)]
                kk = [sb(ph, "kk%d" % i, [128, 512], F32) for i in range(4)]
                Gc = [sb(ph, "Gc%d" % i, [128, 512], F32) for i in range(4)]
                eg = [sb(ph, "eg%d" % i, [128, 512], F32) for i in range(4)]
                kt32 = [sb(ph, "kt32_%d" % i, [128, 512], F32) for i in range(4)]
                qs = [sb(ph, "qs%d" % i, [128, 512], F32) for i in range(4)]
                qraw = [sb(ph, "qraw%d" % i, [128, 512], F32) for i in range(4)]
                gex = [sb(ph, "gex%d" % i, [128, 512], F32) for i in range(2)]
                dec_all = sb(ph, "dec_all", [128, 4, NT], F32)
                q_t = sb(ph, "q_t", [128, 4, 512], BF16)
                k_t = sb(ph, "k_t", [128, 4, 512], BF16)
                kdec = sb(ph, "kdec", [128, 4, 512], BF16)
                kdT = sb(ph, "kdT", [128, 4, 4, 128], BF16)
                vtm = sb(ph, "vtm", [128, 4, 512], BF16)
                gsn = sb(ph, "gsn", [128, 4, 512], F32)
                S32 = sb(ph, "S32", [128, 4, 128], F32)
                yb = [sb(ph, "yb%d" % i, [128, 512], BF16) for i in range(2)]
                ybTs = [sb(ph, "ybTs%d" % i, [128, 4, 512], BF16) for i in range(2)]
                pF = [ps(ph, "pF%d" % i, [128, 512], F32) for i in range(2)]
                pAT = [ps(ph, "pAT%d" % i, [128, 4, 128], F32) for i in range(2)]
                pO = [ps(ph, "pO%d" % i, [128, 4, 128], F32) for i in range(2)]
                pS = ps(ph, "pS", [128, 4, 128], F32)
                AT4 = [sb(ph, "AT4_%d" % i, [128, 4, 128], BF16) for i in range(2)]
                Sbf4 = sb(ph, "Sbf4", [128, 4, 128], BF16)
                sq4 = sb(ph, "sq4", [128, 4, 128], F32)
                tm4 = sb(ph, "tm4", [128, 4, 128], F32)
                ss4 = [sb(ph, "ss4_%d" % i, [128, 4], F32) for i in range(2)]
                st4 = [sb(ph, "st4_%d" % i, [128, 4], F32) for i in range(2)]
                P.op("pool", lambda e: e.memset(Sbf4[:], 0.0), writes=[Sbf4])
                P.op("dve", lambda e: e.memset(S32[:], 0.0), writes=[S32])
                pTk16 = ps(ph, "pTk16", [128, 4, 128], BF16)
                if upto >= 5:
                    zt = sb(ph, "zt", [128, 1024], BF16)
                    P.op("pool", lambda e: e.memset(zt[:], 0.0), writes=[zt])
                    for i in range(NSLOT // 128):
                        P.op("pool", lambda e: e.dma_start(out=XS[i * 128:(i + 1) * 128, :], in_=zt[:]), reads=[zt], dma=True, ring="pre")

                def ld4(bi):
                    P.op("sp", lambda e: e.dma_start(out=hTb[bi % 2][:], in_=HT[bi * 4:(bi + 1) * 4].rearrange("t p k j -> p t k j")),
                         writes=[hTb[bi % 2]], dma=True)

                ld4(0)
                cnt = 0
                oc = 0
                for bi in range(16):
                    if bi + 1 < 16:
                        ld4(bi + 1)
                    own = bi >= 8
                    hb_ = hTb[bi % 2]
                    rr3 = lambda ap: ap.rearrange("p (t j) -> p t j", j=128)
                    for h in range(4):
                        p_ = pF[cnt % 2]
                        cnt += 1
                        for kc in range(8):
                            P.op("pe", lambda e: e.matmul(rr3(p_[:]), lhsT=whf[:, kc, h * 128:(h + 1) * 128],
                                                          rhs=hb_[:, :, kc, :], start=(kc == 0), stop=(kc == 7)), reads=[whf, hb_], writes=[p_])
                        P.op("act", lambda e: e.activation(out=sg[h][:], in_=p_[:], func=AF.Exp, scale=-1.0), reads=[p_], writes=[sg[h]])
                    if own:
                        for h in range(4):
                            p_ = pF[cnt % 2]
                            cnt += 1
                            for kc in range(8):
                                P.op("pe", lambda e: e.matmul(rr3(p_[:]), lhsT=whq[:, kc, h * 128:(h + 1) * 128],
                                                              rhs=hb_[:, :, kc, :], start=(kc == 0), stop=(kc == 7)), reads=[whq, hb_], writes=[p_])
                            P.op("act", lambda e: e.copy(out=qraw[h][:], in_=p_[:]), reads=[p_], writes=[qraw[h]])
                            P.op("act", lambda e: e.activation(out=qs[h][:], in_=p_[:], func=AF.Exp, scale=-1.0), reads=[p_], writes=[qs[h]])
                    for h in range(4):
                        P.op("act", lambda e: e.activation(out=sg[h][:], in_=sg[h][:], func=AF.Ln, bias=1.0, scale=1.0), reads=[sg[h]], writes=[sg[h]])
                        P.op("act", lambda e: e.activation(out=sg[h][:], in_=sg[h][:], func=AF.Exp, scale=-1.0), reads=[sg[h]], writes=[sg[h]])
                        P.op("dve", lambda e: e.tensor_scalar(out=sg[h][:], in0=sg[h][:], scalar1=oml[:, h:h + 1], scalar2=lb[:, h:h + 1],
                                                              op0=ALU.mult, op1=ALU.add), reads=[sg[h], oml, lb], writes=[sg[h]])
                    for h in range(4):
                        P.op("act", lambda e: e.activation(out=lf[h][:], in_=sg[h][:], func=AF.Ln), reads=[sg[h]], writes=[lf[h]])
                    for h in range(4):
                        P.op("dve", lambda e: e.tensor_scalar(out=kk[h][:], in0=sg[h][:], scalar1=-1.0, scalar2=1.0, op0=ALU.mult, op1=ALU.add),
                             reads=[sg[h]], writes=[kk[h]])
                        P.op("dve", lambda e: e.tensor_tensor_scan(out=Gc[h][:], data0=rm[:], data1=lf[h][:], initial=0.0, op0=ALU.mult, op1=ALU.add),
                             reads=[rm, lf[h]], writes=[Gc[h]])
                    for h in range(4):
                        P.op("act", lambda e: e.activation(out=eg[h][:], in_=Gc[h][:], func=AF.Exp, scale=-1.0), reads=[Gc[h]], writes=[eg[h]])
                        P.op("act", lambda e: e.activation(out=dec_all[:, h, bi * 4:(bi + 1) * 4], in_=rr3(Gc[h][:])[:, :, 127], func=AF.Exp),
                             reads=[Gc[h]], writes=[dec_all])
                    for h in range(4):
                        P.op("dve", lambda e: e.tensor_tensor(out=kt32[h][:], in0=kk[h][:], in1=eg[h][:], op=ALU.mult), reads=[kk[h], eg[h]], writes=[kt32[h]])
                        P.op("dve", lambda e: e.tensor_tensor(out=rr3(kdec[:, h, :]), in0=rr3(kt32[h][:]),
                                                              in1=dec_all[:, h, bi * 4:(bi + 1) * 4].unsqueeze(2).to_broadcast([128, 4, 128]),
                                                              op=ALU.mult), reads=[kt32[h], dec_all], writes=[kdec])
                    for h in range(4):
                        for sub in range(4):
                            P.op("pe", lambda e: e.transpose(out=pTk16[:, sub, :], in_=kdec[:, h, sub * 128:(sub + 1) * 128], identity=ident[:]),
                                 reads=[kdec, ident], writes=[pTk16])
                        P.op("act", lambda e: e.copy(out=kdT[:, :, h, :], in_=pTk16[:]), reads=[pTk16], writes=[kdT])
                    if own:
                        for h in range(4):
                            P.op("act", lambda e: e.copy(out=k_t[:, h, :], in_=kt32[h][:]), reads=[kt32[h]], writes=[k_t])
                            P.op("act", lambda e: e.activation(out=eg[h][:], in_=Gc[h][:], func=AF.Exp), reads=[Gc[h]], writes=[eg[h]])
                        for h in range(4):
                            P.op("act", lambda e: e.activation(out=qs[h][:], in_=qs[h][:], func=AF.Ln, bias=1.0, scale=1.0), reads=[qs[h]], writes=[qs[h]])
                            P.op("act", lambda e: e.activation(out=qs[h][:], in_=qs[h][:], func=AF.Exp, scale=-1.0), reads=[qs[h]], writes=[qs[h]])
                            P.op("dve", lambda e: e.tensor_tensor(out=qs[h][:], in0=qs[h][:], in1=qraw[h][:], op=ALU.mult), reads=[qs[h], qraw[h]], writes=[qs[h]])
                            P.op("dve", lambda e: e.tensor_tensor(out=q_t[:, h, :], in0=qs[h][:], in1=eg[h][:], op=ALU.mult), reads=[qs[h], eg[h]], writes=[q_t])
                    for sub in range(4):
                        p_ = pF[cnt % 2]
                        cnt += 1
                        for kc in range(8):
                            P.op("pe", lambda e: e.matmul(p_[:], lhsT=hb_[:, sub, kc, :], rhs=whi[:, kc, :], start=(kc == 0), stop=(kc == 7)),
                                 reads=[hb_, whi], writes=[p_])
                        if own:
                            P.op("act", lambda e: e.copy(out=vtm[:, sub, :], in_=p_[:]), reads=[p_], writes=[vtm])
                        else:
                            P.op("dve", lambda e: e.tensor_scalar(out=vtm[:, sub, :], in0=p_[:], scalar1=flag[:, 0:1], scalar2=None, op0=ALU.mult),
                                 reads=[p_, flag], writes=[vtm])
                        if own:
                            p2 = pF[cnt % 2]
                            cnt += 1
                            gx = gex[sub % 2]
                            for kc in range(8):
                                P.op("pe", lambda e: e.matmul(p2[:], lhsT=hb_[:, sub, kc, :], rhs=whg[:, kc, :], start=(kc == 0), stop=(kc == 7)),
                                     reads=[hb_, whg], writes=[p2])
                            P.op("act", lambda e: e.activation(out=gx[:], in_=p2[:], func=AF.Exp, scale=-1.0), reads=[p2], writes=[gx])
                            P.op("act", lambda e: e.copy(out=gsn[:, sub, :], in_=p2[:]), reads=[p2], writes=[gsn])
                            P.op("act", lambda e: e.activation(out=gx[:], in_=gx[:], func=AF.Ln, bias=1.0, scale=1.0), reads=[gx], writes=[gx])
                            P.op("act", lambda e: e.activation(out=gx[:], in_=gx[:], func=AF.Exp, scale=-1.0), reads=[gx], writes=[gx])
                            P.op("dve", lambda e: e.tensor_tensor(out=gsn[:, sub, :], in0=gsn[:, sub, :], in1=gx[:], op=ALU.mult), reads=[gsn, gx], writes=[gsn])
                            P.op("dve", lambda e: e.tensor_tensor(out=gsn[:, sub, :].rearrange("p (h v) -> p h v", v=128),
                                                                  in0=gsn[:, sub, :].rearrange("p (h v) -> p h v", v=128),
                                                                  in1=hgn_b[:].unsqueeze(1).to_broadcast([128, 4, 128]), op=ALU.mult),
                                 reads=[gsn, hgn_b], writes=[gsn])
                    for sub in range(4):
                        tile = bi * 4 + sub
                        yb_ = yb[sub % 2]
                        cols = slice(sub * 128, (sub + 1) * 128)
                        if own:
                            at_p = pAT[oc % 2]
                            at_s = AT4[oc % 2]
                            o_p = pO[oc % 2]
                            s_ = ss4[oc % 2]
                            st_ = st4[oc % 2]
                            oc += 1
                            for h in range(4):
                                P.op("pe", lambda e: e.matmul(at_p[:, h, :], lhsT=k_t[:, h, cols], rhs=q_t[:, h, cols], start=True, stop=True),
                                     reads=[k_t, q_t], writes=[at_p])
                            P.op("dve", lambda e: e.tensor_tensor(out=at_s[:], in0=at_p[:], in1=cmask[:].unsqueeze(1).to_broadcast([128, 4, 128]), op=ALU.mult),
                                 reads=[at_p, cmask], writes=[at_s])
                            for h in range(4):
                                P.op("pe", lambda e: e.matmul(o_p[:, h, :], lhsT=at_s[:, h, :], rhs=vtm[:, sub, h * 128:(h + 1) * 128], start=True, stop=False),
                                     reads=[at_s, vtm], writes=[o_p])
                                P.op("pe", lambda e: e.matmul(o_p[:, h, :], lhsT=q_t[:, h, cols], rhs=Sbf4[:, h, :], start=False, stop=True),
                                     reads=[q_t, Sbf4], writes=[o_p])
                            P.op("act", lambda e: e.activation(out=sq4[:], in_=o_p[:], func=AF.Square), reads=[o_p], writes=[sq4])
                            P.op("dve", lambda e: e.reduce_sum(out=s_[:], in_=sq4[:], axis=mybir.AxisListType.X), reads=[sq4], writes=[s_])
                            P.op("dve", lambda e: e.tensor_scalar(out=st_[:], in0=s_[:], scalar1=1.0 / 128, scalar2=EPS, op0=ALU.mult, op1=ALU.add),
                                 reads=[s_], writes=[st_])
                            P.op("act", lambda e: e.activation(out=st_[:], in_=st_[:], func=AF.Ln), reads=[st_], writes=[st_])
                            P.op("act", lambda e: e.activation(out=s_[:], in_=st_[:], func=AF.Exp, scale=-0.5), reads=[st_], writes=[s_])
                            P.op("dve", lambda e: e.tensor_tensor(out=tm4[:], in0=o_p[:], in1=s_[:].unsqueeze(2).to_broadcast([128, 4, 128]), op=ALU.mult),
                                 reads=[o_p, s_], writes=[tm4])
                            P.op("dve", lambda e: e.tensor_tensor(out=yb_[:], in0=tm4[:].rearrange("p h v -> p (h v)"), in1=gsn[:, sub, :], op=ALU.mult),
                                 reads=[tm4, gsn], writes=[yb_])
                        for h in range(4):
                            P.op("pe", lambda e: e.matmul(pS[:, h, :], lhsT=kdT[:, sub, h, :], rhs=vtm[:, sub, h * 128:(h + 1) * 128], start=True, stop=True),
                                 reads=[kdT, vtm], writes=[pS])
                        P.op("dve", lambda e: e.tensor_tensor(out=S32[:], in0=S32[:], in1=dec_all[:, :, tile:tile + 1].to_broadcast([128, 4, 128]), op=ALU.mult),
                             reads=[S32, dec_all], writes=[S32])
                        P.op("dve", lambda e: e.tensor_tensor(out=S32[:], in0=S32[:], in1=pS[:], op=ALU.add), reads=[S32, pS], writes=[S32])
                        P.op("act", lambda e: e.copy(out=Sbf4[:], in_=S32[:]), reads=[S32], writes=[Sbf4])
                        if own:
                            for h in range(4):
                                P.op("pe", lambda e: e.transpose(out=pTk16[:, h, :], in_=yb_[:, h * 128:(h + 1) * 128], identity=ident[:]),
                                     reads=[yb_, ident], writes=[pTk16])
                            P.op("act", lambda e: e.copy(out=ybTs[bi % 2][:, :, sub * 128:(sub + 1) * 128], in_=pTk16[:]), reads=[pTk16], writes=[ybTs[bi % 2]])
                    if own:
                        for h in range(4):
                            P.op("sp", lambda e: e.dma_start(out=YBT[h, :, (bi - 8) * 512:(bi - 7) * 512], in_=ybTs[bi % 2][:, h, :]),
                                 reads=[ybTs[bi % 2]], dma=True)
                P.barrier()

        if upto >= 5:
            with ExitStack() as ph:
                wga = sb(ph, "wga", [128, 8, D], BF16)
                wgb = sb(ph, "wgb", [128, 8, D], BF16)
                wa = sb(ph, "wa", [128, 4, D], BF16)
                wb = sb(ph, "wb", [128, 4, D], BF16)
                wo = sb(ph, "wo", [128, 8, D], BF16)
                rw = sb(ph, "rw", [128, 8, NE], BF16)
                rbb = sb(ph, "rbb", [128, NE], F32)
                for w_, c0 in ((wga, 3584), (wgb, 4608)):
                    for kh in range(4):
                        P.op("pool", lambda e: e.dma_start(out=w_[:, kh * 2:(kh + 1) * 2, :], in_=w_in_v[:, kh * 2:(kh + 1) * 2, c0:c0 + D]),
                             writes=[w_], dma=True)
                P.op("pool", lambda e: e.dma_start(out=wa[:], in_=w_ba.rearrange("(h p) n -> p h n", p=128)), writes=[wa], dma=True)
                P.op("pool", lambda e: e.dma_start(out=wb[:], in_=w_bb.rearrange("(h p) n -> p h n", p=128)), writes=[wb], dma=True)
                for kh in range(4):
                    P.op("pool", lambda e: e.dma_start(out=wo[:, kh * 2:(kh + 1) * 2, :], in_=w_out.rearrange("(kc p) n -> p kc n", p=128)[:, kh * 2:(kh + 1) * 2, :]),
                         writes=[wo], dma=True)
                P.op("pool", lambda e: e.dma_start(out=rw[:], in_=router_w.rearrange("(kc p) n -> p kc n", p=128)), writes=[rw], dma=True)
                P.op("sp", lambda e: e.dma_start(out=rbb[:], in_=router_b[0:1, :].partition_broadcast(128)), writes=[rbb], dma=True)
                hTb = [sb(ph, "hTb5_%d" % i, [128, 4, 8, 128], BF16) for i in range(2)]
                yaTb = [sb(ph, "yaTb%d" % i, [128, 4, 512], BF16) for i in range(2)]
                ybTb = [sb(ph, "ybTb%d" % i, [128, 4, 512], BF16) for i in range(2)]
                sga = [sb(ph, "sga%d" % i, [128, 512], F32) for i in range(2)]
                sgb = [sb(ph, "sgb%d" % i, [128, 512], F32) for i in range(2)]
                ta = [sb(ph, "ta%d" % i, [128, 512], F32) for i in range(2)]
                tb = [sb(ph, "tb%d" % i, [128, 512], F32) for i in range(2)]
                mT = [sb(ph, "mT%d" % i, [128, 8, 512], BF16) for i in range(2)]
                xt = [sb(ph, "xt5_%d" % i, [128, D], F32) for i in range(2)]
                tmp = sb(ph, "tmp5", [128, D], F32)
                x1 = [sb(ph, "x1_%d" % i, [128, D], F32) for i in range(2)]
                tmp2 = sb(ph, "tmp5b", [128, D], F32)
                junk = tmp2
                h2 = [sb(ph, "h2_%d" % i, [128, D], BF16) for i in range(2)]
                h2Tt = [sb(ph, "h2Tt%d" % i, [128, 8, 128], BF16) for i in range(2)]
                s2 = [sb(ph, "s5a%d" % i, [128, 2], F32) for i in range(2)]
                s3 = [sb(ph, "s5b%d" % i, [128, 1], F32) for i in range(2)]
                st5 = [sb(ph, "st5_%d" % i, [128, 1], F32) for i in range(2)]
                lg = [sb(ph, "lg%d" % i, [128, NE], F32) for i in range(2)]
                t8 = [sb(ph, "t8_%d" % i, [128, 8], F32) for i in range(2)]
                msk = [sb(ph, "msk%d" % i, [128, NE], F32) for i in range(2)]
                ex = [sb(ph, "ex%d" % i, [128, NE], F32) for i in range(2)]
                gs = [sb(ph, "gs%d" % i, [128, 2], F32) for i in range(2)]
                pG = [ps(ph, "pG%d" % i, [128, 512], F32) for i in range(2)]
                pP = [ps(ph, "pP%d" % i, [128, 512], F32) for i in range(2)]
                pY = [ps(ph, "pY%d" % i, [128, 512], F32) for i in range(2)]
                pT5 = ps(ph, "pT5", [128, 8, 128], BF16)
                pR = ps(ph, "pR", [128, 512], F32)

                def ld5(j):
                    P.op("sp", lambda e: e.dma_start(out=hTb[j % 2][:], in_=HT[32 + j * 4:32 + (j + 1) * 4].rearrange("t p k j -> p t k j")),
                         writes=[hTb[j % 2]], dma=True)
                    P.op("sp", lambda e: e.dma_start(out=yaTb[j % 2][:], in_=YAT[:, :, j * 512:(j + 1) * 512].rearrange("h p t -> p h t")),
                         writes=[yaTb[j % 2]], dma=True)
                    P.op("sp", lambda e: e.dma_start(out=ybTb[j % 2][:], in_=YBT[:, :, j * 512:(j + 1) * 512].rearrange("h p t -> p h t")),
                         writes=[ybTb[j % 2]], dma=True)

                ld5(0)
                cntb = [0, 0]

                def gate_part(j, dcs):
                    hb_ = hTb[j % 2]
                    ya_ = yaTb[j % 2]
                    yb_ = ybTb[j % 2]
                    m_ = mT[j % 2]
                    cnt = cntb[0]
                    for dc in dcs:
                        for (wg_, wbr_, y_, sg_, t_, tag) in ((wga, wa, ya_, sga, ta, 0), (wgb, wb, yb_, sgb, tb, 1)):
                            g_p = pG[cnt % 2]
                            p_p = pP[cnt % 2]
                            s_ = sg_[dc % 2]
                            u_ = t_[dc % 2]
                            cnt += 1
                            for kc in range(8):
                                P.op("pe", lambda e: e.matmul(g_p[:].rearrange("p (t j) -> p t j", j=128), lhsT=wg_[:, kc, dc * 128:(dc + 1) * 128],
                                                              rhs=hb_[:, :, kc, :], start=(kc == 0), stop=(kc == 7)), reads=[wg_, hb_], writes=[g_p])
                            P.op("act", lambda e: e.activation(out=s_[:], in_=g_p[:], func=AF.Exp, scale=-1.0), reads=[g_p], writes=[s_])
                            P.op("act", lambda e: e.activation(out=s_[:], in_=s_[:], func=AF.Ln, bias=1.0, scale=1.0), reads=[s_], writes=[s_])
                            P.op("act", lambda e: e.activation(out=s_[:], in_=s_[:], func=AF.Exp, scale=-1.0), reads=[s_], writes=[s_])
                            for h in range(4):
                                P.op("pe", lambda e: e.matmul(p_p[:], lhsT=wbr_[:, h, dc * 128:(dc + 1) * 128], rhs=y_[:, h, :],
                                                              start=(h == 0), stop=(h == 3)), reads=[wbr_, y_], writes=[p_p])
                            P.op("dve", lambda e: e.tensor_tensor(out=u_[:], in0=p_p[:], in1=s_[:], op=ALU.mult), reads=[p_p, s_], writes=[u_])
                        P.op("dve", lambda e: e.tensor_tensor(out=m_[:, dc, :], in0=ta[dc % 2][:], in1=tb[dc % 2][:], op=ALU.add),
                             reads=[ta[dc % 2], tb[dc % 2]], writes=[m_])
                    cntb[0] = cnt

                def sub_A(j, sub):
                    m_ = mT[j % 2]
                    tl = j * 4 + sub
                    if True:
                        x_ = xt[tl % 2]
                        x1_ = x1[tl % 2]
                        h2_ = h2[tl % 2]
                        hT_ = h2Tt[tl % 2]
                        sa = s2[tl % 2]
                        sb_ = s3[tl % 2]
                        st_ = st5[tl % 2]
                        lg_ = lg[tl % 2]
                        t8_ = t8[tl % 2]
                        mk_ = msk[tl % 2]
                        ex_ = ex[tl % 2]
                        gs_ = gs[tl % 2]
                        P.op("sp", lambda e: e.dma_start(out=x_[:], in_=xall[TOWN + tl * 128:TOWN + (tl + 1) * 128, :]), writes=[x_], dma=True)
                        for half in range(2):
                            for kc in range(8):
                                P.op("pe", lambda e: e.matmul(pY[half][:], lhsT=m_[:, kc, sub * 128:(sub + 1) * 128], rhs=wo[:, kc, half * 512:(half + 1) * 512],
                                                              start=(kc == 0), stop=(kc == 7)), reads=[m_, wo], writes=[pY[half]])
                            P.op("act", lambda e: e.activation(out=junk[:, half * 512:(half + 1) * 512], in_=pY[half][:], func=AF.Square,
                                                               accum_out=sa[:, half:half + 1]), reads=[pY[half]], writes=[junk, sa])
                        P.op("dve", lambda e: e.tensor_tensor(out=sb_[:], in0=sa[:, 0:1], in1=sa[:, 1:2], op=ALU.add), reads=[sa], writes=[sb_])
                        rstd_from_ss(sb_, D, st_)
                        for half in range(2):
                            P.op("dve", lambda e: e.scalar_tensor_tensor(out=tmp[:, half * 512:(half + 1) * 512], in0=pY[half][:], scalar=sb_[:, 0:1],
                                                                         in1=modb[:, 2 * D + half * 512:2 * D + (half + 1) * 512], op0=ALU.mult, op1=ALU.mult),
                                 reads=[pY[half], sb_, modb], writes=[tmp])
                        P.op("dve", lambda e: e.tensor_tensor(out=x1_[:], in0=tmp[:], in1=x_[:], op=ALU.add), reads=[tmp, x_], writes=[x1_])
                        P.op("sp", lambda e: e.dma_start(out=X1[tl * 128:(tl + 1) * 128, :], in_=x1_[:]), reads=[x1_], dma=True)
                        P.op("act", lambda e: e.activation(out=junk[:], in_=x1_[:], func=AF.Square, accum_out=sb_[:, 0:1]), reads=[x1_], writes=[junk, sb_])
                        rstd_from_ss(sb_, D, st_)
                        P.op("dve", lambda e: e.scalar_tensor_tensor(out=tmp2[:], in0=x1_[:], scalar=sb_[:, 0:1], in1=A2(), op0=ALU.mult, op1=ALU.mult),
                             reads=[x1_, sb_, modb], writes=[tmp2])
                        P.op("dve", lambda e: e.tensor_tensor(out=h2_[:], in0=tmp2[:], in1=B2(), op=ALU.add), reads=[tmp2, modb], writes=[h2_])

                def sub_B(j, sub):
                    tl = j * 4 + sub
                    if True:
                        x_ = xt[tl % 2]
                        x1_ = x1[tl % 2]
                        h2_ = h2[tl % 2]
                        hT_ = h2Tt[tl % 2]
                        sa = s2[tl % 2]
                        sb_ = s3[tl % 2]
                        st_ = st5[tl % 2]
                        lg_ = lg[tl % 2]
                        t8_ = t8[tl % 2]
                        mk_ = msk[tl % 2]
                        ex_ = ex[tl % 2]
                        gs_ = gs[tl % 2]
                        for kc in range(8):
                            P.op("pe", lambda e: e.transpose(out=pT5[:, kc, :], in_=h2_[:, kc * 128:(kc + 1) * 128], identity=ident[:]),
                                 reads=[h2_, ident], writes=[pT5])
                        P.op("act", lambda e: e.copy(out=hT_[:], in_=pT5[:]), reads=[pT5], writes=[hT_])
                        P.op("sp", lambda e: e.dma_start(out=H2TM[tl * 128:(tl + 1) * 128, :], in_=h2_[:]), reads=[h2_], dma=True)
                        for kc in range(8):
                            P.op("pe", lambda e: e.matmul(pR[:, 0:NE], lhsT=hT_[:, kc, :], rhs=rw[:, kc, :], start=(kc == 0), stop=(kc == 7)),
                                 reads=[hT_, rw], writes=[pR])
                        P.op("dve", lambda e: e.tensor_tensor(out=lg_[:], in0=pR[:, 0:NE], in1=rbb[:], op=ALU.add), reads=[pR, rbb], writes=[lg_])
                        P.op("dve", lambda e: e.max(out=t8_[:], in_=lg_[:]), reads=[lg_], writes=[t8_])
                        P.op("dve", lambda e: e.tensor_scalar(out=mk_[:], in0=lg_[:], scalar1=t8_[:, 3:4], scalar2=None, op0=ALU.is_ge), reads=[lg_, t8_], writes=[mk_])
                        P.op("dve", lambda e: e.tensor_scalar(out=gs_[:, 0:1], in0=t8_[:, 0:1], scalar1=-1.0, scalar2=None, op0=ALU.mult), reads=[t8_], writes=[gs_])
                        P.op("act", lambda e: e.activation(out=ex_[:], in_=lg_[:], func=AF.Exp, bias=gs_[:, 0:1], scale=1.0), reads=[lg_, gs_], writes=[ex_])
                        P.op("dve", lambda e: e.tensor_tensor(out=ex_[:], in0=ex_[:], in1=mk_[:], op=ALU.mult), reads=[ex_, mk_], writes=[ex_])
                        P.op("dve", lambda e: e.reduce_sum(out=gs_[:, 1:2], in_=ex_[:], axis=mybir.AxisListType.X), reads=[ex_], writes=[gs_])
                        P.op("dve", lambda e: e.reciprocal(out=gs_[:, 1:2], in_=gs_[:, 1:2]), reads=[gs_], writes=[gs_])
                        P.op("dve", lambda e: e.tensor_scalar(out=Gall[:, tl, :], in0=ex_[:], scalar1=gs_[:, 1:2], scalar2=None, op0=ALU.mult),
                             reads=[ex_, gs_], writes=[Gall])

                gate_part(0, range(8))
                for j in range(8):
                    if j + 1 < 8:
                        ld5(j + 1)
                    for sub in range(4):
                        sub_A(j, sub)
                        if sub > 0:
                            sub_B(j, sub - 1)
                        if j + 1 < 8:
                            gate_part(j + 1, (2 * sub, 2 * sub + 1))
                    sub_B(j, 3)
                if dbg:
                    P.op("sp", lambda e: e.dma_start(out=GDBG[:, :, :], in_=Gall[:]), reads=[Gall], dma=True)
                P.barrier()

        if upto >= 5:
            P.op("dve", lambda e: e.tensor_copy(out=G2t[:], in_=G2()), reads=[modb], writes=[G2t])
            with ExitStack() as ph:
                maskf = sb(ph, "maskf", [128, 32, NE], F32)
                maskb = sb(ph, "maskb", [128, 32 * NE], BF16)
                Umat = sb(ph, "Umat", [128, 128], BF16)
                onesb = sb(ph, "onesb", [128, 128], BF16)
                cnt = sb(ph, "cnt", [128, 32, NE], F32)
                tot = sb(ph, "tot", [128, 32, NE], F32)
                base = sb(ph, "base", [128, 32, NE], F32)
                key = sb(ph, "key", [128, 32, NE], F32)
                ntot = sb(ph, "ntot", [128, NE], F32)
                nbi = sb(ph, "nbi", [128, NE], I32)
                nbf = sb(ph, "nbf", [128, NE], F32)
                sbe = sb(ph, "sbe", [128, NE], F32)
                starts = sb(ph, "starts", [128, NE], F32)
                t8r = [sb(ph, "t8r%d" % i, [128, 8], F32) for i in range(2)]
                eqr = [sb(ph, "eqr%d" % i, [128, NE], F32) for i in range(4)]
                dest4f = sb(ph, "dest4f", [128, 32 * 4], F32)
                jidx_i = sb(ph, "jidx_i", [128, NBLK], I32)
                jidx = sb(ph, "jidx", [128, NBLK], F32)
                pidx_i = sb(ph, "pidx_i", [128, 1], I32)
                pidx = sb(ph, "pidx", [128, 1], F32)
                cmp = sb(ph, "cmp", [128, NBLK, NE], F32)
                Ej = sb(ph, "Ej", [128, NBLK], F32)
                bw = sb(ph, "bw", [128, NBLK], F32)
                offwf = sb(ph, "offwf", [128, NBLK, 8], F32)
                offbf = sb(ph, "offbf", [128, NBLK], F32)
                pc = [ps(ph, "pc%d" % i, [128, 512], F32) for i in range(2)]
                ptt = [ps(ph, "ptt%d" % i, [128, 512], F32) for i in range(2)]
                P.op("dve", lambda e: e.tensor_scalar(out=maskf[:], in0=Gall[:], scalar1=0.0, scalar2=None, op0=ALU.is_gt), reads=[Gall], writes=[maskf])
                P.op("dve", lambda e: e.tensor_copy(out=maskb[:], in_=maskf[:].rearrange("p t e -> p (t e)")), reads=[maskf], writes=[maskb])
                P.op("pool", lambda e: e.memset(Umat[:], 1.0), writes=[Umat])
                P.op("pool", lambda e: e.affine_select(out=Umat[:], in_=Umat[:], pattern=[[1, 128]], compare_op=ALU.is_gt, fill=0.0, base=0,
                                                       channel_multiplier=-1), reads=[Umat], writes=[Umat])
                P.op("pool", lambda e: e.memset(onesb[:], 1.0), writes=[onesb])
                for half in range(2):
                    P.op("pe", lambda e: e.matmul(pc[half][:], lhsT=Umat[:], rhs=maskb[:, half * 512:(half + 1) * 512], start=True, stop=True),
                         reads=[Umat, maskb], writes=[pc[half]])
                    P.op("pe", lambda e: e.matmul(ptt[half][:], lhsT=onesb[:], rhs=maskb[:, half * 512:(half + 1) * 512], start=True, stop=True),
                         reads=[onesb, maskb], writes=[ptt[half]])
                    P.op("dve", lambda e: e.tensor_copy(out=cnt[:, half * 16:(half + 1) * 16, :], in_=pc[half][:].rearrange("p (t e) -> p t e", e=NE)),
                         reads=[pc[half]], writes=[cnt])
                    P.op("dve", lambda e: e.tensor_copy(out=tot[:, half * 16:(half + 1) * 16, :], in_=ptt[half][:].rearrange("p (t e) -> p t e", e=NE)),
                         reads=[ptt[half]], writes=[tot])
                P.op("dve", lambda e: e.memset(base[:, 0, :], 0.0), writes=[base])
                for t in range(1, 32):
                    P.op("dve", lambda e: e.tensor_tensor(out=base[:, t, :], in0=base[:, t - 1, :], in1=tot[:, t - 1, :], op=ALU.add), reads=[base, tot], writes=[base])
                P.op("dve", lambda e: e.tensor_tensor(out=ntot[:], in0=base[:, 31, :], in1=tot[:, 31, :], op=ALU.add), reads=[base, tot], writes=[ntot])
                P.op("dve", lambda e: e.tensor_scalar(out=ntot[:], in0=ntot[:], scalar1=float(BLK - 1), scalar2=None, op0=ALU.add), reads=[ntot], writes=[ntot])
                P.op("dve", lambda e: e.tensor_copy(out=nbi[:], in_=ntot[:]), reads=[ntot], writes=[nbi])
                P.op("dve", lambda e: e.tensor_scalar(out=nbi[:], in0=nbi[:], scalar1=9, scalar2=None, op0=ALU.arith_shift_right), reads=[nbi], writes=[nbi])
                P.op("dve", lambda e: e.tensor_copy(out=nbf[:], in_=nbi[:]), reads=[nbi], writes=[nbf])
                ones_ne = sb(ph, "ones_ne", [128, NE], F32)
                P.op("dve", lambda e: e.memset(ones_ne[:], 1.0), writes=[ones_ne])
                P.op("dve", lambda e: e.tensor_tensor_scan(out=sbe[:], data0=ones_ne[:], data1=nbf[:], initial=0.0, op0=ALU.mult, op1=ALU.add),
                     reads=[ones_ne, nbf], writes=[sbe])
                P.op("dve", lambda e: e.tensor_tensor(out=sbe[:], in0=sbe[:], in1=nbf[:], op=ALU.subtract), reads=[sbe, nbf], writes=[sbe])
                P.op("dve", lambda e: e.tensor_scalar(out=starts[:], in0=sbe[:], scalar1=float(BLK), scalar2=None, op0=ALU.mult), reads=[sbe], writes=[starts])
                P.op("dve", lambda e: e.tensor_tensor(out=key[:], in0=cnt[:], in1=base[:], op=ALU.add), reads=[cnt, base], writes=[key])
                P.op("dve", lambda e: e.tensor_tensor(out=key[:], in0=key[:], in1=starts[:].unsqueeze(1).to_broadcast([128, 32, NE]), op=ALU.add),
                     reads=[key, starts], writes=[key])
                P.op("dve", lambda e: e.scalar_tensor_tensor(out=key[:], in0=key[:], scalar=1.0, in1=maskf[:], op0=ALU.add, op1=ALU.mult),
                     reads=[key, maskf], writes=[key])
                t8all = sb(ph, "t8all", [128, 32, 8], F32)
                eqb = sb(ph, "eqb", [128, 32, NE], F32)
                for t in range(32):
                    P.op("dve", lambda e: e.max(out=t8all[:, t, :], in_=key[:, t, :]), reads=[key], writes=[t8all])
                P.op("dve", lambda e: e.tensor_scalar(out=dest4f[:].rearrange("p (t k) -> p t k", k=4), in0=t8all[:, :, 0:4], scalar1=-1.0, scalar2=None, op0=ALU.add),
                     reads=[t8all], writes=[dest4f])
                for k in range(4):
                    P.op("dve", lambda e: e.tensor_tensor(out=eqb[:], in0=key[:], in1=t8all[:, :, k:k + 1].to_broadcast([128, 32, NE]), op=ALU.is_equal),
                         reads=[key, t8all], writes=[eqb])
                    P.op("dve", lambda e: e.tensor_tensor(out=eqb[:], in0=eqb[:], in1=Gall[:], op=ALU.mult), reads=[eqb, Gall], writes=[eqb])
                    P.op("dve", lambda e: e.reduce_sum(out=G4[:, :, k], in_=eqb[:], axis=mybir.AxisListType.X), reads=[eqb], writes=[G4])
                P.op("dve", lambda e: e.tensor_copy(out=dest4u[:], in_=dest4f[:]), reads=[dest4f], writes=[dest4u])
                P.op("pool", lambda e: e.iota(jidx_i[:], pattern=[[1, NBLK]], base=0, channel_multiplier=0), writes=[jidx_i])
                P.op("dve", lambda e: e.tensor_copy(out=jidx[:], in_=jidx_i[:]), reads=[jidx_i], writes=[jidx])
                P.op("pool", lambda e: e.iota(pidx_i[:], pattern=[[0, 1]], base=0, channel_multiplier=1), writes=[pidx_i])
                P.op("dve", lambda e: e.tensor_copy(out=pidx[:], in_=pidx_i[:]), reads=[pidx_i], writes=[pidx])
                P.op("dve", lambda e: e.tensor_tensor(out=cmp[:], in0=sbe[:].unsqueeze(1).to_broadcast([128, NBLK, NE]),
                                                      in1=jidx[:].unsqueeze(2).to_broadcast([128, NBLK, NE]), op=ALU.is_le), reads=[sbe, jidx], writes=[cmp])
                P.op("dve", lambda e: e.reduce_sum(out=Ej[:], in_=cmp[:], axis=mybir.AxisListType.X), reads=[cmp], writes=[Ej])
                P.op("dve", lambda e: e.tensor_scalar(out=Ej[:], in0=Ej[:], scalar1=-1.0, scalar2=None, op0=ALU.add), reads=[Ej], writes=[Ej])
                P.op("dve", lambda e: e.tensor_scalar(out=bw[:], in0=Ej[:], scalar1=float(D), scalar2=pidx[:, 0:1], op0=ALU.mult, op1=ALU.add),
                     reads=[Ej, pidx], writes=[bw])
                for kc in range(8):
                    P.op("dve", lambda e: e.tensor_scalar(out=offwf[:, :, kc], in0=bw[:], scalar1=float(kc * 128), scalar2=None, op0=ALU.add),
                         reads=[bw], writes=[offwf])
                P.op("dve", lambda e: e.tensor_copy(out=OFFW[:], in_=offwf[:]), reads=[offwf], writes=[OFFW])
                P.op("dve", lambda e: e.tensor_scalar(out=offbf[:], in0=Ej[:], scalar1=128.0, scalar2=pidx[:, 0:1], op0=ALU.mult, op1=ALU.add),
                     reads=[Ej, pidx], writes=[offbf])
                P.op("dve", lambda e: e.tensor_copy(out=OFFB[:], in_=offbf[:]), reads=[offbf], writes=[OFFB])
                P.op("dve", lambda e: e.tensor_copy(out=OFFD[:], in_=Ej[:]), reads=[Ej], writes=[OFFD])
                if dbg:
                    P.op("sp", lambda e: e.dma_start(out=RDBG[:, 0:128], in_=dest4f[:]), reads=[dest4f], dma=True)
                    P.op("sp", lambda e: e.dma_start(out=RDBG[:, 128:256], in_=G4[:].rearrange("p t k -> p (t k)")), reads=[G4], dma=True)
                    P.op("sp", lambda e: e.dma_start(out=RDBG[:, 256:256 + NBLK], in_=Ej[:]), reads=[Ej], dma=True)
                h2t = [sb(ph, "h2t%d" % i, [128, D], BF16) for i in range(3)]
                for t in range(32):
                    h_ = h2t[t % 3]
                    P.op("sp", lambda e: e.dma_start(out=h_[:], in_=H2TM[t * 128:(t + 1) * 128, :]), writes=[h_], dma=True)
                    for k in range(4):
                        P.op("pool", lambda e: e.indirect_dma_start(out=XS[:, :], out_offset=bass.IndirectOffsetOnAxis(ap=dest4u[:, t * 4 + k:t * 4 + k + 1], axis=0),
                                                                    in_=h_[:], in_offset=None), reads=[dest4u, h_], dma=True)
                P.barrier()

        mes.close()
        if upto >= 6:
            with ExitStack() as ph:
                wgb_ = [sb(ph, "wgub%d" % i, [128, 8, 2 * D], BF16) for i in range(2)]
                wdb_ = [sb(ph, "wdnb%d" % i, [128, 8, D], BF16) for i in range(2)]
                bgb_ = [sb(ph, "bgub%d" % i, [128, 16], F32) for i in range(2)]
                bdb_ = [sb(ph, "bdb%d" % i, [128, D], F32) for i in range(2)]
                xtok = [sb(ph, "xtok%d" % i, [128, 4, D], BF16) for i in range(1)]
                xT = [sb(ph, "xT%d" % i, [128, 8, 512], BF16) for i in range(2)]
                actT = [sb(ph, "actT%d" % i, [128, 8, 512], BF16) for i in range(2)]
                gbt = [sb(ph, "gbt%d" % i, [128, 512], F32) for i in range(2)]
                sig = [sb(ph, "sig%d" % i, [128, 512], F32) for i in range(2)]
                ubt = [sb(ph, "ubt%d" % i, [128, 512], F32) for i in range(2)]
                ys = [sb(ph, "ys%d" % i, [128, 512], F32) for i in range(12)]
                pTx = [ps(ph, "pTx%d" % i, [128, 8, 128], BF16) for i in range(2)]
                pg = [ps(ph, "pg%d" % i, [128, 512], F32) for i in range(2)]
                pu = [ps(ph, "pu%d" % i, [128, 512], F32) for i in range(2)]
                py = [ps(ph, "py%d" % i, [128, 512], F32) for i in range(2)]

                def ldblk(j):
                    b = j % 2
                    P.op("pool", lambda e: e.indirect_dma_start(out=wgb_[b][:].rearrange("p k n -> p (k n)"), out_offset=None,
                                                                in_=WGUB.rearrange("(r k) n -> r (k n)", k=8),
                                                                in_offset=bass.IndirectOffsetOnAxis(ap=OFFB[:, j:j + 1], axis=0)),
                         reads=[OFFB], writes=[wgb_[b]], dma=True)
                    P.op("pool", lambda e: e.indirect_dma_start(out=wdb_[b][:].rearrange("p k n -> p (k n)"), out_offset=None,
                                                                in_=WDNB.rearrange("(r k) n -> r (k n)", k=8),
                                                                in_offset=bass.IndirectOffsetOnAxis(ap=OFFB[:, j:j + 1], axis=0)),
                         reads=[OFFB], writes=[wdb_[b]], dma=True)
                    P.op("pool", lambda e: e.indirect_dma_start(out=bgb_[b][:], out_offset=None, in_=bgu_d[:, :],
                                                                in_offset=bass.IndirectOffsetOnAxis(ap=OFFB[:, j:j + 1], axis=0)),
                         reads=[OFFB], writes=[bgb_[b]], dma=True)
                    P.op("pool", lambda e: e.indirect_dma_start(out=bdb_[b][:], out_offset=None, in_=b_dn[:, :],
                                                                in_offset=bass.IndirectOffsetOnAxis(ap=OFFD[:, j:j + 1], axis=0)),
                         reads=[OFFD], writes=[bdb_[b]], dma=True)

                def ldx(j):
                    P.op("sp", lambda e: e.dma_start(out=xtok[0][:], in_=XS[j * BLK:(j + 1) * BLK, :].rearrange("(s p) d -> p s d", p=128)),
                         writes=[xtok[0]], dma=True)

                ldblk(0)
                ldx(0)
                fcn = 0
                yc = 0
                tcx = [0]

                def xpose(jj):
                    xk_ = xtok[0]
                    for sub in range(4):
                        p_ = pTx[tcx[0] % 2]
                        tcx[0] += 1
                        for kc in range(8):
                            P.op("pe", lambda e: e.transpose(out=p_[:, kc, :], in_=xk_[:, sub, kc * 128:(kc + 1) * 128], identity=ident[:]),
                                 reads=[xk_, ident], writes=[p_])
                        P.op("act", lambda e: e.copy(out=xT[jj % 2][:, :, sub * 128:(sub + 1) * 128], in_=p_[:]), reads=[p_], writes=[xT[jj % 2]])
                    if jj + 1 < NBLK:
                        ldx(jj + 1)
                for j in range(NBLK):
                    if j + 1 < NBLK:
                        ldblk(j + 1)
                    b = j % 2
                    wg_ = wgb_[b]
                    wd_ = wdb_[b]
                    bg_ = bgb_[b]
                    bd_ = bdb_[b]
                    xT_ = xT[b]
                    a_ = actT[b]
                    if j == 0:
                        xpose(0)
                    for fc in range(8):
                        g_p = pg[fcn % 2]
                        u_p = pu[fcn % 2]
                        gb_ = gbt[fcn % 2]
                        sg_ = sig[fcn % 2]
                        ub_ = ubt[fcn % 2]
                        fcn += 1
                        for kc in range(8):
                            P.op("pe", lambda e: e.matmul(g_p[:], lhsT=wg_[:, kc, fc * 128:(fc + 1) * 128], rhs=xT_[:, kc, :], start=(kc == 0), stop=(kc == 7)),
                                 reads=[wg_, xT_], writes=[g_p])
                        for kc in range(8):
                            P.op("pe", lambda e: e.matmul(u_p[:], lhsT=wg_[:, kc, D + fc * 128:D + (fc + 1) * 128], rhs=xT_[:, kc, :], start=(kc == 0), stop=(kc == 7)),
                                 reads=[wg_, xT_], writes=[u_p])
                        P.op("act", lambda e: e.activation(out=ub_[:], in_=u_p[:], func=AF.Identity, bias=bg_[:, 8 + fc:9 + fc], scale=1.0),
                             reads=[u_p, bg_], writes=[ub_])
                        P.op("dve", lambda e: e.tensor_scalar(out=gb_[:], in0=g_p[:], scalar1=bg_[:, fc:fc + 1], scalar2=7.0, op0=ALU.add, op1=ALU.min),
                             reads=[g_p, bg_], writes=[gb_])
                        P.op("act", lambda e: e.activation(out=sg_[:], in_=gb_[:], func=AF.Sigmoid, scale=1.702), reads=[gb_], writes=[sg_])
                        P.op("dve", lambda e: e.tensor_scalar(out=ub_[:], in0=ub_[:], scalar1=7.0, scalar2=-7.0, op0=ALU.min, op1=ALU.max), reads=[ub_], writes=[ub_])
                        P.op("dve", lambda e: e.tensor_tensor(out=gb_[:], in0=gb_[:], in1=sg_[:], op=ALU.mult), reads=[gb_, sg_], writes=[gb_])
                        P.op("dve", lambda e: e.scalar_tensor_tensor(out=a_[:, fc, :], in0=ub_[:], scalar=1.0, in1=gb_[:], op0=ALU.add, op1=ALU.mult),
                             reads=[ub_, gb_], writes=[a_])
                    if j + 1 < NBLK:
                        xpose(j + 1)
                    for sub in range(4):
                        for half in range(2):
                            y_p = py[yc % 2]
                            y_ = ys[yc % 12]
                            yc += 1
                            for fc in range(8):
                                P.op("pe", lambda e: e.matmul(y_p[:], lhsT=a_[:, fc, sub * 128:(sub + 1) * 128], rhs=wd_[:, fc, half * 512:(half + 1) * 512],
                                                              start=(fc == 0), stop=(fc == 7)), reads=[a_, wd_], writes=[y_p])
                            P.op("dve", lambda e: e.tensor_tensor(out=y_[:], in0=y_p[:], in1=bd_[:, half * 512:(half + 1) * 512], op=ALU.add),
                                 reads=[y_p, bd_], writes=[y_])
                            P.op("sp", lambda e: e.dma_start(out=YS[j * BLK + sub * 128:j * BLK + (sub + 1) * 128, half * 512:(half + 1) * 512], in_=y_[:]),
                                 reads=[y_], dma=True)
                P.barrier()

        if upto >= 6:
            with ExitStack() as ph:
                yk = [[sb(ph, "yk%d_%d" % (i, k), [128, D], F32) for k in range(4)] for i in range(2)]
                xt = [sb(ph, "xt7_%d" % i, [128, D], F32) for i in range(2)]
                acc = [sb(ph, "acc7_%d" % i, [128, D], F32) for i in range(2)]
                ot = [sb(ph, "ot7_%d" % i, [128, D], F32) for i in range(2)]
                s7 = [sb(ph, "s7_%d" % i, [128, 1], F32) for i in range(2)]
                st7 = [sb(ph, "st7_%d" % i, [128, 1], F32) for i in range(2)]

                def ld7(t):
                    for k in range(4):
                        P.op("pool", lambda e: e.indirect_dma_start(out=yk[t % 2][k][:], out_offset=None, in_=YS[:, :],
                                                                    in_offset=bass.IndirectOffsetOnAxis(ap=dest4u[:, t * 4 + k:t * 4 + k + 1], axis=0)),
                             reads=[dest4u], writes=[yk[t % 2][k]], dma=True)
                    P.op("sp", lambda e: e.dma_start(out=xt[t % 2][:], in_=X1[t * 128:(t + 1) * 128, :]), writes=[xt[t % 2]], dma=True)

                ld7(0)
                for t in range(32):
                    if t + 1 < 32:
                        ld7(t + 1)
                    y4 = yk[t % 2]
                    a_ = acc[t % 2]
                    o_ = ot[t % 2]
                    x_ = xt[t % 2]
                    s_ = s7[t % 2]
                    st_ = st7[t % 2]
                    P.op("dve", lambda e: e.tensor_scalar(out=a_[:], in0=y4[0][:], scalar1=G4[:, t, 0:1], scalar2=None, op0=ALU.mult), reads=[y4[0], G4], writes=[a_])
                    for k in range(1, 4):
                        P.op("dve", lambda e: e.scalar_tensor_tensor(out=a_[:], in0=y4[k][:], scalar=G4[:, t, k:k + 1], in1=a_[:], op0=ALU.mult, op1=ALU.add),
                             reads=[y4[k], G4, a_], writes=[a_])
                    P.op("act", lambda e: e.activation(out=o_[:], in_=a_[:], func=AF.Square, accum_out=s_[:, 0:1]), reads=[a_], writes=[o_, s_])
                    rstd_from_ss(s_, D, st_)
                    P.op("dve", lambda e: e.scalar_tensor_tensor(out=o_[:], in0=a_[:], scalar=s_[:, 0:1], in1=G2t[:], op0=ALU.mult, op1=ALU.mult),
                         reads=[a_, s_, G2t], writes=[o_])
                    P.op("dve", lambda e: e.tensor_tensor(out=o_[:], in0=o_[:], in1=x_[:], op=ALU.add), reads=[o_, x_], writes=[o_])
                    P.op("sp", lambda e: e.dma_start(out=y_out[t * 128:(t + 1) * 128, :], in_=o_[:]), reads=[o_], dma=True)
                P.barrier()
        P.barrier()
        print("ops", P.nops, "waits", P.nwaits, flush=True)
    return nc


def _host_inputs(inputs):
    x = np.asarray(inputs["x"], np.float32)
    c = np.asarray(inputs["c"], np.float32)
    inv = (10000.0 ** (-np.arange(0, 64, 2, dtype=np.float32) / np.float32(64))).astype(np.float32)
    shared = {
        "w_mod": np.ascontiguousarray(inputs["w_mod"][0], np.float32),
        "b_mod": np.ascontiguousarray(inputs["b_mod"][0:1], np.float32),
        "norm_pre_mix": np.ascontiguousarray(inputs["norm_pre_mix"][0:1], np.float32),
        "norm_post_mix": np.ascontiguousarray(inputs["norm_post_mix"][0:1], np.float32),
        "w_in": np.ascontiguousarray(inputs["w_in"][0], np.float32),
        "da_lambda_q1": np.ascontiguousarray(inputs["da_lambda_q1"][0:1], np.float32),
        "da_lambda_k1": np.ascontiguousarray(inputs["da_lambda_k1"][0:1], np.float32),
        "da_lambda_q2": np.ascontiguousarray(inputs["da_lambda_q2"][0:1], np.float32),
        "da_lambda_k2": np.ascontiguousarray(inputs["da_lambda_k2"][0:1], np.float32),
        "da_subln": np.ascontiguousarray(inputs["da_subln"][0:1], np.float32),
        "subln_col": np.ascontiguousarray(np.asarray(inputs["da_subln"], np.float32)[0].reshape(128, 1)),
        "lbl": np.ascontiguousarray(np.asarray(inputs["hg_lb_logits"], np.float32).reshape(2, 4, 128).transpose(2, 0, 1)),
        "hg_norm": np.ascontiguousarray(inputs["hg_norm"][0:1], np.float32),
        "w_branch_a": np.ascontiguousarray(inputs["w_branch_a"][0], np.float32),
        "w_branch_b": np.ascontiguousarray(inputs["w_branch_b"][0], np.float32),
        "w_out": np.ascontiguousarray(inputs["w_out"][0], np.float32),
        "norm_pre_ffn": np.ascontiguousarray(inputs["norm_pre_ffn"][0:1], np.float32),
        "norm_post_ffn": np.ascontiguousarray(inputs["norm_post_ffn"][0:1], np.float32),
        "router_w": np.ascontiguousarray(inputs["router_w"][0], np.float32),
        "router_b": np.ascontiguousarray(inputs["router_b"][0:1], np.float32),
        "w_gate_up": np.ascontiguousarray(np.asarray(inputs["w_gate_up"][0], np.float32).reshape(NE * D, 2 * D)),
        "bgu": np.ascontiguousarray(np.asarray(inputs["b_gate_up"][0], np.float32).reshape(NE, 16, 128).transpose(0, 2, 1).reshape(NE * 128, 16)),
        "w_down": np.ascontiguousarray(np.asarray(inputs["w_down"][0], np.float32).reshape(NE * D, D)),
        "b_down": np.ascontiguousarray(inputs["b_down"][0], np.float32),
    }
    in_maps = []
    p = np.arange(128)
    sign = np.where((p % 64) < 32, -1.0, 1.0).astype(np.float32)[:, None]
    for core in range(8):
        b, hf = core // 2, core % 2
        if hf == 1:
            xall = x[b]
            pos = np.arange(SEQ, dtype=np.float32)
        else:
            xall = np.concatenate([x[b, :TOWN], x[b, :TOWN]], axis=0)
            pos = np.concatenate([np.arange(TOWN), np.arange(TOWN)]).astype(np.float32)
        ang = (pos[None, :] * inv[p % 32][:, None]).astype(np.float32)
        m = dict(shared)
        m["xall"] = np.ascontiguousarray(xall)
        m["cosT"] = np.ascontiguousarray(np.cos(ang).astype(np.float32))
        m["sinT"] = np.ascontiguousarray((np.sin(ang) * sign).astype(np.float32))
        m["flag"] = np.full((128, 1), float(hf), np.float32)
        m["c2"] = np.ascontiguousarray(c[b].reshape(8, 128).T)
        in_maps.append(m)
    return in_maps


_NC_CACHE = {}


def kernel(**inputs):
    in_maps = _host_inputs(inputs)
    if "nc" not in _NC_CACHE:
        _NC_CACHE["nc"] = build_nc()
    nc = _NC_CACHE["nc"]
    res = run_bass_kernel_spmd(nc, in_maps, core_ids=list(range(8)))
    out = np.empty((NB, SEQ, D), np.float32)
    for core in range(8):
        b, hf = core // 2, core % 2
        out[b, hf * TOWN:(hf + 1) * TOWN] = res.results[core]["y"]
    return out
```
